# Optimizing a Trainium2 kernel written in Bass

```python
import math
import jax, jax.numpy as jnp
from jax import lax
import numpy as np

D_MODEL = 1024
BATCH = 2
SEQ = 8192
DEPTH = 2

RW_HEAD = 64
RW_WIDTH = D_MODEL // 2
RW_HEADS = RW_WIDTH // RW_HEAD
RW_DECAY_LORA = 64
RW_AAA_LORA = 64
RW_GATE_LORA = 128
RW_GN_EPS = 64e-5
RW_IN = 3 * RW_WIDTH + RW_DECAY_LORA + RW_AAA_LORA + RW_GATE_LORA
GLA_HEADS = 4
GLA_DK = 64
GLA_DV = 128
GLA_GATE_LORA = 16
GLA_GATE_TAU = 16.0
GLA_IN = 2 * GLA_HEADS * GLA_DK + 2 * GLA_HEADS * GLA_DV + GLA_GATE_LORA
S5_GROUP = 16
S5_WIDTH = D_MODEL // 2
S5_GROUPS = S5_WIDTH // S5_GROUP
S5_STATE = 64
HG_DK = 128
HG_WIDTH = D_MODEL // 2
HG_HEADS = HG_WIDTH // HG_DK
HG_DV = HG_WIDTH // HG_HEADS
CD_IN = S5_WIDTH + 4 * HG_WIDTH
CHUNK = 64
NORM_EPS = 1e-5
MOE_GROUPS = 4
MOE_PER_GROUP = 8
N_EXPERTS = MOE_GROUPS * MOE_PER_GROUP
EXPERT_HIDDEN = 512
TOP_K = 2
MOE_BLOCK = 128
DN_ALPHA = (2.0 * DEPTH) ** 0.25
DN_BETA = (8.0 * DEPTH) ** -0.25
N_EVEN = (DEPTH + 1) // 2
N_ODD = DEPTH // 2

kernel_name = "hybrid_rwkv7_gla_s5_hgrn2_hmoe_deepnorm"

F32 = jnp.float32


def _split(t, sizes):
    return jnp.split(t, np.cumsum(sizes)[:-1].tolist(), axis=-1)


def _heads(t, n):
    return t.reshape(t.shape[:-1] + (n, t.shape[-1] // n))


def _layer_norm(x, g, b):
    xf = x.astype(F32)
    mu = jnp.mean(xf, -1, keepdims=True)
    var = jnp.mean(jnp.square(xf - mu), -1, keepdims=True)
    return ((xf - mu) * lax.rsqrt(var + NORM_EPS) * g + b).astype(x.dtype)


def _gated_head_rmsnorm(o, gate, g):
    of = o.astype(F32)
    of = of * lax.rsqrt(jnp.mean(of * of, -1, keepdims=True) + NORM_EPS) * g
    out = of * jax.nn.silu(gate.astype(F32))
    return out.reshape(out.shape[:-2] + (-1,))


def _chunked_gated_linear_attention(q, k, v, log_g):
    B_, L, H, K = q.shape
    V = v.shape[-1]
    n = L // CHUNK

    def to_chunks(t):
        t = t.astype(F32).reshape(B_, n, CHUNK, H, t.shape[-1])
        return jnp.transpose(t, (1, 0, 3, 2, 4))

    qc, kc, vc, gc = (to_chunks(t) for t in (q, k, v, log_g))
    causal = jnp.tril(jnp.ones((CHUNK, CHUNK), bool))[:, :, None]

    def step(S, inp):
        q_c, k_c, v_c, g_c = inp
        b = jnp.cumsum(g_c, axis=2)
        b_last = b[:, :, -1]
        o_inter = jnp.einsum('bhck,bhkv->bhcv', q_c * jnp.exp(b), S)
        diff = b[:, :, :, None, :] - b[:, :, None, :, :]
        decay = jnp.exp(jnp.where(causal, diff, -jnp.inf))
        scores = jnp.einsum('bhik,bhjk,bhijk->bhij', q_c, k_c, decay)
        o = o_inter + jnp.einsum('bhij,bhjv->bhiv', scores, v_c)
        k_dec = k_c * jnp.exp(b_last[:, :, None, :] - b)
        S = jnp.exp(b_last)[..., None] * S + jnp.einsum('bhjk,bhjv->bhkv', k_dec, v_c)
        return S, o

    S0 = jnp.zeros((B_, H, K, V), F32)
    _, o = lax.scan(step, S0, (qc, kc, vc, gc))
    return jnp.transpose(o, (1, 0, 3, 2, 4)).reshape(B_, L, H, V).astype(v.dtype)


def _rwkv7_time_mix(p, mu, w0, w2, a0, a2, g2, k_k, k_a, r_k, gn_g, gn_b):
    B_, L, _ = p.shape
    prev = jnp.pad(p, ((0, 0), (1, 0), (0, 0)))[:, :-1]
    p = p + mu * (prev - p)
    r, wl, k, v, al, gl = _split(p, [RW_WIDTH, RW_DECAY_LORA, RW_WIDTH, RW_WIDTH, RW_AAA_LORA, RW_GATE_LORA])
    w = -jax.nn.softplus(-(w0 + jnp.tanh(wl) @ w2)) - 0.5
    a = jax.nn.sigmoid(a0 + al @ a2)
    g = jax.nn.sigmoid(gl) @ g2
    kk = _heads((k * k_k).astype(F32), RW_HEADS)
    kk = kk / jnp.maximum(jnp.sqrt(jnp.sum(kk * kk, -1, keepdims=True)), 1e-12)
    k = k * (1.0 + (a - 1.0) * k_a)
    rh, kh, vh, ah = (_heads(t.astype(F32), RW_HEADS) for t in (r, k, v, a))
    decay = jnp.exp(-jnp.exp(_heads(w.astype(F32), RW_HEADS)))
    xs = tuple(jnp.moveaxis(t, 1, 0) for t in (rh, decay, kh, vh, -kk, kk * ah))

    def step(S, inp):
        r_t, w_t, k_t, v_t, a_t, b_t = inp
        sa = jnp.einsum('bhvk,bhk->bhv', S, a_t)
        S = S * w_t[:, :, None, :] + sa[..., None] * b_t[:, :, None, :] + v_t[..., None] * k_t[:, :, None, :]
        return S, jnp.einsum('bhvk,bhk->bhv', S, r_t)

    S0 = jnp.zeros((B_, RW_HEADS, RW_HEAD, RW_HEAD), F32)
    _, y = lax.scan(step, S0, xs)
    y = jnp.moveaxis(y, 0, 1)
    mean = jnp.mean(y, -1, keepdims=True)
    var = jnp.mean(jnp.square(y - mean), -1, keepdims=True)
    y = ((y - mean) * lax.rsqrt(var + RW_GN_EPS)).reshape(B_, L, RW_WIDTH) * gn_g + gn_b
    bonus = (jnp.sum(rh * kh * r_k, -1, keepdims=True) * vh).reshape(B_, L, RW_WIDTH)
    return ((y + bonus) * g).astype(p.dtype)


def _gla(q, k, v, gk_low, gate, gk_w2, gk_b, norm_g):
    log_a = jax.nn.log_sigmoid((gk_low @ gk_w2 + gk_b).astype(F32)) / GLA_GATE_TAU
    o = _chunked_gated_linear_attention(
        _heads(q, GLA_HEADS) * (GLA_DK ** -0.5), _heads(k, GLA_HEADS),
        _heads(v, GLA_HEADS), _heads(log_a, GLA_HEADS))
    return _gated_head_rmsnorm(o, _heads(gate, GLA_HEADS), norm_g).astype(q.dtype)


def _hgrn2(q, f, i, gate, lb, norm_g):
    ff = f.astype(F32)
    forget = lb + (1.0 - lb) * jax.nn.sigmoid(ff)
    inp_gate = (1.0 - lb) * jax.nn.sigmoid(-ff)
    o = _chunked_gated_linear_attention(
        _heads(jax.nn.silu(q), HG_HEADS), _heads(inp_gate, HG_HEADS),
        _heads(i, HG_HEADS), _heads(jnp.log(forget), HG_HEADS))
    return _gated_head_rmsnorm(o, _heads(gate, HG_HEADS), norm_g).astype(q.dtype)


def _s5(u, a_re, a_im, log_dt, b_re, b_im, c_re, c_im, d, glu_w, glu_b):
    B_, L, _ = u.shape
    uf = u.astype(F32)
    ug = uf.reshape(B_, L, S5_GROUPS, S5_GROUP)
    lam_re = jnp.minimum(a_re.astype(F32), -1e-4)
    lam_im = a_im.astype(F32)
    dt = jnp.exp(log_dt.astype(F32))[:, None]
    mag = jnp.exp(lam_re * dt)
    abar_re = mag * jnp.cos(lam_im * dt)
    abar_im = mag * jnp.sin(lam_im * dt)
    den = lam_re * lam_re + lam_im * lam_im
    num_re = abar_re - 1.0
    z_re = (num_re * lam_re + abar_im * lam_im) / den
    z_im = (abar_im * lam_re - num_re * lam_im) / den
    bu_re = jnp.einsum('blgc,gnc->blgn', ug, b_re.astype(F32))
    bu_im = jnp.einsum('blgc,gnc->blgn', ug, b_im.astype(F32))
    x_re = z_re * bu_re - z_im * bu_im
    x_im = z_re * bu_im + z_im * bu_re
    at_re = jnp.broadcast_to(abar_re, x_re.shape)
    at_im = jnp.broadcast_to(abar_im, x_im.shape)

    def combine(e1, e2):
        a1r, a1i, b1r, b1i = e1
        a2r, a2i, b2r, b2i = e2
        return (a2r * a1r - a2i * a1i, a2r * a1i + a2i * a1r,
                a2r * b1r - a2i * b1i + b2r, a2r * b1i + a2i * b1r + b2i)

    _, _, h_re, h_im = lax.associative_scan(combine, (at_re, at_im, x_re, x_im), axis=1)
    y = (jnp.einsum('blgn,gcn->blgc', h_re, c_re.astype(F32))
         - jnp.einsum('blgn,gcn->blgc', h_im, c_im.astype(F32)))
    y = y.reshape(B_, L, S5_WIDTH) + d * uf
    y = jax.nn.gelu(y)
    y = y * jax.nn.sigmoid(y @ glu_w.astype(F32) + glu_b)
    return y.astype(u.dtype)


def _mixer_rwkv7_gla(h, w_in, rw_mu, rw_w0, rw_w2, rw_a0, rw_a2, rw_g2, rw_k_k, rw_k_a,
                     rw_r_k, rw_gn_g, rw_gn_b, gla_gk_w2, gla_gk_b, gla_norm_g, w_out):
    p = h @ w_in
    p_rw, q, k, v, gk_low, gate = _split(
        p, [RW_IN, GLA_HEADS * GLA_DK, GLA_HEADS * GLA_DK, GLA_HEADS * GLA_DV, GLA_GATE_LORA, GLA_HEADS * GLA_DV])
    y_rw = _rwkv7_time_mix(p_rw, rw_mu, rw_w0, rw_w2, rw_a0, rw_a2, rw_g2, rw_k_k, rw_k_a, rw_r_k, rw_gn_g, rw_gn_b)
    y_gla = _gla(q, k, v, gk_low, gate, gla_gk_w2, gla_gk_b, gla_norm_g)
    return jnp.concatenate([y_rw, y_gla], -1).astype(h.dtype) @ w_out


def _mixer_s5_hgrn2(h, w_in, s5_a_re, s5_a_im, s5_log_dt, s5_b_re, s5_b_im, s5_c_re, s5_c_im,
                    s5_d, s5_glu_w, s5_glu_b, lb, hg_norm_g, w_out):
    p = h @ w_in
    u, q, f, i, gate = _split(p, [S5_WIDTH, HG_WIDTH, HG_WIDTH, HG_WIDTH, HG_WIDTH])
    y_s5 = _s5(u, s5_a_re, s5_a_im, s5_log_dt, s5_b_re, s5_b_im, s5_c_re, s5_c_im, s5_d, s5_glu_w, s5_glu_b)
    y_hg = _hgrn2(q, f, i, gate, lb, hg_norm_g)
    return jnp.concatenate([y_s5, y_hg], -1).astype(h.dtype) @ w_out


def _expert_dispatch(xt, expert, gate, w1, w3, w2):
    T, D = xt.shape
    A = T * TOP_K
    e_flat = expert.reshape(A)
    tok_flat = jnp.repeat(jnp.arange(T, dtype=jnp.int32), TOP_K)
    g_flat = gate.reshape(A)
    order = jnp.argsort(e_flat)
    e_s, tok_s, g_s = e_flat[order], tok_flat[order], g_flat[order]
    counts = jnp.bincount(e_flat, length=N_EXPERTS)
    padded = (counts + MOE_BLOCK - 1) // MOE_BLOCK * MOE_BLOCK
    start = jnp.cumsum(counts) - counts
    pend = jnp.cumsum(padded)
    pstart = pend - padded
    dest = pstart[e_s] + (jnp.arange(A, dtype=jnp.int32) - start[e_s])
    n_blocks = -(-A // MOE_BLOCK) + N_EXPERTS
    R = n_blocks * MOE_BLOCK
    row_tok = jnp.full((R,), T, jnp.int32).at[dest].set(tok_s)
    x_pad = jnp.concatenate([xt, jnp.zeros((1, D), xt.dtype)], 0)
    x_rows = x_pad[row_tok].reshape(n_blocks, MOE_BLOCK, D)
    block_e = jnp.minimum(jnp.searchsorted(pend, jnp.arange(n_blocks) * MOE_BLOCK, side='right'), N_EXPERTS - 1)

    def run_block(args):
        xb, e = args
        hid = jax.nn.silu(xb @ w1[e]) * (xb @ w3[e])
        return hid @ w2[e]

    y_rows = lax.map(run_block, (x_rows, block_e)).reshape(R, D)
    y = jnp.zeros((T, D), F32).at[tok_s].add(g_s[:, None] * y_rows[dest].astype(F32))
    return y.astype(xt.dtype)


def _hierarchical_moe(x, wg, bg, we, be, w1, w3, w2):
    B_, L, D = x.shape
    T = B_ * L
    xt = x.reshape(T, D)
    xf = xt.astype(F32)
    coarse = jax.nn.softmax(xf @ wg.astype(F32) + bg, axis=-1)
    p_grp, grp = lax.top_k(coarse, 1)
    grp = grp[:, 0]
    fine_all = jnp.einsum('td,gde->tge', xf, we.astype(F32)) + be
    fine = jax.nn.softmax(fine_all[jnp.arange(T), grp], axis=-1)
    top_p, top_j = lax.top_k(fine, TOP_K)
    gate = p_grp * (top_p / jnp.sum(top_p, -1, keepdims=True))
    expert = grp[:, None] * MOE_PER_GROUP + top_j
    return _expert_dispatch(xt, expert, gate, w1, w3, w2).reshape(B_, L, D)


def setup_inputs(seed: int = 0) -> dict:
    key = jax.random.key(seed)
    keys = jax.random.split(key, 48)
    ks = iter([keys[n] for n in range(48)])

    def nrm(shape, scale):
        return jax.random.normal(next(ks), shape, F32) * scale

    E, O = N_EVEN, N_ODD
    ratio = jnp.arange(RW_WIDTH, dtype=F32) / (RW_WIDTH - 1)
    w0_base = -6.5 + 5.0 * ratio ** 0.85
    n_idx = jnp.arange(S5_STATE, dtype=F32)
    return {
        "x": nrm((BATCH, SEQ, D_MODEL), 1.0),
        "ab_w_in": nrm((E, D_MODEL, RW_IN + GLA_IN), D_MODEL ** -0.5),
        "rw_mu": jax.random.uniform(next(ks), (E, RW_IN), F32),
        "rw_w0": w0_base + nrm((E, RW_WIDTH), 0.1),
        "rw_w2": nrm((E, RW_DECAY_LORA, RW_WIDTH), 0.5 * RW_DECAY_LORA ** -0.5),
        "rw_a0": nrm((E, RW_WIDTH), 0.1),
        "rw_a2": nrm((E, RW_AAA_LORA, RW_WIDTH), RW_AAA_LORA ** -0.5),
        "rw_g2": nrm((E, RW_GATE_LORA, RW_WIDTH), RW_GATE_LORA ** -0.5),
        "rw_k_k": 0.85 + nrm((E, RW_WIDTH), 0.02),
        "rw_k_a": 1.0 + nrm((E, RW_WIDTH), 0.02),
        "rw_r_k": -0.04 + nrm((E, RW_HEADS, RW_HEAD), 0.1),
        "rw_gn_g": 1.0 + nrm((E, RW_WIDTH), 0.02),
        "rw_gn_b": nrm((E, RW_WIDTH), 0.02),
        "gla_gk_w2": nrm((E, GLA_GATE_LORA, GLA_HEADS * GLA_DK), GLA_GATE_LORA ** -0.5),
        "gla_gk_b": nrm((E, GLA_HEADS * GLA_DK), 0.02),
        "gla_norm_g": 1.0 + nrm((E, GLA_DV), 0.02),
        "ab_w_out": nrm((E, D_MODEL, D_MODEL), DN_BETA * D_MODEL ** -0.5),
        "cd_w_in": nrm((O, D_MODEL, CD_IN), D_MODEL ** -0.5),
        "s5_a_re": -0.5 + nrm((O, S5_GROUPS, S5_STATE), 0.01),
        "s5_a_im": math.pi * n_idx + nrm((O, S5_GROUPS, S5_STATE), 0.01),
        "s5_log_dt": jax.random.uniform(next(ks), (O, S5_GROUPS), F32, math.log(1e-3), math.log(1e-1)),
        "s5_b_re": nrm((O, S5_GROUPS, S5_STATE, S5_GROUP), (2 * S5_GROUP) ** -0.5),
        "s5_b_im": nrm((O, S5_GROUPS, S5_STATE, S5_GROUP), (2 * S5_GROUP) ** -0.5),
        "s5_c_re": nrm((O, S5_GROUPS, S5_GROUP, S5_STATE), S5_STATE ** -0.5),
        "s5_c_im": nrm((O, S5_GROUPS, S5_GROUP, S5_STATE), S5_STATE ** -0.5),
        "s5_d": nrm((O, S5_WIDTH), 1.0),
        "s5_glu_w": nrm((O, S5_WIDTH, S5_WIDTH), S5_WIDTH ** -0.5),
        "s5_glu_b": nrm((O, S5_WIDTH), 0.02),
        "hg_lb": 1.0 + nrm((DEPTH, HG_WIDTH), 0.1),
        "hg_norm_g": 1.0 + nrm((O, HG_DV), 0.02),
        "cd_w_out": nrm((O, D_MODEL, D_MODEL), DN_BETA * D_MODEL ** -0.5),
        "ln1_g": 1.0 + nrm((DEPTH, D_MODEL), 0.02),
        "ln1_b": nrm((DEPTH, D_MODEL), 0.02),
        "moe_wg": nrm((DEPTH, D_MODEL, MOE_GROUPS), D_MODEL ** -0.5),
        "moe_bg": nrm((DEPTH, MOE_GROUPS), 0.01),
        "moe_we": nrm((DEPTH, MOE_GROUPS, D_MODEL, MOE_PER_GROUP), D_MODEL ** -0.5),
        "moe_be": nrm((DEPTH, MOE_GROUPS, MOE_PER_GROUP), 0.01),
        "moe_w1": nrm((DEPTH, N_EXPERTS, D_MODEL, EXPERT_HIDDEN), D_MODEL ** -0.5),
        "moe_w3": nrm((DEPTH, N_EXPERTS, D_MODEL, EXPERT_HIDDEN), D_MODEL ** -0.5),
        "moe_w2": nrm((DEPTH, N_EXPERTS, EXPERT_HIDDEN, D_MODEL), DN_BETA * EXPERT_HIDDEN ** -0.5),
        "ln2_g": 1.0 + nrm((DEPTH, D_MODEL), 0.02),
        "ln2_b": nrm((DEPTH, D_MODEL), 0.02),
    }


def reference(x, ab_w_in, rw_mu, rw_w0, rw_w2, rw_a0, rw_a2, rw_g2, rw_k_k, rw_k_a, rw_r_k,
              rw_gn_g, rw_gn_b, gla_gk_w2, gla_gk_b, gla_norm_g, ab_w_out,
              cd_w_in, s5_a_re, s5_a_im, s5_log_dt, s5_b_re, s5_b_im, s5_c_re, s5_c_im,
              s5_d, s5_glu_w, s5_glu_b, hg_lb, hg_norm_g, cd_w_out,
              ln1_g, ln1_b, moe_wg, moe_bg, moe_we, moe_be, moe_w1, moe_w3, moe_w2, ln2_g, ln2_b):
    h = x
    lb_sm = jax.nn.softmax(hg_lb.astype(F32), axis=0)
    lower_bounds = jnp.cumsum(lb_sm, axis=0) - lb_sm[0]
    for layer in range(DEPTH):
        j = layer // 2
        if layer % 2 == 0:
            mix = _mixer_rwkv7_gla(h, ab_w_in[j], rw_mu[j], rw_w0[j], rw_w2[j], rw_a0[j], rw_a2[j], rw_g2[j],
                                   rw_k_k[j], rw_k_a[j], rw_r_k[j], rw_gn_g[j], rw_gn_b[j],
                                   gla_gk_w2[j], gla_gk_b[j], gla_norm_g[j], ab_w_out[j])
        else:
            mix = _mixer_s5_hgrn2(h, cd_w_in[j], s5_a_re[j], s5_a_im[j], s5_log_dt[j], s5_b_re[j], s5_b_im[j],
                                  s5_c_re[j], s5_c_im[j], s5_d[j], s5_glu_w[j], s5_glu_b[j],
                                  lower_bounds[layer], hg_norm_g[j], cd_w_out[j])
        h = _layer_norm(DN_ALPHA * h + mix, ln1_g[layer], ln1_b[layer])
        ffn = _hierarchical_moe(h, moe_wg[layer], moe_bg[layer], moe_we[layer], moe_be[layer],
                                moe_w1[layer], moe_w3[layer], moe_w2[layer])
        h = _layer_norm(DN_ALPHA * h + ffn, ln2_g[layer], ln2_b[layer])
    return h
```

```python
import numpy as np
from contextlib import ExitStack
import concourse.bass as bass
import concourse.mybir as mybir
from concourse.bass_utils import run_bass_kernel_spmd

F32 = mybir.dt.float32
BF16 = mybir.dt.bfloat16
I32 = mybir.dt.int32
AF = mybir.ActivationFunctionType
ALU = mybir.AluOpType
AX = mybir.AxisListType

class Buf:
    def __init__(self, t, name):
        self.t = t; self.name = name
        self.w = None
        self.r = {}
    def __getitem__(self, key):
        return self.t[key]

class KB:
    def __init__(self, nc, n_dma_sems=32):
        self.nc = nc
        self.es = ExitStack()
        self.scopes = [self.es]
        self.eng = {'pe': nc.tensor, 'act': nc.scalar, 'dve': nc.vector, 'pool': nc.gpsimd, 'sp': nc.sync}
        self.sem = {}
        self.cnt = {}
        for e in self.eng:
            self.sem[e] = self.es.enter_context(nc.semaphore('s_' + e))
            self.cnt[e] = 0
        self.nd = n_dma_sems
        for i in range(n_dma_sems):
            self.sem['d%d' % i] = self.es.enter_context(nc.semaphore('s_d%d' % i))
            self.cnt['d%d' % i] = 0
        self.dma_rr = 0
        self.seen = {e: {} for e in self.eng}
        self.nbuf = 0
    def sb(self, shape, dtype=F32, name=None):
        self.nbuf += 1
        name = ('%s_%d' % (name, self.nbuf)) if name else ('b%d' % self.nbuf)
        t = self.scopes[-1].enter_context(self.nc.sbuf_tensor(name, list(shape), dtype))
        return Buf(t, name)
    def ps(self, shape, dtype=F32, name=None):
        self.nbuf += 1
        name = ('%s_%d' % (name, self.nbuf)) if name else ('p%d' % self.nbuf)
        t = self.scopes[-1].enter_context(self.nc.psum_tensor(name, list(shape), dtype))
        return Buf(t, name)
    def _need(self, e, key, val):
        if e == 'pe' and key == 'pe':
            return
        if val <= self.seen[e].get(key, 0):
            return
        self.eng[e].wait_ge(self.sem[key], val)
        self.seen[e][key] = val
    def _deps(self, e, r, w):
        for b in r:
            if b.w is not None:
                self._need(e, *b.w)
        for b in w:
            if b.w is not None:
                self._need(e, *b.w)
            for key, val in b.r.items():
                self._need(e, key, val)
    def op(self, e, ins_fn, r=(), w=()):
        self._deps(e, r, w)
        ins = ins_fn(self.eng[e])
        self.cnt[e] += 1
        ins.then_inc(self.sem[e], 1)
        v = self.cnt[e]
        for b in r:
            b.r[e] = v
        for b in w:
            b.w = (e, v); b.r = {}
        return ins
    def dma(self, out_ap, in_ap, r=(), w=(), q='sp', **kw):
        self._deps(q, r, w)
        key = 'd%d' % self.dma_rr
        self.dma_rr = (self.dma_rr + 1) % self.nd
        if self.cnt[key] > 0:
            self._need(q, key, self.cnt[key])
        ins = self.eng[q].dma_start(out=out_ap, in_=in_ap, **kw)
        self.cnt[key] += 16
        ins.then_inc(self.sem[key], 16)
        v = self.cnt[key]
        for b in r:
            b.r[key] = v
        for b in w:
            b.w = (key, v); b.r = {}
        return ins
    def push(self):
        self.scopes.append(ExitStack())
    def pop(self):
        self.barrier()
        self.scopes.pop().close()
    def allgather(self, src_ap, dst_ap, dstbuf, groups):
        for key in list(self.cnt):
            if key.startswith('d') and self.cnt[key] > 0:
                self._need('pool', key, self.cnt[key])
        for key, val in dstbuf.r.items():
            self._need('pool', key, val)
        key = 'cc%d' % len([x for x in self.sem if x.startswith('cc')])
        sem = self.es.enter_context(self.nc.semaphore('s_' + key))
        ins = self.nc.gpsimd.collective_compute("AllGather", ALU.bypass, replica_groups=groups, ins=[src_ap.opt()], outs=[dst_ap.opt()])
        ins.then_inc(sem)
        self.sem[key] = sem; self.cnt[key] = 1
        dstbuf.w = (key, 1); dstbuf.r = {}
    def ind(self, out, out_off, in_, in_off, r=(), w=(), bounds=None):
        self._deps('pool', r, w)
        key = 'd%d' % self.dma_rr
        self.dma_rr = (self.dma_rr + 1) % self.nd
        if self.cnt[key] > 0:
            self._need('pool', key, self.cnt[key])
        kw = {}
        if bounds is not None:
            if not hasattr(self, '_breg'):
                self._breg = {}
            if bounds not in self._breg:
                self._breg[bounds] = self.nc.gpsimd.alloc_register('bnd%d' % len(self._breg))
            self.nc.gpsimd.reg_mov(self._breg[bounds], int(bounds))
            kw['bounds_check'] = self._breg[bounds]; kw['oob_is_err'] = False
        ins = self.nc.gpsimd.indirect_dma_start(out=out, out_offset=out_off, in_=in_, in_offset=in_off, **kw)
        self.cnt[key] += 16
        ins.then_inc(self.sem[key], 16)
        v = self.cnt[key]
        for b in r:
            b.r[key] = v
        for b in w:
            b.w = (key, v); b.r = {}
        return ins
    def finish(self):
        for i in range(self.nd):
            key = 'd%d' % i
            if self.cnt[key] > 0:
                self._need('sp', key, self.cnt[key])
    def close(self):
        self.es.close()

def _mm(self, out, lhsT, rhs, start=True, stop=True, r=(), w=()):
    return self.op('pe', lambda e: e.matmul(out, lhsT=lhsT, rhs=rhs, start=start, stop=stop), r=r, w=w)
def _tr(self, out, in_, ident, r=(), w=()):
    return self.op('pe', lambda e: e.transpose(out=out, in_=in_, identity=ident), r=r, w=w)
def _act(self, out, in_, func, bias=None, scale=1.0, accum_out=None, r=(), w=()):
    kw = {}
    if bias is not None: kw['bias'] = bias
    if accum_out is not None: kw['accum_out'] = accum_out
    return self.op('act', lambda e: e.activation(out=out, in_=in_, func=func, scale=scale, **kw), r=r, w=w)
def _tt(self, out, in0, in1, op, r=(), w=(), e='dve'):
    return self.op(e, lambda g: g.tensor_tensor(out=out, in0=in0, in1=in1, op=op), r=r, w=w)
def _ts(self, out, in0, s1, s2=None, op0=ALU.mult, op1=None, r=(), w=(), e='dve', accum_out=None):
    kw = {}
    if op1 is not None: kw['op1'] = op1
    if accum_out is not None: kw['accum_out'] = accum_out
    return self.op(e, lambda g: g.tensor_scalar(out=out, in0=in0, scalar1=s1, scalar2=s2, op0=op0, **kw), r=r, w=w)
def _stt(self, out, in0, scalar, in1, op0, op1, r=(), w=()):
    return self.op('dve', lambda g: g.scalar_tensor_tensor(out=out, in0=in0, scalar=scalar, in1=in1, op0=op0, op1=op1), r=r, w=w)
def _cp(self, out, in_, r=(), w=(), e='dve'):
    if e == 'act':
        return self.op('act', lambda g: g.activation(out=out, in_=in_, func=AF.Copy), r=r, w=w)
    return self.op(e, lambda g: g.tensor_copy(out=out, in_=in_), r=r, w=w)
def _barrier(self):
    for e in self.eng:
        for key in self.cnt:
            if key != e and self.cnt[key] > 0:
                self._need(e, key, self.cnt[key])
KB.mm = _mm; KB.tr = _tr; KB.act = _act; KB.tt = _tt; KB.ts = _ts; KB.stt = _stt; KB.cp = _cp; KB.barrier = _barrier


ALPHA = 4.0 ** 0.25
DEBUG = False
GBLK = 512
EPS = 1e-5

def layer_norm(k, src, dst, gbc, bbc, tmp):
    st, mv, rs = tmp
    k.op('dve', lambda e: e.bn_stats(out=st[:, 0:6], in_=src[:, 0:512]), r=[src], w=[st])
    k.op('dve', lambda e: e.bn_stats(out=st[:, 6:12], in_=src[:, 512:1024]), r=[src], w=[st])
    k.op('dve', lambda e: e.bn_aggr(out=mv[:, 0:2], in_=st[:, 0:12]), r=[st], w=[mv])
    k.act(rs[:, 0:1], mv[:, 1:2], AF.Sqrt, bias=k.eps_ap, r=[mv], w=[rs])
    k.op('dve', lambda e: e.reciprocal(out=rs[:, 1:2], in_=rs[:, 0:1]), r=[rs], w=[rs])
    k.ts(dst[:, :], src[:, :], mv[:, 0:1], rs[:, 1:2], op0=ALU.subtract, op1=ALU.mult, r=[src, mv, rs], w=[dst])
    k.tt(dst[:, :], dst[:, :], gbc[:, :], ALU.mult, r=[dst, gbc], w=[dst])
    k.tt(dst[:, :], dst[:, :], bbc[:, :], ALU.add, r=[dst, bbc], w=[dst])

def post_consts(NT):
    G = min(GBLK, NT); J = max(1, NT // G); NOV = (2 * NT) // G if J > 1 else 0; NBLK = 32 + NOV
    c = {}
    c["ident"] = np.eye(128, dtype=np.float32)
    c["ut"] = np.triu(np.ones((128, 128), np.float32), 1)
    c["ones"] = np.ones((128, 128), np.float32)
    c["thr"] = np.tile(((np.arange(max(J - 1, 1), dtype=np.float32) + 1) * G)[None, None, :], (128, 32, 1)).reshape(128, 32 * max(J - 1, 1))
    c["biota"] = np.tile(np.arange(max(NOV, 1), dtype=np.float32)[None, :, None], (128, 1, 32)).reshape(128, max(NOV, 1) * 32)
    c["eiota"] = np.tile((np.arange(32, dtype=np.float32) * G)[None, :], (128, 1))
    c["iotap"] = np.arange(128, dtype=np.float32)[:, None].copy()
    return c

def build_post(NT, layer1, NE=32, ctx=None):
    own = ctx is None
    nc = bass.Bass("TRN2", target_bir_lowering=False) if own else ctx['nc']
    pfx = '' if own else ctx['pfx']
    over = {} if own else ctx['over']
    D = 1024
    ntile = NT // 128
    TG = min(GBLK, NT); ntg = NT // TG
    def din(name, shape):
        if name in over:
            return over[name]
        return nc.dram_tensor(pfx + name, shape, F32, kind="ExternalInput").ap()
    h_tok = din("h_tok", [NT, D]); yT = din("yT", [D, NT]) if 'yT_src' not in over else None; w_out = din("w_out", [D, D])
    ln = din("ln", [4, D])
    wr = din("wr", [D, 36]); br = din("br", [1, 36])
    w1r = din("w1r", [4096, 4096]); w3r = din("w3r", [4096, 4096]); w2r = din("w2r", [4096, 4096])
    G = min(GBLK, NT); J = max(1, NT // G); NOV = (2 * NT) // G if J > 1 else 0; NBLK = 32 + NOV; NSUB = G // 128; NROWS = NBLK * G
    JT = max(J - 1, 1); NOVA = max(NOV, 1)
    cd = {n: din(n, list(v.shape)) for n, v in post_consts(NT).items()}
    ident_d = cd["ident"]
    pfx_ = 'L1' if layer1 else 'L0'
    kind_ = "ExternalOutput" if DEBUG else "Internal"
    h1f_d = nc.dram_tensor(pfx_ + "h1f_d", [NT, D], F32, kind=kind_).ap(); h1b_d = nc.dram_tensor(pfx_ + "h1b_d", [NT, D], BF16, kind=kind_).ap()
    xs_d = nc.dram_tensor(pfx_ + "xs_d", [NROWS + 1, D], BF16, kind=kind_).ap(); yrows_d = nc.dram_tensor(pfx_ + "yrows_d", [NROWS, D], BF16, kind=kind_).ap()
    if layer1:
        glu_w = din("glu_w", [512, 512]); glu_b = din("glu_b", [128, 4])
    out = over["out"] if "out" in over else nc.dram_tensor(pfx + "out", [NT, D], F32, kind="ExternalOutput").ap()
    k = KB(nc) if own else ctx['k']
    if not own:
        k.push()
    ident = k.sb([128, 128]); k.dma(ident[:], ident_d, w=[ident])
    epsb = k.sb([128, 1]); k.op('pool', lambda e: e.memset(epsb[:], EPS), w=[epsb]); k.eps_ap = epsb[:, 0:1]
    cgate = [k.sb([128, 32], name='cg%d' % i) for i in range(ntile)]
    CC = {}
    for n_ in ["ut", "ones", "thr", "biota", "iotap", "eiota"]:
        CC[n_] = k.sb(list(post_consts(NT)[n_].shape), name='pc_' + n_); k.dma(CC[n_][:], cd[n_], w=[CC[n_]])
    identb = k.sb([128, 128], BF16); k.cp(identb[:], ident[:], r=[ident], w=[identb])
    ln2g = k.sb([128, D]); ln2b = k.sb([128, D])
    k.dma(ln2g[:], ln[2:3, :].partition_broadcast(128), w=[ln2g]); k.dma(ln2b[:], ln[3:4, :].partition_broadcast(128), w=[ln2b])
    st = k.sb([128, 12]); mv = k.sb([128, 2]); rs = k.sb([128, 2]); lntmp = (st, mv, rs)
    k.push()
    woutb = k.sb([128, 8, D], BF16); k.dma(woutb[:], w_out.rearrange("(kc p) d -> p kc d", p=128), w=[woutb], q='pool')
    ln1g = k.sb([128, D]); ln1b = k.sb([128, D])
    k.dma(ln1g[:], ln[0:1, :].partition_broadcast(128), w=[ln1g]); k.dma(ln1b[:], ln[1:2, :].partition_broadcast(128), w=[ln1b])
    wrs = k.sb([128, 8, 36]); k.dma(wrs[:], wr.rearrange("(kc p) n -> p kc n", p=128), w=[wrs])
    brb = k.sb([128, 36]); k.dma(brb[:], br.partition_broadcast(128), w=[brb])
    if layer1:
        glub = k.sb([128, 4, 512], BF16); k.dma(glub[:], glu_w.rearrange("(kc p) n -> p kc n", p=128), w=[glub], q='pool')
        glubias = k.sb([128, 4]); k.dma(glubias[:], glu_b, w=[glubias])
    htile = [k.sb([128, D]) for _ in range(2)]
    ytb = [k.sb([128, 8, 128], BF16) for _ in range(2)]
    ytfA = [k.sb([128, 4, 128]) for _ in range(2)]; ytfB = [k.sb([128, 4, 128]) for _ in range(2)]
    rbuf = [k.sb([128, D]) for _ in range(2)]
    h1 = [k.sb([128, D]) for _ in range(2)]
    h1bt = [k.sb([128, D], BF16) for _ in range(2)]
    h1Tf = [k.sb([128, 8, 128]) for _ in range(2)]
    pmix = k.ps([128, D]); pT = k.ps([128, D]); plg = k.ps([128, 36])
    pglu = k.ps([128, 512]) if layer1 else None
    sm = {n: k.sb([128, w_], name='sm_' + n) for n, w_ in [('lgs', 36), ('negm', 1), ('oh', 4), ('e4', 4), ('s4', 1), ('pg', 1), ('fs', 8), ('t8', 8), ('sel', 8), ('negm1', 1), ('ex', 8), ('e2', 1), ('coef', 1), ('cf', 8)]}
    if layer1:
        ysbs = [k.sb([128, 4, 128], BF16) for _ in range(2)]; zss = [k.sb([128, 4, 128]) for _ in range(2)]
    yT3 = yT.rearrange("(kc p) t -> p kc t", p=128) if yT is not None else None
    def load_yT(i, b):
        tsl_ = slice(i * 128, (i + 1) * 128)
        if yT3 is not None:
            k.dma(ytfA[b][:], yT3[:, 0:4, tsl_], w=[ytfA[b]])
            k.dma(ytfB[b][:], yT3[:, 4:8, tsl_], w=[ytfB[b]])
        else:
            if i == 0:
                over['yT_src'].pre()
            srcA, srcB, bufs = over['yT_src'](i)
            k.dma(ytfA[b][:], srcA, r=bufs, w=[ytfA[b]])
            k.dma(ytfB[b][:], srcB, r=bufs, w=[ytfB[b]])
    for i in range(ntile):
        b = i % 2
        tsl = slice(i * 128, (i + 1) * 128)
        k.dma(htile[b][:], h_tok[tsl, :], w=[htile[b]])
        if layer1:
            load_yT(i, b)
            k.cp(ytb[b][:, 4:8, :], ytfB[b][:], r=[ytfB[b]], w=[ytb[b]], e='act')
            ysb = ysbs[b]; zs = zss[b]
            k.cp(ysb[:], ytfA[b][:], r=[ytfA[b]], w=[ysb], e='act')
            for oc in range(4):
                for kc in range(4):
                    k.mm(pglu[:, oc * 128:(oc + 1) * 128], glub[:, kc, oc * 128:(oc + 1) * 128], ysb[:, kc, :], start=(kc == 0), stop=(kc == 3), r=[glub, ysb], w=[pglu])
            for oc in range(4):
                k.act(zs[:, oc, :], pglu[:, oc * 128:(oc + 1) * 128], AF.Sigmoid, bias=glubias[:, oc:oc + 1], r=[pglu, glubias], w=[zs])
            k.tt(ytb[b][:, 0:4, :], zs[:], ytfA[b][:], ALU.mult, r=[zs, ytfA[b]], w=[ytb[b]])
        else:
            load_yT(i, b)
            k.cp(ytb[b][:, 0:4, :], ytfA[b][:], r=[ytfA[b]], w=[ytb[b]], e='dve')
            k.cp(ytb[b][:, 4:8, :], ytfB[b][:], r=[ytfB[b]], w=[ytb[b]], e='act')
        for half in range(2):
            for kc in range(8):
                k.mm(pmix[:, half * 512:(half + 1) * 512], ytb[b][:, kc, :], woutb[:, kc, half * 512:(half + 1) * 512], start=(kc == 0), stop=(kc == 7), r=[ytb[b], woutb], w=[pmix])
        k.stt(rbuf[b][:, :], htile[b][:, :], ALPHA, pmix[:, :], ALU.mult, ALU.add, r=[htile[b], pmix], w=[rbuf[b]])
        layer_norm(k, rbuf[b], h1[b], ln1g, ln1b, lntmp)
        k.dma(h1f_d[tsl, :], h1[b][:, :], r=[h1[b]])
        k.cp(h1bt[b][:, :], h1[b][:, :], r=[h1[b]], w=[h1bt[b]], e='act')
        k.dma(h1b_d[tsl, :], h1bt[b][:, :], r=[h1bt[b]])
        for kc in range(8):
            k.tr(pT[:, kc * 128:(kc + 1) * 128], h1[b][:, kc * 128:(kc + 1) * 128], ident[:], r=[h1[b], ident], w=[pT])
        k.cp(h1Tf[b][:], pT[:, :].rearrange("p (kc t) -> p kc t", t=128), r=[pT], w=[h1Tf[b]], e='act')
        for kc in range(8):
            k.mm(plg[:, :], h1Tf[b][:, kc, :], wrs[:, kc, :], start=(kc == 0), stop=(kc == 7), r=[h1Tf[b], wrs], w=[plg])
        S = sm
        k.tt(S['lgs'][:], plg[:], brb[:], ALU.add, r=[plg, brb], w=[S['lgs']])
        k.op('dve', lambda e: e.reduce_max(out=S['negm'][:], in_=S['lgs'][:, 0:4], axis=AX.X), r=[S['lgs']], w=[S['negm']])
        k.ts(S['negm'][:], S['negm'][:], -1.0, None, op0=ALU.mult, r=[S['negm']], w=[S['negm']])
        k.ts(S['oh'][:], S['lgs'][:, 0:4], S['negm'][:, 0:1], 0.0, op0=ALU.add, op1=ALU.is_ge, r=[S['lgs'], S['negm']], w=[S['oh']])
        k.act(S['e4'][:], S['lgs'][:, 0:4], AF.Exp, bias=S['negm'][:, 0:1], accum_out=S['s4'][:, 0:1], r=[S['lgs'], S['negm']], w=[S['e4'], S['s4']])
        k.op('dve', lambda e: e.reciprocal(out=S['pg'][:], in_=S['s4'][:]), r=[S['s4']], w=[S['pg']])
        k.ts(S['fs'][:], S['lgs'][:, 4:12], S['oh'][:, 0:1], None, op0=ALU.mult, r=[S['lgs'], S['oh']], w=[S['fs']])
        for g in range(1, 4):
            k.stt(S['fs'][:], S['lgs'][:, 4 + 8 * g:12 + 8 * g], S['oh'][:, g:g + 1], S['fs'][:], ALU.mult, ALU.add, r=[S['lgs'], S['oh'], S['fs']], w=[S['fs']])
        k.op('dve', lambda e: e.max(out=S['t8'][:], in_=S['fs'][:]), r=[S['fs']], w=[S['t8']])
        k.ts(S['sel'][:], S['fs'][:], S['t8'][:, 1:2], None, op0=ALU.is_ge, r=[S['fs'], S['t8']], w=[S['sel']])
        k.ts(S['negm1'][:], S['t8'][:, 0:1], -1.0, None, op0=ALU.mult, r=[S['t8']], w=[S['negm1']])
        k.act(S['ex'][:], S['fs'][:], AF.Exp, bias=S['negm1'][:, 0:1], r=[S['fs'], S['negm1']], w=[S['ex']])
        k.act(S['e2'][:], S['t8'][:, 1:2], AF.Exp, bias=S['negm1'][:, 0:1], r=[S['t8'], S['negm1']], w=[S['e2']])
        k.ts(S['e2'][:], S['e2'][:], 1.0, None, op0=ALU.add, r=[S['e2']], w=[S['e2']])
        k.op('dve', lambda e: e.reciprocal(out=S['coef'][:], in_=S['e2'][:]), r=[S['e2']], w=[S['coef']])
        k.tt(S['coef'][:], S['coef'][:], S['pg'][:], ALU.mult, r=[S['coef'], S['pg']], w=[S['coef']])
        k.tt(S['cf'][:], S['ex'][:], S['sel'][:], ALU.mult, r=[S['ex'], S['sel']], w=[S['cf']])
        k.ts(S['cf'][:], S['cf'][:], S['coef'][:, 0:1], None, op0=ALU.mult, r=[S['cf'], S['coef']], w=[S['cf']])
        for g in range(4):
            k.ts(cgate[i][:, 8 * g:8 * g + 8], S['cf'][:], S['oh'][:, g:g + 1], None, op0=ALU.mult, r=[S['cf'], S['oh']], w=[cgate[i]])
    k.pop()
    didx = [k.sb([128, 4], I32, name='didx%d' % i) for i in range(ntile)]
    gts = [k.sb([128, 2], name='gts%d' % i) for i in range(ntile)]
    k.push()
    runb = k.sb([128, 32]); k.op('pool', lambda e: e.memset(runb[:], 0.0), w=[runb])
    mm_ = k.sb([128, 32]); ranks = [k.sb([128, 32], name='rank%d' % i) for i in range(ntile)]
    ppre = k.ps([128, 512], name='ppre_pAT')
    for i in range(ntile):
        k.ts(mm_[:], cgate[i][:], 0.0, None, op0=ALU.is_gt, r=[cgate[i]], w=[mm_])
        k.mm(ppre[:, 0:32], CC['ut'][:], mm_[:], r=[CC['ut'], mm_], w=[ppre])
        k.mm(ppre[:, 32:64], CC['ones'][:], mm_[:], r=[CC['ones'], mm_], w=[ppre])
        k.tt(ranks[i][:], ppre[:, 0:32], runb[:], ALU.add, r=[ppre, runb], w=[ranks[i]])
        k.tt(runb[:], ppre[:, 32:64], runb[:], ALU.add, r=[ppre, runb], w=[runb])
    cmp1 = k.sb([128, 32, JT]); nb = k.sb([128, 32]); pend = k.sb([128, 32]); delta = k.sb([128, 32]); ones32 = k.sb([128, 32])
    k.op('pool', lambda e: e.memset(ones32[:], 1.0), w=[ones32])
    if J > 1:
        k.tt(cmp1[:], runb[:].unsqueeze(2).to_broadcast([128, 32, JT]), CC['thr'][:].rearrange("p (e j) -> p e j", j=JT), ALU.is_gt, r=[runb, CC['thr']], w=[cmp1])
        k.op('dve', lambda e: e.reduce_sum(out=nb[:], in_=cmp1[:], axis=AX.X), r=[cmp1], w=[nb])
    else:
        k.op('pool', lambda e: e.memset(nb[:], 0.0), w=[nb])
    k.op('dve', lambda e: e.tensor_tensor_scan(out=pend[:], data0=ones32[:], data1=nb[:], initial=0.0, op0=ALU.mult, op1=ALU.add), r=[ones32, nb], w=[pend])
    k.tt(delta[:], pend[:], nb[:], ALU.subtract, r=[pend, nb], w=[delta])
    k.ts(delta[:], delta[:], float(G), float(31 * G), op0=ALU.mult, op1=ALU.add, r=[delta], w=[delta])
    k.tt(delta[:], delta[:], CC['eiota'][:], ALU.subtract, r=[delta, CC['eiota']], w=[delta])
    cmp2 = k.sb([128, NOVA, 32]); bef = k.sb([128, NOVA]); widx = k.sb([128, NOVA], I32)
    k.tt(cmp2[:], pend[:].unsqueeze(1).to_broadcast([128, NOVA, 32]), CC['biota'][:].rearrange("p (b e) -> p b e", e=32), ALU.is_le, r=[pend, CC['biota']], w=[cmp2])
    k.op('dve', lambda e: e.reduce_sum(out=bef[:], in_=cmp2[:], axis=AX.X), r=[cmp2], w=[bef])
    k.ts(bef[:], bef[:], 31.0, 128.0, op0=ALU.min, op1=ALU.mult, r=[bef], w=[bef])
    k.ts(bef[:], bef[:], CC['iotap'][:, 0:1], None, op0=ALU.add, r=[bef, CC['iotap']], w=[bef])
    k.cp(widx[:], bef[:], r=[bef], w=[widx])
    selb = k.sb([128, 32])
    Dm = k.sb([128, 32]); eq = k.sb([128, 32]); dd = k.sb([128, 4]); df = k.sb([128, 4]); dneg = k.sb([128, 2])
    for i in range(ntile):
        k.ts(mm_[:], cgate[i][:], 0.0, None, op0=ALU.is_gt, r=[cgate[i]], w=[mm_])
        k.ts(selb[:], ranks[i][:], float(G), None, op0=ALU.is_ge, r=[ranks[i]], w=[selb])
        k.tt(selb[:], selb[:], delta[:], ALU.mult, r=[selb, delta], w=[selb])
        k.tt(Dm[:], ranks[i][:], CC['eiota'][:], ALU.add, r=[ranks[i], CC['eiota']], w=[Dm])
        k.tt(Dm[:], Dm[:], selb[:], ALU.add, r=[Dm, selb], w=[Dm])
        k.stt(Dm[:], Dm[:], 1.0, mm_[:], ALU.add, ALU.mult, r=[Dm, mm_], w=[Dm])
        k.op('dve', lambda e: e.reduce_max(out=dd[:, 0:1], in_=Dm[:], axis=AX.X), r=[Dm], w=[dd])
        k.op('dve', lambda e: e.reduce_sum(out=dd[:, 1:2], in_=Dm[:], axis=AX.X), r=[Dm], w=[dd])
        k.ts(df[:, 0:1], dd[:, 0:1], -1.0, None, op0=ALU.add, r=[dd], w=[df])
        k.stt(df[:, 1:2], dd[:, 1:2], -1.0, dd[:, 0:1], ALU.add, ALU.subtract, r=[dd], w=[df])
        k.ts(dneg[:], df[:, 0:2], 0.0, None, op0=ALU.is_lt, r=[df], w=[dneg])
        k.ts(df[:, 2:4], df[:, 0:2], 0.0, None, op0=ALU.max, r=[df], w=[df])
        k.stt(df[:, 0:2], dneg[:], float(NROWS + 1), df[:, 0:2], ALU.mult, ALU.add, r=[dneg, df], w=[df])
        k.cp(didx[i][:], df[:], r=[df], w=[didx[i]])
        k.ts(eq[:], Dm[:], dd[:, 0:1], None, op0=ALU.is_equal, r=[Dm, dd], w=[eq])
        k.tt(eq[:], eq[:], cgate[i][:], ALU.mult, r=[eq, cgate[i]], w=[eq])
        k.op('dve', lambda e: e.reduce_sum(out=gts[i][:, 0:1], in_=eq[:], axis=AX.X), r=[eq], w=[gts[i]])
        k.op('dve', lambda e: e.reduce_sum(out=dd[:, 2:3], in_=cgate[i][:], axis=AX.X), r=[cgate[i]], w=[dd])
        k.tt(gts[i][:, 1:2], dd[:, 2:3], gts[i][:, 0:1], ALU.subtract, r=[dd, gts[i]], w=[gts[i]])
    hbt = [k.sb([128, D], BF16) for _ in range(2)]
    XS = Buf(None, 'xs')
    for i in range(ntile):
        b = i % 2
        k.dma(hbt[b][:], h1b_d[i * 128:(i + 1) * 128, :], w=[hbt[b]])
        k.ind(xs_d, bass.IndirectOffsetOnAxis(ap=didx[i][:, 0:1], axis=0), hbt[b][:], None, r=[didx[i], hbt[b]], bounds=NROWS)
        k.ind(xs_d, bass.IndirectOffsetOnAxis(ap=didx[i][:, 1:2], axis=0), hbt[b][:], None, r=[didx[i], hbt[b]], bounds=NROWS)
    if DEBUG:
        dbg_cg = nc.dram_tensor("dbg_cg", [NT, 32], F32, kind="ExternalOutput").ap()
        dbg_di = nc.dram_tensor("dbg_di", [NT, 2], I32, kind="ExternalOutput").ap()
        dbg_gt = nc.dram_tensor("dbg_gt", [NT, 2], F32, kind="ExternalOutput").ap()
        dbg_wi = nc.dram_tensor("dbg_wi", [128, NOVA], I32, kind="ExternalOutput").ap()
        dbg_rk = nc.dram_tensor("dbg_rk", [NT, 32], F32, kind="ExternalOutput").ap()
        for i in range(ntile):
            k.dma(dbg_cg[i * 128:(i + 1) * 128, :], cgate[i][:], r=[cgate[i]])
            k.dma(dbg_di[i * 128:(i + 1) * 128, :], didx[i][:, 0:2], r=[didx[i]])
            k.dma(dbg_gt[i * 128:(i + 1) * 128, :], gts[i][:], r=[gts[i]])
            k.dma(dbg_rk[i * 128:(i + 1) * 128, :], ranks[i][:], r=[ranks[i]])
        k.dma(dbg_wi, widx[:], r=[widx])
    k.barrier()
    w1s = [k.sb([128, 4096])] * 2; w3s = [k.sb([128, 4096])] * 2; w2s = [k.sb([128, 4096])] * 2
    w1b = [k.sb([128, 4096], BF16) for _ in range(2)]; w3b = [k.sb([128, 4096], BF16) for _ in range(2)]; w2b = [k.sb([128, 4096], BF16) for _ in range(2)]
    xt = [k.sb([128, NSUB, D], BF16) for _ in range(2)]
    XT = [k.sb([128, 8, 128], BF16) for _ in range(2)]
    sl = [k.sb([128, 512]) for _ in range(2)]; actb = [k.sb([128, 512], BF16) for _ in range(2)]
    actT = [k.sb([128, 4, 128], BF16) for _ in range(2)]
    yrow = [k.sb([128, D], BF16) for _ in range(2)]
    pXT = k.ps([128, D], BF16); ph1 = k.ps([128, 512]); ph3 = k.ps([128, 512]); pATv = ppre[:, :].bitcast(BF16)
    py = [k.ps([128, 512]) for _ in range(2)]
    def load_blk(bk):
        sb_ = bk % 2
        if bk < 32:
            k.dma(w1s[sb_][:], w1r[bk * 128:(bk + 1) * 128, :], w=[w1s[sb_]])
            k.dma(w3s[sb_][:], w3r[bk * 128:(bk + 1) * 128, :], w=[w3s[sb_]], q='act')
            k.dma(w2s[sb_][:], w2r[bk * 128:(bk + 1) * 128, :], w=[w2s[sb_]])
        else:
            off = bass.IndirectOffsetOnAxis(ap=widx[:, bk - 32:bk - 31], axis=0)
            k.ind(w1s[sb_][:], None, w1r, off, r=[widx], w=[w1s[sb_]], bounds=4095)
            k.ind(w3s[sb_][:], None, w3r, off, r=[widx], w=[w3s[sb_]], bounds=4095)
            k.ind(w2s[sb_][:], None, w2r, off, r=[widx], w=[w2s[sb_]], bounds=4095)
        k.dma(xt[sb_][:], xs_d[bk * G:(bk + 1) * G, :].rearrange("(s p) d -> p s d", p=128), w=[xt[sb_]])
    ph1b = [ph1, k.ps([128, 512])]; ph3b = [ph3, k.ps([128, 512])]
    U = NBLK * NSUB
    def casts(bk):
        sb_ = bk % 2
        k.cp(w1b[sb_][:], w1s[sb_][:], r=[w1s[sb_]], w=[w1b[sb_]], e='act')
        k.cp(w3b[sb_][:], w3s[sb_][:], r=[w3s[sb_]], w=[w3b[sb_]], e='dve')
        k.cp(w2b[sb_][:, 0:2048], w2s[sb_][:, 0:2048], r=[w2s[sb_]], w=[w2b[sb_]], e='act')
        k.cp(w2b[sb_][:, 2048:4096], w2s[sb_][:, 2048:4096], r=[w2s[sb_]], w=[w2b[sb_]], e='dve')
        if bk + 1 < NBLK:
            load_blk(bk + 1)
    def S1(u):
        bk, s_ = divmod(u, NSUB); q = u % 2
        for kc in range(8):
            k.tr(pXT[:, kc * 128:(kc + 1) * 128], xt[bk % 2][:, s_, kc * 128:(kc + 1) * 128], identb[:], r=[xt[bk % 2], identb], w=[pXT])
        k.cp(XT[q][:], pXT[:, :].rearrange("p (kc t) -> p kc t", t=128), r=[pXT], w=[XT[q]], e='act')
    def S2(u):
        bk, s_ = divmod(u, NSUB); q = u % 2; sb_ = bk % 2
        if s_ == 0:
            casts(bk)
        for kc in range(8):
            k.mm(ph1b[q][:, :], XT[q][:, kc, :], w1b[sb_][:, kc * 512:(kc + 1) * 512], start=(kc == 0), stop=(kc == 7), r=[XT[q], w1b[sb_]], w=[ph1b[q]])
        for kc in range(8):
            k.mm(ph3b[q][:, :], XT[q][:, kc, :], w3b[sb_][:, kc * 512:(kc + 1) * 512], start=(kc == 0), stop=(kc == 7), r=[XT[q], w3b[sb_]], w=[ph3b[q]])
        k.act(sl[q][:, :], ph1b[q][:, :], AF.Silu, r=[ph1b[q]], w=[sl[q]])
        k.tt(actb[q][:, :], sl[q][:, :], ph3b[q][:, :], ALU.mult, r=[sl[q], ph3b[q]], w=[actb[q]])
    def S3a(u):
        q = u % 2
        for hc in range(4):
            k.tr(pATv[:, hc * 128:(hc + 1) * 128], actb[q][:, hc * 128:(hc + 1) * 128], identb[:], r=[actb[q], identb], w=[ppre])
        k.cp(actT[q][:], pATv[:, 0:512].rearrange("p (hc t) -> p hc t", t=128), r=[ppre], w=[actT[q]], e='dve')
    def S3b(u):
        bk, s_ = divmod(u, NSUB); q = u % 2; sb_ = bk % 2
        for half in range(2):
            for hc in range(4):
                k.mm(py[half][:, :], actT[q][:, hc, :], w2b[sb_][:, hc * 1024 + half * 512:hc * 1024 + (half + 1) * 512], start=(hc == 0), stop=(hc == 3), r=[actT[q], w2b[sb_]], w=[py[half]])
        k.cp(yrow[q][:, 0:512], py[0][:, :], r=[py[0]], w=[yrow[q]], e='act')
        k.cp(yrow[q][:, 512:1024], py[1][:, :], r=[py[1]], w=[yrow[q]], e='dve')
        r0 = bk * G + s_ * 128
        k.dma(yrows_d[r0:r0 + 128, :], yrow[q][:, :], r=[yrow[q]])
    load_blk(0)
    S1(0)
    for step in range(U + 1):
        if step - 1 >= 0:
            S3a(step - 1)
        if step + 1 < U:
            S1(step + 1)
        if step < U:
            S2(step)
        if step - 1 >= 0:
            S3b(step - 1)
    k.pop()
    ob = [k.sb([128, D]) for _ in range(2)]
    hT_out = over.get('hT_dst')
    if hT_out is not None:
        pT3 = k.ps([128, D]); obT = [k.sb([128, 8, 128], BF16) for _ in range(2)]
    NB3 = 2
    hf = [k.sb([128, D]) for _ in range(NB3)]; rhi = [k.sb([128, D], BF16) for _ in range(NB3)]; rlo = [k.sb([128, D], BF16) for _ in range(NB3)]
    YR = Buf(None, 'yrows')
    for i in range(ntile):
        b = i % NB3
        k.dma(hf[b][:], h1f_d[i * 128:(i + 1) * 128, :], w=[hf[b]])
        k.ind(rhi[b][:], None, yrows_d, bass.IndirectOffsetOnAxis(ap=didx[i][:, 2:3], axis=0), r=[didx[i]], w=[rhi[b]], bounds=NROWS - 1)
        k.ind(rlo[b][:], None, yrows_d, bass.IndirectOffsetOnAxis(ap=didx[i][:, 3:4], axis=0), r=[didx[i]], w=[rlo[b]], bounds=NROWS - 1)
        if DEBUG:
            if i == 0:
                dbg_rhi = nc.dram_tensor("dbg_rhi", [NT, D], BF16, kind="ExternalOutput").ap(); dbg_pre = nc.dram_tensor("dbg_pre", [NT, D], F32, kind="ExternalOutput").ap()
            k.dma(dbg_rhi[i * 128:(i + 1) * 128, :], rhi[b][:, :], r=[rhi[b]])
        k.act(hf[b][:, :], hf[b][:, :], AF.Copy, scale=ALPHA, r=[hf[b]], w=[hf[b]])
        k.stt(hf[b][:, :], rhi[b][:, :], gts[i][:, 0:1], hf[b][:, :], ALU.mult, ALU.add, r=[rhi[b], gts[i], hf[b]], w=[hf[b]])
        k.stt(hf[b][:, :], rlo[b][:, :], gts[i][:, 1:2], hf[b][:, :], ALU.mult, ALU.add, r=[rlo[b], gts[i], hf[b]], w=[hf[b]])
        if DEBUG:
            k.dma(dbg_pre[i * 128:(i + 1) * 128, :], hf[b][:, :], r=[hf[b]])
        layer_norm(k, hf[b], ob[i % 2], ln2g, ln2b, lntmp)
        k.dma(out[i * 128:(i + 1) * 128, :], ob[i % 2][:, :], r=[ob[i % 2]])
        if hT_out is not None:
            for kc in range(8):
                k.tr(pT3[:, kc * 128:(kc + 1) * 128], ob[i % 2][:, kc * 128:(kc + 1) * 128], ident[:], r=[ob[i % 2], ident], w=[pT3])
            k.cp(obT[i % 2][:], pT3[:, :].rearrange("p (kc t) -> p kc t", t=128), r=[pT3], w=[obT[i % 2]], e='act')
            k.dma(hT_out(i), obT[i % 2][:], r=[obT[i % 2]])
            over['after_tile'](i)
    if own:
        k.finish()
        k.close()
        return nc
    k.pop()


TB = 512
NCH = 8
GN_EPS = 64e-5
NORM_EPS = 1e-5

def consts0():
    c = {}
    c["ident"] = np.eye(128, dtype=np.float32)
    bo = np.zeros((128, 128), np.float32); bo[:64, :64] = 1; bo[64:, 64:] = 1
    c["blockones"] = bo
    c["ones"] = np.ones((128, 128), np.float32)
    p = np.arange(128)[:, None] % 64; q = np.arange(512)[None, :] % 64
    c["m_us"] = (q > p).astype(np.float32)
    c["m_ui"] = (q >= p).astype(np.float32)
    c["m_ls"] = (p > q).astype(np.float32)
    c["ident4"] = np.tile(np.eye(128, dtype=np.float32), (1, 4))
    hm = np.zeros((128, 4), np.float32); hm[:64, 0] = 1; hm[64:, 1] = 1; hm[:64, 2] = -1; hm[64:, 3] = -1
    c["hm"] = hm
    rm = np.ones((128, 512), np.float32); rm[:, ::64] = 0
    c["resetm"] = rm
    return c

def build_mix0(L, ctx=None):
    own = ctx is None
    nc = bass.Bass("TRN2", target_bir_lowering=False) if own else ctx['nc']
    pfx = '' if own else ctx['pfx']
    over = {} if own else ctx['over']
    nblk = L // TB
    def din(name, shape):
        if name in over:
            return over[name]
        return nc.dram_tensor(pfx + name, shape, F32, kind="ExternalInput").ap()
    hT = din("hT", [1024, L])
    w_rw = din("w_rw", [1024, 640]); w_gla = din("w_gla", [1024, 400])
    rwvec = din("rwvec", [128, 16])
    lora_wa = din("lora_wa", [128, 128]); g2c = din("g2c", [128, 128])
    gk_w2 = din("gk_w2", [16, 64]); glavec = din("glavec", [128, 2])
    cd = {n: din(n, list(v.shape)) for n, v in consts0().items()}
    y_rw = None if "y_dst" in over else nc.dram_tensor("y_rw", [128, L], F32, kind="ExternalOutput").ap()
    y_gla = None if "y_dst" in over else nc.dram_tensor("y_gla", [128, L], F32, kind="ExternalOutput").ap()
    k = KB(nc) if own else ctx['k']
    if not own:
        k.push()
    C = {}
    for n, v in consts0().items():
        C[n] = k.sb(list(v.shape), name='c_' + n); k.dma(C[n][:], cd[n], w=[C[n]])
    vec = k.sb([128, 16]); k.dma(vec[:], rwvec, w=[vec])
    MU = lambda i: vec[:, i:i + 1]
    W0, A0, KK, KA, RK, GNG, GNB = [vec[:, 5 + i:6 + i] for i in range(7)]
    vx = k.sb([128, 4])
    k.ts(vx[:, 0:1], KA, -1.0, 1.0, op0=ALU.mult, op1=ALU.add, r=[vec], w=[vx])
    k.op('pool', lambda e: e.memset(vx[:, 1:2], GN_EPS), w=[vx])
    k.op('pool', lambda e: e.memset(vx[:, 2:3], NORM_EPS), w=[vx])
    gv = k.sb([128, 2]); k.dma(gv[:], glavec, w=[gv])
    k.ts(vx[:, 3:4], gv[:, 0:1], -1.0, None, op0=ALU.mult, r=[gv, vx], w=[vx])
    lwa = k.sb([128, 128]); k.dma(lwa[:], lora_wa, w=[lwa])
    g2s = k.sb([128, 128]); k.dma(g2s[:], g2c, w=[g2s])
    gkw = k.sb([16, 64]); k.dma(gkw[:], gk_w2, w=[gkw])
    wrwb = k.sb([128, 8, 640], BF16); k.dma(wrwb[:], w_rw.rearrange("(kc p) n -> p kc n", p=128), w=[wrwb], q='pool')
    wglb = k.sb([128, 8, 400], BF16); k.dma(wglb[:], w_gla.rearrange("(kc p) n -> p kc n", p=128), w=[wglb], q='pool')
    hTb = [k.sb([128, 8, TB], BF16) for _ in range(2)]
    hT3 = hT.rearrange("(kc p) t -> p kc t", p=128)
    pp = k.ps([128, 512]); pA = k.ps([128, 512])
    pM = k.ps([128, 512]); pN = k.ps([128, 512]); pP = k.ps([128, 512])
    pWUS = k.ps([128, 512]); pY = k.ps([128, 512]); pO = k.ps([128, 512])
    T = lambda name: k.sb([128, TB], name=name)
    psh = [k.sb([128, TB + 1], name='psh%d' % i) for i in range(5)]
    for b_ in psh:
        k.op('pool', lambda e: e.memset(b_[:, 0:1], 0.0), w=[b_])
    xs = [T('xs%d' % i) for i in range(5)]
    tmp = T('tmp'); tmp2 = T('tmp2')
    ld = T('ld'); a_sb = T('a_sb'); g_sb = T('g_sb'); kkn = T('kkn'); k2 = T('k2'); bvec = T('bvec'); bonus = T('bonus')
    bcum = T('bcum'); eb = T('eb'); enb = T('enb'); ebx = T('ebx')
    BD = lambda name: k.sb([128, NCH, 2, 64], name=name)
    r_bd, k_bd, b_bd, a_bd, kd_bd, bd_bd, v_bd = [BD(n) for n in ['r_bd', 'k_bd', 'b_bd', 'a_bd', 'kd_bd', 'bd_bd', 'v_bd']]
    AM = lambda name: k.sb([128, NCH, 128], name=name)
    AakT, ArbT, ArkT, Vtok, Kdt, Bdt = [AM(n) for n in ['AakT', 'ArbT', 'ArkT', 'Vtok', 'Kdt', 'Bdt']]
    NtA, MA, Pfin = [k.sb([128, NCH, 128], BF16, name=n) for n in ['NtA', 'MA', 'Pfin']]
    Ntb = [k.sb([128, 4, 128], BF16, name='Ntb%d' % i) for i in range(4)]
    Mb = [k.sb([128, 4, 128], BF16, name='Mb%d' % i) for i in range(4)]
    Pb = [k.sb([128, 4, 128], BF16, name='Pb%d' % i) for i in range(4)]
    ident4b = k.sb([128, 512], BF16, name='ident4b'); k.cp(ident4b[:], C['ident4'][:], r=[C['ident4']], w=[ident4b])
    Wsb = k.sb([128, 128], BF16, name='Wsb'); Usb = k.sb([128, 128], name='Usb')
    Sbd = [k.sb([128, 128], name='Sbd%d' % i) for i in range(2)]
    k.op('pool', lambda e: e.memset(Sbd[0][:], 0.0), w=[Sbd[0]])
    yT = T('yT'); yo = T('yo')
    gq = k.sb([64, TB], name='gq'); gk = k.sb([64, TB], name='gk'); ggate = T('ggate'); gkl = k.sb([16, TB], name='gkl')
    gvt = k.sb([64, NCH, 128], name='gvt')
    gsp = k.sb([64, TB], name='gsp'); gb = k.sb([64, TB], name='gb'); geb = k.sb([64, TB], name='geb'); genb = k.sb([64, TB], name='genb')
    gqt = k.sb([64, TB], name='gqt'); gkt = k.sb([64, TB], name='gkt'); gkd = k.sb([64, TB], name='gkd')
    gAT = k.sb([64, NCH, 64], name='gAT'); gkdt = k.sb([64, NCH, 64], name='gkdt')
    gS = [k.sb([64, 128], name='gS%d' % i) for i in range(2)]
    k.op('pool', lambda e: e.memset(gS[0][:], 0.0), w=[gS[0]])
    goT = yT; gsq = tmp2; gout = tmp
    sidx = 0; gsidx = 0
    c3 = lambda buf: buf[:, :].rearrange("p (c t) -> p c t", t=64)
    for blk in range(nblk):
        hb = hTb[blk % 2]
        cs = slice(blk * TB, (blk + 1) * TB)
        k.dma(hb[:], hT3[:, :, cs], w=[hb], q='pool')
        for ct in range(5):
            for kc in range(8):
                k.mm(pp[:, :], wrwb[:, kc, ct * 128:(ct + 1) * 128], hb[:, kc, :], start=(kc == 0), stop=(kc == 7), r=[wrwb, hb], w=[pp])
            k.cp(psh[ct][:, 1:TB + 1], pp[:, :], r=[pp], w=[psh[ct]], e='act')
            k.tt(tmp[:, :], psh[ct][:, 0:TB], psh[ct][:, 1:TB + 1], ALU.subtract, r=[psh[ct]], w=[tmp])
            k.stt(xs[ct][:, :], tmp[:, :], MU(ct), psh[ct][:, 1:TB + 1], ALU.mult, ALU.add, r=[tmp, psh[ct], vec], w=[xs[ct]])
            k.cp(psh[ct][:, 0:1], psh[ct][:, TB:TB + 1], r=[psh[ct]], w=[psh[ct]], e='pool')
        xr, xk, xv, xwa, xgl = xs
        k.act(xwa[0:64, :], xwa[0:64, :], AF.Tanh, r=[xwa], w=[xwa])
        k.mm(pp[:, :], lwa[0:64, :], xwa[0:64, :], r=[lwa, xwa], w=[pp])
        k.act(tmp[:, :], pp[:, :], AF.Sigmoid, bias=W0, r=[pp, vec], w=[tmp])
        k.ts(ld[:, :], tmp[:, :], -0.6065306597126334, None, op0=ALU.mult, r=[tmp], w=[ld])
        k.mm(pA[:, :], lwa[64:128, :], xwa[64:128, :], r=[lwa, xwa], w=[pA])
        k.act(a_sb[:, :], pA[:, :], AF.Sigmoid, bias=A0, r=[pA, vec], w=[a_sb])
        k.act(tmp[:, :], xgl[:, :], AF.Sigmoid, r=[xgl], w=[tmp])
        k.mm(pp[:, :], g2s[:, :], tmp[:, :], r=[g2s, tmp], w=[pp])
        k.cp(g_sb[:, :], pp[:, :], r=[pp], w=[g_sb], e='act')
        k.ts(kkn[:, :], xk[:, :], KK, None, op0=ALU.mult, r=[xk, vec], w=[kkn])
        k.tt(tmp[:, :], kkn[:, :], kkn[:, :], ALU.mult, r=[kkn], w=[tmp])
        k.mm(pA[:, :], C['blockones'][:, :], tmp[:, :], r=[C['blockones'], tmp], w=[pA])
        k.act(tmp2[:, :], pA[:, :], AF.Sqrt, r=[pA], w=[tmp2])
        k.ts(tmp2[:, :], tmp2[:, :], 1e-12, None, op0=ALU.max, r=[tmp2], w=[tmp2])
        k.op('dve', lambda e: e.reciprocal(out=tmp2[:, :], in_=tmp2[:, :]), r=[tmp2], w=[tmp2])
        k.tt(kkn[:, :], kkn[:, :], tmp2[:, :], ALU.mult, r=[kkn, tmp2], w=[kkn])
        k.ts(tmp[:, :], a_sb[:, :], KA, vx[:, 0:1], op0=ALU.mult, op1=ALU.add, r=[a_sb, vec, vx], w=[tmp])
        k.tt(k2[:, :], xk[:, :], tmp[:, :], ALU.mult, r=[xk, tmp], w=[k2])
        k.tt(bvec[:, :], kkn[:, :], a_sb[:, :], ALU.mult, r=[kkn, a_sb], w=[bvec])
        k.stt(tmp[:, :], xr[:, :], RK, k2[:, :], ALU.mult, ALU.mult, r=[xr, vec, k2], w=[tmp])
        k.mm(pp[:, :], C['blockones'][:, :], tmp[:, :], r=[C['blockones'], tmp], w=[pp])
        k.tt(bonus[:, :], pp[:, :], xv[:, :], ALU.mult, r=[pp, xv], w=[bonus])
        k.op('dve', lambda e: e.tensor_tensor_scan(out=bcum[:, :], data0=C['resetm'][:, :], data1=ld[:, :], initial=0.0, op0=ALU.mult, op1=ALU.add), r=[C['resetm'], ld], w=[bcum])
        k.act(eb[:, :], bcum[:, :], AF.Exp, r=[bcum], w=[eb])
        k.act(enb[:, :], bcum[:, :], AF.Exp, scale=-1.0, r=[bcum], w=[enb])
        k.tt(tmp[:, :], bcum[:, :], ld[:, :], ALU.subtract, r=[bcum, ld], w=[tmp])
        k.act(ebx[:, :], tmp[:, :], AF.Exp, r=[tmp], w=[ebx])
        gC = c3(eb)[:, :, 63:64].to_broadcast([128, NCH, 64])
        for h in range(2):
            hm = C['hm'][:, h:h + 1]; hmn = C['hm'][:, 2 + h:3 + h]
            k.stt(r_bd[:, :, h, :], c3(xr), hm, c3(eb), ALU.mult, ALU.mult, r=[xr, eb, C['hm']], w=[r_bd])
            k.stt(k_bd[:, :, h, :], c3(k2), hm, c3(enb), ALU.mult, ALU.mult, r=[k2, enb, C['hm']], w=[k_bd])
            k.stt(b_bd[:, :, h, :], c3(bvec), hm, c3(enb), ALU.mult, ALU.mult, r=[bvec, enb, C['hm']], w=[b_bd])
            k.stt(a_bd[:, :, h, :], c3(kkn), hmn, c3(ebx), ALU.mult, ALU.mult, r=[kkn, ebx, C['hm']], w=[a_bd])
            k.tt(kd_bd[:, :, h, :], k_bd[:, :, h, :], gC, ALU.mult, r=[k_bd, eb], w=[kd_bd])
            k.tt(bd_bd[:, :, h, :], b_bd[:, :, h, :], gC, ALU.mult, r=[b_bd, eb], w=[bd_bd])
            k.ts(v_bd[:, :, h, :], c3(xv), hm, None, op0=ALU.mult, r=[xv, C['hm']], w=[v_bd], e='pool')
        f2 = lambda bd, c: bd[:, c, :, :].rearrange("p h t -> p (h t)")
        def amat(dst, lbd, rbd, mask):
            for g4 in range(2):
                for cc in range(4):
                    c = g4 * 4 + cc
                    k.mm(pA[:, cc * 128:(cc + 1) * 128], f2(lbd, c), f2(rbd, c), r=[lbd, rbd], w=[pA])
                if mask is None:
                    k.cp(dst[:, g4 * 4:(g4 + 1) * 4, :], pA[:, :].rearrange("p (c t) -> p c t", t=128), r=[pA], w=[dst], e='act')
                else:
                    k.tt(dst[:, g4 * 4:(g4 + 1) * 4, :], pA[:, :].rearrange("p (c t) -> p c t", t=128), mask[:, :].rearrange("p (c t) -> p c t", t=128), ALU.mult, r=[pA, mask], w=[dst])
        amat(NtA, b_bd, a_bd, C['m_us'])
        amat(MA, a_bd, b_bd, C['m_ls'])
        amat(AakT, k_bd, a_bd, C['m_us'])
        amat(ArbT, b_bd, r_bd, C['m_ui'])
        amat(ArkT, k_bd, r_bd, C['m_ui'])
        def tmat(dst, src):
            for g4 in range(2):
                for cc in range(4):
                    c = g4 * 4 + cc
                    k.tr(pA[:, cc * 128:(cc + 1) * 128], f2(src, c), C['ident'][:, :], r=[src, C['ident']], w=[pA])
                k.cp(dst[:, g4 * 4:(g4 + 1) * 4, :], pA[:, :].rearrange("p (c t) -> p c t", t=128), r=[pA], w=[dst], e='act')
        tmat(Vtok, v_bd); tmat(Kdt, kd_bd); tmat(Bdt, bd_bd)
        def A3(buf):
            return buf[:, :, :] if len(buf.t.shape) == 3 else buf[:, :].rearrange("p (c t) -> p c t", t=128)
        sets = [dict(pM=pM, pN=pN, pP=pP, N=Ntb[0:2], M=Mb[0:2], P=Pb[0:2]),
                dict(pM=pp, pN=pA, pP=pO, N=Ntb[2:4], M=Mb[2:4], P=Pb[2:4])]
        stt_ = []
        i4 = ident4b[:, :].rearrange("p (c t) -> p c t", t=128)
        for g4 in range(2):
            S_ = sets[g4]; gs = slice(g4 * 4, (g4 + 1) * 4)
            k.tt(A3(S_['P'][0]), NtA[:, gs, :], i4, ALU.add, r=[NtA, ident4b], w=[S_['P'][0]])
            stt_.append(dict(N=None, M=None, pi=0))
        def Nsl(g4, c):
            s_ = stt_[g4]
            return (NtA[:, g4 * 4 + c, :], NtA) if s_['N'] is None else (A3(s_['N'])[:, c, :], s_['N'])
        def Msl(g4, c):
            s_ = stt_[g4]
            return (MA[:, g4 * 4 + c, :], MA) if s_['M'] is None else (A3(s_['M'])[:, c, :], s_['M'])
        for lvl in range(1, 6):
            for g4 in range(2):
                S_ = sets[g4]
                for c in range(4):
                    (na, nb_), (ma, mb_) = Nsl(g4, c), Msl(g4, c)
                    k.mm(S_['pM'][:, c * 128:(c + 1) * 128], na, ma, r=[nb_, mb_], w=[S_['pM']])
                if lvl < 5:
                    for c in range(4):
                        (na, nb_), (ma, mb_) = Nsl(g4, c), Msl(g4, c)
                        k.mm(S_['pN'][:, c * 128:(c + 1) * 128], ma, na, r=[nb_, mb_], w=[S_['pN']])
            for g4 in range(2):
                S_ = sets[g4]
                Mn = S_['M'][lvl % 2]; Nn = S_['N'][lvl % 2]
                k.cp(A3(Mn), S_['pM'][:, :].rearrange("p (c t) -> p c t", t=128), r=[S_['pM']], w=[Mn], e='act')
                if lvl < 5:
                    k.cp(A3(Nn), S_['pN'][:, :].rearrange("p (c t) -> p c t", t=128), r=[S_['pN']], w=[Nn], e='act')
            for g4 in range(2):
                S_ = sets[g4]; Mn = S_['M'][lvl % 2]; Pc = S_['P'][stt_[g4]['pi']]
                for c in range(4):
                    k.mm(S_['pP'][:, c * 128:(c + 1) * 128], A3(Mn)[:, c, :], A3(Pc)[:, c, :], r=[Mn, Pc], w=[S_['pP']])
            for g4 in range(2):
                S_ = sets[g4]; s_ = stt_[g4]; gs = slice(g4 * 4, (g4 + 1) * 4)
                Pc = S_['P'][s_['pi']]
                if lvl < 5:
                    Pn = S_['P'][1 - s_['pi']]
                    k.tt(A3(Pn), S_['pP'][:, :].rearrange("p (c t) -> p c t", t=128), A3(Pc), ALU.add, r=[S_['pP'], Pc], w=[Pn])
                    s_['pi'] = 1 - s_['pi']
                else:
                    k.tt(Pfin[:, gs, :], S_['pP'][:, :].rearrange("p (c t) -> p c t", t=128), A3(Pc), ALU.add, r=[S_['pP'], Pc], w=[Pfin])
                s_['M'] = S_['M'][lvl % 2]
                if lvl < 5:
                    s_['N'] = S_['N'][lvl % 2]
        for c in range(NCH):
            S = Sbd[sidx % 2]; Sn = Sbd[(sidx + 1) % 2]; sidx += 1
            k.mm(pWUS[:, 0:128], AakT[:, c, :], Vtok[:, c, :], start=True, stop=False, r=[AakT, Vtok], w=[pWUS])
            k.mm(pWUS[:, 0:128], f2(a_bd, c), S[:, :], start=False, stop=True, r=[a_bd, S], w=[pWUS])
            k.cp(Wsb[:, :], pWUS[:, 0:128], r=[pWUS], w=[Wsb], e='act')
            k.mm(pWUS[:, 128:256], Pfin[:, c, :], Wsb[:, :], r=[Pfin, Wsb], w=[pWUS])
            k.cp(Usb[:, :], pWUS[:, 128:256], r=[pWUS], w=[Usb], e='dve')
            cc = c % 4
            k.mm(pY[:, cc * 128:(cc + 1) * 128], Vtok[:, c, :], ArkT[:, c, :], start=True, stop=False, r=[Vtok, ArkT], w=[pY])
            k.mm(pY[:, cc * 128:(cc + 1) * 128], S[:, :], f2(r_bd, c), start=False, stop=False, r=[S, r_bd], w=[pY])
            k.mm(pY[:, cc * 128:(cc + 1) * 128], Usb[:, :], ArbT[:, c, :], start=False, stop=True, r=[Usb, ArbT], w=[pY])
            k.mm(pWUS[:, 256:384], Kdt[:, c, :], Vtok[:, c, :], start=True, stop=False, r=[Kdt, Vtok], w=[pWUS])
            k.mm(pWUS[:, 256:384], Bdt[:, c, :], Usb[:, :], start=False, stop=True, r=[Bdt, Usb], w=[pWUS])
            k.stt(Sn[:, :], S[:, :], eb[:, c * 64 + 63:c * 64 + 64], pWUS[:, 256:384], ALU.mult, ALU.add, r=[S, eb, pWUS], w=[Sn])
            if cc == 3:
                g4 = c // 4
                pv = pY[:, :].rearrange("p (c h t) -> p c h t", h=2, t=64)
                k.cp(yT[0:64, g4 * 256:(g4 + 1) * 256].rearrange("p (c t) -> p c t", t=64), pv[0:64, :, 0, :], r=[pY], w=[yT], e='act')
                k.cp(yT[64:128, g4 * 256:(g4 + 1) * 256].rearrange("p (c t) -> p c t", t=64), pv[64:128, :, 1, :], r=[pY], w=[yT], e='act')
        k.mm(pp[:, :], C['blockones'][:, :], yT[:, :], r=[C['blockones'], yT], w=[pp])
        k.stt(tmp[:, :], pp[:, :], -1.0 / 64, yT[:, :], ALU.mult, ALU.add, r=[pp, yT], w=[tmp])
        k.tt(tmp2[:, :], tmp[:, :], tmp[:, :], ALU.mult, r=[tmp], w=[tmp2])
        k.mm(pp[:, :], C['blockones'][:, :], tmp2[:, :], r=[C['blockones'], tmp2], w=[pp])
        k.act(tmp2[:, :], pp[:, :], AF.Sqrt, bias=vx[:, 1:2], scale=1.0 / 64, r=[pp, vx], w=[tmp2])
        k.op('dve', lambda e: e.reciprocal(out=tmp2[:, :], in_=tmp2[:, :]), r=[tmp2], w=[tmp2])
        k.tt(tmp[:, :], tmp[:, :], tmp2[:, :], ALU.mult, r=[tmp, tmp2], w=[tmp])
        k.ts(tmp[:, :], tmp[:, :], GNG, GNB, op0=ALU.mult, op1=ALU.add, r=[tmp, vec], w=[tmp])
        k.tt(tmp[:, :], tmp[:, :], bonus[:, :], ALU.add, r=[tmp, bonus], w=[tmp])
        k.tt(yo[:, :], tmp[:, :], g_sb[:, :], ALU.mult, r=[tmp, g_sb], w=[yo])
        k.dma(over['y_dst'](0, blk) if 'y_dst' in over else y_rw[:, cs], yo[:, :], r=[yo])
        for (dst, c0, c1) in [(gq, 0, 64), (gk, 64, 128), (ggate, 256, 384), (gkl, 384, 400)]:
            m = c1 - c0
            for kc in range(8):
                k.mm(pp[0:m, :], wglb[:, kc, c0:c1], hb[:, kc, :], start=(kc == 0), stop=(kc == 7), r=[wglb, hb], w=[pp])
            k.cp(dst[0:m, :], pp[0:m, :], r=[pp], w=[dst], e='act')
        for g4 in range(2):
            for cc in range(4):
                c = g4 * 4 + cc
                for kc in range(8):
                    k.mm(pA[0:64, cc * 128:(cc + 1) * 128], hb[:, kc, c * 64:(c + 1) * 64], wglb[:, kc, 128:256], start=(kc == 0), stop=(kc == 7), r=[wglb, hb], w=[pA])
            k.cp(gvt[:, g4 * 4:(g4 + 1) * 4, :], pA[0:64, :].rearrange("p (c t) -> p c t", t=128), r=[pA], w=[gvt], e='act')
        k.mm(pp[0:64, :], gkw[:, :], gkl[:, :], r=[gkw, gkl], w=[pp])
        k.act(gsp[:, :], pp[0:64, :], AF.Exp, bias=vx[0:64, 3:4], scale=-1.0, r=[pp, vx], w=[gsp])
        k.act(gsp[:, :], gsp[:, :], AF.Ln, bias=1.0, r=[gsp], w=[gsp])
        k.op('dve', lambda e: e.tensor_tensor_scan(out=gb[:, :], data0=C['resetm'][0:64, :], data1=gsp[:, :], initial=0.0, op0=ALU.mult, op1=ALU.add), r=[C['resetm'], gsp], w=[gb])
        k.act(geb[:, :], gb[:, :], AF.Exp, scale=-1.0 / 16, r=[gb], w=[geb])
        k.act(genb[:, :], gb[:, :], AF.Exp, scale=1.0 / 16, r=[gb], w=[genb])
        k.stt(gqt[:, :], gq[:, :], 0.125, geb[:, :], ALU.mult, ALU.mult, r=[gq, geb], w=[gqt])
        k.tt(gkt[:, :], gk[:, :], genb[:, :], ALU.mult, r=[gk, genb], w=[gkt])
        g3 = lambda buf: buf[:, :].rearrange("p (c t) -> p c t", t=64)
        k.tt(g3(gkd), g3(gkt), g3(geb)[:, :, 63:64].to_broadcast([64, NCH, 64]), ALU.mult, r=[gkt, geb], w=[gkd])
        for c in range(NCH):
            k.mm(pA[0:64, c * 64:(c + 1) * 64], gkt[:, c * 64:(c + 1) * 64], gqt[:, c * 64:(c + 1) * 64], r=[gkt, gqt], w=[pA])
        k.tt(gAT[:, :, :], pA[0:64, :].rearrange("p (c t) -> p c t", t=64), C['m_ui'][0:64, :].rearrange("p (c t) -> p c t", t=64), ALU.mult, r=[pA, C['m_ui']], w=[gAT])
        for c in range(NCH):
            k.tr(pA[0:64, c * 64:(c + 1) * 64], gkd[:, c * 64:(c + 1) * 64], C['ident'][0:64, 0:64], r=[gkd, C['ident']], w=[pA])
        k.cp(gkdt[:, :, :], pA[0:64, :].rearrange("p (c t) -> p c t", t=64), r=[pA], w=[gkdt], e='act')
        for c in range(NCH):
            S = gS[gsidx % 2]; Sn = gS[(gsidx + 1) % 2]; gsidx += 1
            k.mm(pO[:, c * 64:(c + 1) * 64], gvt[:, c, :], gAT[:, c, :], start=True, stop=False, r=[gvt, gAT], w=[pO])
            k.mm(pO[:, c * 64:(c + 1) * 64], S[:, :], gqt[:, c * 64:(c + 1) * 64], start=False, stop=True, r=[S, gqt], w=[pO])
            k.mm(pWUS[0:64, 384:512], gkdt[:, c, :], gvt[:, c, :], r=[gkdt, gvt], w=[pWUS])
            k.stt(Sn[:, :], S[:, :], geb[:, c * 64 + 63:c * 64 + 64], pWUS[0:64, 384:512], ALU.mult, ALU.add, r=[S, geb, pWUS], w=[Sn])
        k.cp(goT[:, :], pO[:, :], r=[pO], w=[goT], e='act')
        k.tt(gsq[:, :], goT[:, :], goT[:, :], ALU.mult, r=[goT], w=[gsq])
        k.mm(pp[:, :], C['ones'][:, :], gsq[:, :], r=[C['ones'], gsq], w=[pp])
        k.act(gsq[:, :], pp[:, :], AF.Sqrt, bias=vx[:, 2:3], scale=1.0 / 128, r=[pp, vx], w=[gsq])
        k.op('dve', lambda e: e.reciprocal(out=gsq[:, :], in_=gsq[:, :]), r=[gsq], w=[gsq])
        k.stt(goT[:, :], goT[:, :], gv[:, 1:2], gsq[:, :], ALU.mult, ALU.mult, r=[goT, gv, gsq], w=[goT])
        k.act(gsq[:, :], ggate[:, :], AF.Silu, r=[ggate], w=[gsq])
        k.tt(gout[:, :], goT[:, :], gsq[:, :], ALU.mult, r=[goT, gsq], w=[gout])
        k.dma(over['y_dst'](1, blk) if 'y_dst' in over else y_gla[:, cs], gout[:, :], r=[gout])
        if 'after_blk' in over:
            over['after_blk'](blk)
    if own:
        k.finish()
        k.close()
        return nc
    k.pop()

def mix0_inputs(d, hT_b, j):
    W = d['ab_w_in'][0]
    ch = slice(128 * j, 128 * j + 128)
    RW = 512
    o_r, o_wl, o_k, o_v, o_al, o_gl = 0, 512, 576, 1088, 1600, 1664
    cols = np.concatenate([np.arange(o_r + 128 * j, o_r + 128 * j + 128), np.arange(o_k + 128 * j, o_k + 128 * j + 128),
                           np.arange(o_v + 128 * j, o_v + 128 * j + 128), np.arange(o_wl, o_wl + 64), np.arange(o_al, o_al + 64),
                           np.arange(o_gl, o_gl + 128)])
    w_rw = np.ascontiguousarray(W[:, cols])
    mu = d['rw_mu'][0][cols].reshape(5, 128).T
    G0 = 1792
    gq, gk, gv, gkl, gg = G0, G0 + 256, G0 + 512, G0 + 1024, G0 + 1040
    gcols = np.concatenate([np.arange(gq + 64 * j, gq + 64 * j + 64), np.arange(gk + 64 * j, gk + 64 * j + 64),
                            np.arange(gv + 128 * j, gv + 128 * j + 128), np.arange(gg + 128 * j, gg + 128 * j + 128), np.arange(gkl, gkl + 16)])
    w_gla = np.ascontiguousarray(W[:, gcols])
    rwvec = np.zeros((128, 16), np.float32)
    rwvec[:, 0:5] = mu
    for i, n in enumerate(['rw_w0', 'rw_a0', 'rw_k_k', 'rw_k_a']):
        rwvec[:, 5 + i] = d[n][0][ch]
    rwvec[:, 9] = d['rw_r_k'][0].reshape(512)[ch]
    rwvec[:, 10] = d['rw_gn_g'][0][ch]; rwvec[:, 11] = d['rw_gn_b'][0][ch]
    lora_wa = np.concatenate([d['rw_w2'][0][:, ch], d['rw_a2'][0][:, ch]], 0)
    g2c = np.ascontiguousarray(d['rw_g2'][0][:, ch])
    gk_w2 = np.ascontiguousarray(d['gla_gk_w2'][0][:, 64 * j:64 * j + 64])
    glavec = np.zeros((128, 2), np.float32)
    glavec[:64, 0] = d['gla_gk_b'][0][64 * j:64 * j + 64]; glavec[:, 1] = d['gla_norm_g'][0]
    im = {"hT": np.ascontiguousarray(hT_b), "w_rw": w_rw, "w_gla": w_gla, "rwvec": rwvec, "lora_wa": np.ascontiguousarray(lora_wa), "g2c": g2c,
          "gk_w2": gk_w2, "glavec": glavec}
    im.update(consts0())
    return im


import math

TB = 512
NCH = 8
NORM_EPS = 1e-5
TWO_PI = 2.0 * math.pi

def consts1():
    c = {}
    c["ident"] = np.eye(128, dtype=np.float32)
    c["ones"] = np.ones((128, 128), np.float32)
    p = np.arange(128)[:, None] % 64; q = np.arange(512)[None, :] % 64
    c["m_ui"] = (q >= p).astype(np.float32)
    rm = np.ones((128, 512), np.float32); rm[:, ::64] = 0
    c["resetm"] = rm
    c["iota"] = np.tile(np.arange(512, dtype=np.float32)[None, :], (128, 1))
    return c

def sincos(k, u, sn, cs, t1, t2, ti, sl):
    def wrap(x):
        k.ts(sl(t2), sl(x), 0.5, None, op0=ALU.is_gt, r=[x], w=[t2])
        k.tt(sl(x), sl(x), sl(t2), ALU.subtract, r=[x, t2], w=[x])
        k.ts(sl(t2), sl(x), -0.5, None, op0=ALU.is_lt, r=[x], w=[t2])
        k.tt(sl(x), sl(x), sl(t2), ALU.add, r=[x, t2], w=[x])
    k.cp(sl(ti), sl(u), r=[u], w=[ti])
    k.cp(sl(t1), sl(ti), r=[ti], w=[t1])
    k.tt(sl(t1), sl(u), sl(t1), ALU.subtract, r=[u, t1], w=[t1])
    wrap(t1)
    k.act(sl(sn), sl(t1), AF.Sin, scale=TWO_PI, r=[t1], w=[sn])
    k.ts(sl(t1), sl(t1), 0.25, None, op0=ALU.add, r=[t1], w=[t1])
    wrap(t1)
    k.act(sl(cs), sl(t1), AF.Sin, scale=TWO_PI, r=[t1], w=[cs])

def build_mix1(L, ctx=None):
    own = ctx is None
    nc = bass.Bass("TRN2", target_bir_lowering=False) if own else ctx['nc']
    pfx = '' if own else ctx['pfx']
    over = {} if own else ctx['over']
    nblk = L // TB
    def din(name, shape):
        if name in over:
            return over[name]
        return nc.dram_tensor(pfx + name, shape, F32, kind="ExternalInput").ap()
    hT = din("hT", [1024, L]) if "hT_blk" not in over else None
    w_cd = din("w_cd", [1024, 640])
    Bre = din("Bre", [4, 128, 128]); Bim = din("Bim", [4, 128, 128]); Cre = din("Cre", [4, 128, 128]); Cim = din("Cim", [4, 128, 128])
    s5vec = din("s5vec", [128, 16]); hgvec = din("hgvec", [128, 4])
    cd = {n: din(n, list(v.shape)) for n, v in consts1().items()}
    y_s5 = None if "y_dst" in over else nc.dram_tensor("y_s5", [128, L], F32, kind="ExternalOutput").ap()
    y_hg = None if "y_dst" in over else nc.dram_tensor("y_hg", [128, L], F32, kind="ExternalOutput").ap()
    k = KB(nc) if own else ctx['k']
    if not own:
        k.push()
    C = {}
    for n, v in consts1().items():
        C[n] = k.sb(list(v.shape), name='c_' + n); k.dma(C[n][:], cd[n], w=[C[n]])
    sv = k.sb([128, 16]); k.dma(sv[:], s5vec, w=[sv])
    hv = k.sb([128, 4]); k.dma(hv[:], hgvec, w=[hv])
    BreS = k.sb([128, 4, 128]); BimS = k.sb([128, 4, 128]); CreS = k.sb([128, 4, 128]); CimS = k.sb([128, 4, 128])
    for (dst, src) in [(BreS, Bre), (BimS, Bim), (CreS, Cre), (CimS, Cim)]:
        k.dma(dst[:], src.rearrange("s p n -> p s n"), w=[dst])
    k.ts(CimS[:, :, :], CimS[:, :, :], -1.0, None, op0=ALU.mult, r=[CimS], w=[CimS])
    wcdb = k.sb([128, 8, 640], BF16); k.dma(wcdb[:], w_cd.rearrange("(kc p) n -> p kc n", p=128), w=[wcdb], q='pool')
    hTb = [k.sb([128, 8, TB], BF16) for _ in range(2)]
    hT3 = hT.rearrange("(kc p) t -> p kc t", p=128) if hT is not None else None
    pp = k.ps([128, 512]); pA = k.ps([128, 512]); pbr = k.ps([128, 512]); pbi = k.ps([128, 512])
    py = k.ps([128, 512]); pO = k.ps([128, 512]); pS = k.ps([128, 512])
    T = lambda name: k.sb([128, TB], name=name)
    S4 = lambda name: k.sb([128, 4], name=name)
    lre, lim, dt, rho, th, f4, sn4, cs4, t4a, t4b, are, aim, nre, den, zre, zim, u512, s512, c512 = [S4(n) for n in
        ['lre', 'lim', 'dt', 'rho', 'th', 'f4', 'sn4', 'cs4', 't4a', 't4b', 'are', 'aim', 'nre', 'den', 'zre', 'zim', 'u512', 's512', 'c512']]
    ti4 = k.sb([128, 4], I32, name='ti4')
    A_ = lambda b: b[:, :]
    k.ts(A_(lre), sv[:, 0:4], -1e-4, None, op0=ALU.min, r=[sv], w=[lre])
    k.cp(A_(lim), sv[:, 4:8], r=[sv], w=[lim])
    k.act(A_(dt), sv[:, 8:12], AF.Exp, r=[sv], w=[dt])
    k.tt(A_(t4a), A_(lre), A_(dt), ALU.mult, r=[lre, dt], w=[t4a])
    k.act(A_(rho), A_(t4a), AF.Exp, r=[t4a], w=[rho])
    k.tt(A_(th), A_(lim), A_(dt), ALU.mult, r=[lim, dt], w=[th])
    k.ts(A_(f4), A_(th), 1.0 / TWO_PI, None, op0=ALU.mult, r=[th], w=[f4])
    sincos(k, f4, sn4, cs4, t4a, t4b, ti4, A_)
    k.tt(A_(are), A_(rho), A_(cs4), ALU.mult, r=[rho, cs4], w=[are])
    k.tt(A_(aim), A_(rho), A_(sn4), ALU.mult, r=[rho, sn4], w=[aim])
    k.ts(A_(nre), A_(are), -1.0, None, op0=ALU.add, r=[are], w=[nre])
    k.tt(A_(den), A_(lre), A_(lre), ALU.mult, r=[lre], w=[den])
    k.tt(A_(t4a), A_(lim), A_(lim), ALU.mult, r=[lim], w=[t4a])
    k.tt(A_(den), A_(den), A_(t4a), ALU.add, r=[den, t4a], w=[den])
    k.op('dve', lambda e: e.reciprocal(out=A_(den), in_=A_(den)), r=[den], w=[den])
    k.tt(A_(zre), A_(nre), A_(lre), ALU.mult, r=[nre, lre], w=[zre])
    k.tt(A_(t4a), A_(aim), A_(lim), ALU.mult, r=[aim, lim], w=[t4a])
    k.tt(A_(zre), A_(zre), A_(t4a), ALU.add, r=[zre, t4a], w=[zre])
    k.tt(A_(zre), A_(zre), A_(den), ALU.mult, r=[zre, den], w=[zre])
    k.tt(A_(zim), A_(aim), A_(lre), ALU.mult, r=[aim, lre], w=[zim])
    k.tt(A_(t4a), A_(nre), A_(lim), ALU.mult, r=[nre, lim], w=[t4a])
    k.tt(A_(zim), A_(zim), A_(t4a), ALU.subtract, r=[zim, t4a], w=[zim])
    k.tt(A_(zim), A_(zim), A_(den), ALU.mult, r=[zim, den], w=[zim])
    k.ts(A_(u512), A_(f4), float(TB), None, op0=ALU.mult, r=[f4], w=[u512])
    sincos(k, u512, s512, c512, t4a, t4b, ti4, A_)
    cosl = [T('cosl%d' % s) for s in range(4)]; sinl = [T('sinl%d' % s) for s in range(4)]
    Ere = [T('Ere%d' % s) for s in range(4)]; Eim = [T('Eim%d' % s) for s in range(4)]
    rhoT = [T('rhoT%d' % s) for s in range(4)]
    tu = T('tu'); tt1 = T('tt1'); tt2 = T('tt2'); tti = k.sb([128, TB], I32, name='tti')
    for s in range(4):
        k.ts(tu[:, :], C['iota'][:, :], f4[:, s:s + 1], None, op0=ALU.mult, r=[C['iota'], f4], w=[tu])
        sincos(k, tu, sinl[s], cosl[s], tt1, tt2, tti, A_)
        k.ts(Ere[s][:, :], cosl[s][:, :], zre[:, s:s + 1], None, op0=ALU.mult, r=[cosl[s], zre], w=[Ere[s]])
        k.stt(Ere[s][:, :], sinl[s][:, :], zim[:, s:s + 1], Ere[s][:, :], ALU.mult, ALU.add, r=[sinl[s], zim, Ere[s]], w=[Ere[s]])
        k.ts(Eim[s][:, :], cosl[s][:, :], zim[:, s:s + 1], None, op0=ALU.mult, r=[cosl[s], zim], w=[Eim[s]])
        k.stt(tt1[:, :], sinl[s][:, :], zre[:, s:s + 1], Eim[s][:, :], ALU.mult, ALU.subtract, r=[sinl[s], zre, Eim[s]], w=[tt1])
        k.ts(Eim[s][:, :], tt1[:, :], -1.0, None, op0=ALU.mult, r=[tt1], w=[Eim[s]])
        k.op('pool', lambda e: e.memset(rhoT[s][:, :], 1.0), w=[rhoT[s]])
        k.ts(rhoT[s][:, :], rhoT[s][:, :], rho[:, s:s + 1], None, op0=ALU.mult, r=[rhoT[s], rho], w=[rhoT[s]])
    carry = [k.sb([128, 2], name='carry%d' % s) for s in range(4)]
    for s in range(4):
        k.op('pool', lambda e: e.memset(carry[s][:, :], 0.0), w=[carry[s]])
    ctmp = k.sb([128, 2], name='ctmp')
    hx = k.sb([128, 4], name='hx')
    k.tt(hx[:, 0:1], hv[:, 1:2], hv[:, 0:1], ALU.subtract, r=[hv], w=[hx])
    k.act(hx[:, 0:1], hx[:, 0:1], AF.Sigmoid, r=[hx], w=[hx])
    k.ts(hx[:, 1:2], hx[:, 0:1], -1.0, 1.0, op0=ALU.mult, op1=ALU.add, r=[hx], w=[hx])
    k.ts(hx[:, 2:3], hx[:, 1:2], -1.0, None, op0=ALU.mult, r=[hx], w=[hx])
    k.op('pool', lambda e: e.memset(hx[:, 3:4], NORM_EPS), w=[hx])
    uT = T('uT'); xre = T('xre'); xim = T('xim'); gre = T('gre'); gim = T('gim'); w1_ = T('w1_'); w2_ = T('w2_'); hre = T('hre'); him = T('him')
    ys = T('ys'); yo = T('yo')
    hq = T('hq'); hf = T('hf'); hgate = T('hgate'); hit = k.sb([64, NCH, 128], name='hit')
    hlg = T('hlg'); hb = T('hb'); heb = T('heb'); henb = T('henb'); hqt = T('hqt'); hkt = T('hkt'); hkd = T('hkd')
    hAT = k.sb([64, NCH, 64], name='hAT'); hkdt = k.sb([64, NCH, 128], name='hkdt')
    hS = [k.sb([128, 128], name='hS%d' % i) for i in range(2)]
    k.op('pool', lambda e: e.memset(hS[0][:], 0.0), w=[hS[0]])
    hoT = T('hoT'); hsq = T('hsq'); hout = T('hout')
    hsidx = 0
    for blk in range(nblk):
        hb_ = hTb[blk % 2]
        cs = slice(blk * TB, (blk + 1) * TB)
        if hT3 is not None:
            k.dma(hb_[:], hT3[:, :, cs], w=[hb_], q='pool')
        else:
            hsrc_, hbufs_ = over['hT_blk'](blk)
            k.dma(hb_[:], hsrc_, r=hbufs_, w=[hb_], q='pool')
        for (dst, c0) in [(uT, 0), (hq, 128), (hf, 256), (hgate, 512)]:
            for kc in range(8):
                k.mm(pp[:, :], wcdb[:, kc, c0:c0 + 128], hb_[:, kc, :], start=(kc == 0), stop=(kc == 7), r=[wcdb, hb_], w=[pp])
            k.cp(dst[:, :], pp[:, :], r=[pp], w=[dst], e='act')
        for g4 in range(2):
            for cc in range(4):
                c = g4 * 4 + cc
                for kc in range(8):
                    k.mm(pA[0:64, cc * 128:(cc + 1) * 128], hb_[:, kc, c * 64:(c + 1) * 64], wcdb[:, kc, 384:512], start=(kc == 0), stop=(kc == 7), r=[wcdb, hb_], w=[pA])
            k.cp(hit[:, g4 * 4:(g4 + 1) * 4, :], pA[0:64, :].rearrange("p (c t) -> p c t", t=128), r=[pA], w=[hit], e='act')
        for s in range(4):
            k.mm(pbr[:, :], BreS[:, s, :], uT[:, :], r=[BreS, uT], w=[pbr])
            k.mm(pbi[:, :], BimS[:, s, :], uT[:, :], r=[BimS, uT], w=[pbi])
            k.tt(xre[:, :], pbr[:, :], Ere[s][:, :], ALU.mult, r=[pbr, Ere[s]], w=[xre])
            k.tt(w1_[:, :], pbi[:, :], Eim[s][:, :], ALU.mult, r=[pbi, Eim[s]], w=[w1_])
            k.tt(xre[:, :], xre[:, :], w1_[:, :], ALU.subtract, r=[xre, w1_], w=[xre])
            k.tt(xim[:, :], pbr[:, :], Eim[s][:, :], ALU.mult, r=[pbr, Eim[s]], w=[xim])
            k.tt(w2_[:, :], pbi[:, :], Ere[s][:, :], ALU.mult, r=[pbi, Ere[s]], w=[w2_])
            k.tt(xim[:, :], xim[:, :], w2_[:, :], ALU.add, r=[xim, w2_], w=[xim])
            k.op('dve', lambda e: e.tensor_tensor_scan(out=gre[:, :], data0=rhoT[s][:, :], data1=xre[:, :], initial=carry[s][:, 0:1], op0=ALU.mult, op1=ALU.add), r=[rhoT[s], xre, carry[s]], w=[gre])
            k.op('dve', lambda e: e.tensor_tensor_scan(out=gim[:, :], data0=rhoT[s][:, :], data1=xim[:, :], initial=carry[s][:, 1:2], op0=ALU.mult, op1=ALU.add), r=[rhoT[s], xim, carry[s]], w=[gim])
            k.ts(ctmp[:, 0:1], gim[:, TB - 1:TB], s512[:, s:s + 1], None, op0=ALU.mult, r=[gim, s512], w=[ctmp])
            k.ts(ctmp[:, 1:2], gim[:, TB - 1:TB], c512[:, s:s + 1], None, op0=ALU.mult, r=[gim, c512], w=[ctmp])
            k.stt(carry[s][:, 0:1], gre[:, TB - 1:TB], c512[:, s:s + 1], ctmp[:, 0:1], ALU.mult, ALU.subtract, r=[gre, c512, ctmp], w=[carry[s]])
            k.stt(carry[s][:, 1:2], gre[:, TB - 1:TB], s512[:, s:s + 1], ctmp[:, 1:2], ALU.mult, ALU.add, r=[gre, s512, ctmp], w=[carry[s]])
            k.tt(hre[:, :], gre[:, :], cosl[s][:, :], ALU.mult, r=[gre, cosl[s]], w=[hre])
            k.tt(w1_[:, :], gim[:, :], sinl[s][:, :], ALU.mult, r=[gim, sinl[s]], w=[w1_])
            k.tt(hre[:, :], hre[:, :], w1_[:, :], ALU.subtract, r=[hre, w1_], w=[hre])
            k.tt(him[:, :], gre[:, :], sinl[s][:, :], ALU.mult, r=[gre, sinl[s]], w=[him])
            k.tt(w2_[:, :], gim[:, :], cosl[s][:, :], ALU.mult, r=[gim, cosl[s]], w=[w2_])
            k.tt(him[:, :], him[:, :], w2_[:, :], ALU.add, r=[him, w2_], w=[him])
            k.mm(py[:, :], CreS[:, s, :], hre[:, :], start=(s == 0), stop=False, r=[CreS, hre], w=[py])
            k.mm(py[:, :], CimS[:, s, :], him[:, :], start=False, stop=(s == 3), r=[CimS, him], w=[py])
        k.stt(ys[:, :], uT[:, :], sv[:, 12:13], py[:, :], ALU.mult, ALU.add, r=[uT, sv, py], w=[ys])
        k.tt(w1_[:, :], ys[:, :], ys[:, :], ALU.mult, r=[ys], w=[w1_])
        k.ts(w1_[:, :], w1_[:, :], 0.044715, 1.0, op0=ALU.mult, op1=ALU.add, r=[w1_], w=[w1_])
        k.tt(w1_[:, :], w1_[:, :], ys[:, :], ALU.mult, r=[w1_, ys], w=[w1_])
        k.act(w1_[:, :], w1_[:, :], AF.Sigmoid, scale=1.5957691216057308, r=[w1_], w=[w1_])
        k.tt(yo[:, :], ys[:, :], w1_[:, :], ALU.mult, r=[ys, w1_], w=[yo])
        k.dma(over['y_dst'](0, blk) if 'y_dst' in over else y_s5[:, cs], yo[:, :], r=[yo])
        k.act(hsq[:, :], hf[:, :], AF.Sigmoid, r=[hf], w=[hsq])
        k.ts(hlg[:, :], hsq[:, :], hx[:, 1:2], hx[:, 0:1], op0=ALU.mult, op1=ALU.add, r=[hsq, hx], w=[hlg])
        k.act(hlg[:, :], hlg[:, :], AF.Ln, r=[hlg], w=[hlg])
        k.ts(hkt[:, :], hsq[:, :], hx[:, 2:3], hx[:, 1:2], op0=ALU.mult, op1=ALU.add, r=[hsq, hx], w=[hkt])
        k.op('dve', lambda e: e.tensor_tensor_scan(out=hb[:, :], data0=C['resetm'][:, :], data1=hlg[:, :], initial=0.0, op0=ALU.mult, op1=ALU.add), r=[C['resetm'], hlg], w=[hb])
        k.act(heb[:, :], hb[:, :], AF.Exp, r=[hb], w=[heb])
        k.act(henb[:, :], hb[:, :], AF.Exp, scale=-1.0, r=[hb], w=[henb])
        k.act(hqt[:, :], hq[:, :], AF.Silu, r=[hq], w=[hqt])
        k.tt(hqt[:, :], hqt[:, :], heb[:, :], ALU.mult, r=[hqt, heb], w=[hqt])
        k.tt(hkt[:, :], hkt[:, :], henb[:, :], ALU.mult, r=[hkt, henb], w=[hkt])
        g3 = lambda buf: buf[:, :].rearrange("p (c t) -> p c t", t=64)
        k.tt(g3(hkd), g3(hkt), g3(heb)[:, :, 63:64].to_broadcast([128, NCH, 64]), ALU.mult, r=[hkt, heb], w=[hkd])
        for c in range(NCH):
            k.mm(pA[0:64, c * 64:(c + 1) * 64], hkt[:, c * 64:(c + 1) * 64], hqt[:, c * 64:(c + 1) * 64], r=[hkt, hqt], w=[pA])
        k.tt(hAT[:, :, :], pA[0:64, :].rearrange("p (c t) -> p c t", t=64), C['m_ui'][0:64, :].rearrange("p (c t) -> p c t", t=64), ALU.mult, r=[pA, C['m_ui']], w=[hAT])
        for g4 in range(2):
            for cc in range(4):
                c = g4 * 4 + cc
                k.tr(pA[0:64, cc * 128:(cc + 1) * 128], hkd[:, c * 64:(c + 1) * 64], C['ident'][:, :], r=[hkd, C['ident']], w=[pA])
            k.cp(hkdt[:, g4 * 4:(g4 + 1) * 4, :], pA[0:64, :].rearrange("p (c t) -> p c t", t=128), r=[pA], w=[hkdt], e='act')
        for c in range(NCH):
            S = hS[hsidx % 2]; Sn = hS[(hsidx + 1) % 2]; hsidx += 1
            k.mm(pO[:, c * 64:(c + 1) * 64], hit[:, c, :], hAT[:, c, :], start=True, stop=False, r=[hit, hAT], w=[pO])
            k.mm(pO[:, c * 64:(c + 1) * 64], S[:, :], hqt[:, c * 64:(c + 1) * 64], start=False, stop=True, r=[S, hqt], w=[pO])
            k.mm(pS[:, 0:128], hkdt[:, c, :], hit[:, c, :], r=[hkdt, hit], w=[pS])
            k.stt(Sn[:, :], S[:, :], heb[:, c * 64 + 63:c * 64 + 64], pS[:, 0:128], ALU.mult, ALU.add, r=[S, heb, pS], w=[Sn])
        k.cp(hoT[:, :], pO[:, :], r=[pO], w=[hoT], e='act')
        k.tt(hsq[:, :], hoT[:, :], hoT[:, :], ALU.mult, r=[hoT], w=[hsq])
        k.mm(pp[:, :], C['ones'][:, :], hsq[:, :], r=[C['ones'], hsq], w=[pp])
        k.act(hsq[:, :], pp[:, :], AF.Sqrt, bias=hx[:, 3:4], scale=1.0 / 128, r=[pp, hx], w=[hsq])
        k.op('dve', lambda e: e.reciprocal(out=hsq[:, :], in_=hsq[:, :]), r=[hsq], w=[hsq])
        k.stt(hoT[:, :], hoT[:, :], hv[:, 2:3], hsq[:, :], ALU.mult, ALU.mult, r=[hoT, hv, hsq], w=[hoT])
        k.act(hsq[:, :], hgate[:, :], AF.Silu, r=[hgate], w=[hsq])
        k.tt(hout[:, :], hoT[:, :], hsq[:, :], ALU.mult, r=[hoT, hsq], w=[hout])
        k.dma(over['y_dst'](1, blk) if 'y_dst' in over else y_hg[:, cs], hout[:, :], r=[hout])
        if 'after_blk' in over:
            over['after_blk'](blk)
    if own:
        k.finish()
        k.close()
        return nc
    k.pop()

def mix1_inputs(d, hT_b, j):
    W = d['cd_w_in'][0]
    cols = np.concatenate([np.arange(128 * j, 128 * j + 128)] + [np.arange(512 * m + 128 * j, 512 * m + 128 * j + 128) for m in (1, 2, 3, 4)])
    w_cd = np.ascontiguousarray(W[:, cols])
    Bre = np.zeros((4, 128, 128), np.float32); Bim = np.zeros_like(Bre); Cre = np.zeros_like(Bre); Cim = np.zeros_like(Bre)
    s5vec = np.zeros((128, 16), np.float32)
    for st in range(4):
        for gl in range(2):
            g = 8 * j + 2 * st + gl
            chs = slice((2 * st + gl) * 16, (2 * st + gl) * 16 + 16); ps = slice(gl * 64, gl * 64 + 64)
            Bre[st, chs, ps] = d['s5_b_re'][0][g].T; Bim[st, chs, ps] = d['s5_b_im'][0][g].T
            Cre[st, ps, chs] = d['s5_c_re'][0][g].T; Cim[st, ps, chs] = d['s5_c_im'][0][g].T
            s5vec[ps, st] = d['s5_a_re'][0][g]; s5vec[ps, 4 + st] = d['s5_a_im'][0][g]; s5vec[ps, 8 + st] = d['s5_log_dt'][0][g]
    s5vec[:, 12] = d['s5_d'][0][128 * j:128 * j + 128]
    hgvec = np.zeros((128, 4), np.float32)
    hgvec[:, 0] = d['hg_lb'][0][128 * j:128 * j + 128]; hgvec[:, 1] = d['hg_lb'][1][128 * j:128 * j + 128]; hgvec[:, 2] = d['hg_norm_g'][0]
    im = {"hT": np.ascontiguousarray(hT_b), "w_cd": w_cd, "Bre": Bre, "Bim": Bim, "Cre": Cre, "Cim": Cim, "s5vec": s5vec, "hgvec": hgvec}
    im.update(consts1())
    return im


GROUPS = [[0, 1, 2, 3], [4, 5, 6, 7]]

def build_fused(L, NT):
    nc = bass.Bass("TRN2", target_bir_lowering=False)
    k = KB(nc)
    D = 1024
    CW = min(1024, NT)
    NQY = L // CW; BPC = CW // TB
    HW_ = 512
    NQH = NT // HW_
    def ydram(tag):
        src = nc.dram_tensor(tag + "src", [NQY, 256, CW], F32).ap(); dst = nc.dram_tensor(tag + "all", [NQY, 1024, CW], F32).ap()
        return src, dst, [Buf(None, tag + 'all%d' % q) for q in range(NQY)]
    y0src, y0all, Y0 = ydram("y0"); y1src, y1all, Y1 = ydram("y1")
    h0tok = nc.dram_tensor("h0tok", [NT, D], F32).ap()
    h0Tsrc = nc.dram_tensor("h0Tsrc", [NQH, D, HW_], BF16).ap(); h0Tall = nc.dram_tensor("h0Tall", [NQH, 4 * D, HW_], BF16).ap()
    H0 = [Buf(None, 'h0Tall%d' % q) for q in range(NQH)]
    pid = nc.sync.partition_id()
    rank = pid % 4
    tag_of = {}
    def y_hooks(ysrc, yall, YB):
        def y_dst(which, blk):
            q = blk // BPC; c0 = (blk % BPC) * TB
            return ysrc[q][which * 128:(which + 1) * 128, c0:c0 + TB]
        def after_blk(blk):
            if blk % BPC == BPC - 1:
                q = blk // BPC
                k.allgather(ysrc[q], yall[q], YB[q], GROUPS)
        NM = NT // CW
        ymine = nc.dram_tensor(tag_of[id(ysrc)] + "mine", [NM, 1024, CW], F32).ap()
        YM = [[Buf(None, 'ym%d_%d' % (m, hh)) for hh in range(2)] for m in range(NM)]
        def pre():
            for m in range(NM):
                qe = rank * NM + m
                for hh in range(2):
                    rs = slice(hh * 512, (hh + 1) * 512)
                    k.dma(ymine[m][rs, :], yall[bass.ds(qe, 1)].rearrange("o r t -> (o r) t")[rs, :], r=YB, w=[YM[m][hh]])
        def yT_src(i):
            m = (i * 128) // CW
            c0 = (i * 128) % CW
            v = ymine[m].rearrange("(r two p) t -> p r two t", two=2, p=128)
            return v[:, :, 0, c0:c0 + 128], v[:, :, 1, c0:c0 + 128], YM[m]
        yT_src.pre = pre
        return y_dst, after_blk, yT_src
    tag_of[id(y0src)] = 'y0'; tag_of[id(y1src)] = 'y1'
    yd0, ab0, ys0 = y_hooks(y0src, y0all, Y0)
    yd1, ab1, ys1 = y_hooks(y1src, y1all, Y1)
    build_mix0(L, ctx=dict(nc=nc, k=k, pfx='m0_', over={"y_dst": yd0, "after_blk": ab0}))
    def hT_dst(i):
        q = (i * 128) // HW_; c0 = (i * 128) % HW_
        return h0Tsrc[q].rearrange("(kc p) t -> p kc t", p=128)[:, :, c0:c0 + 128]
    def after_tile(i):
        if ((i + 1) * 128) % HW_ == 0:
            q = (i * 128) // HW_
            k.allgather(h0Tsrc[q], h0Tall[q], H0[q], GROUPS)
    build_post(NT, False, ctx=dict(nc=nc, k=k, pfx='p0_', over={"yT_src": ys0, "out": h0tok, "hT_dst": hT_dst, "after_tile": after_tile}))
    def hT_blk(blk):
        r = blk // NQH; lb = blk % NQH
        return h0Tall[lb].rearrange("(r kc p) t -> p r kc t", r=4, p=128)[:, r, :, :], [H0[lb]]
    build_mix1(L, ctx=dict(nc=nc, k=k, pfx='m1_', over={"y_dst": yd1, "after_blk": ab1, "hT_blk": hT_blk}))
    build_post(NT, True, ctx=dict(nc=nc, k=k, pfx='p1_', over={"yT_src": ys1, "h_tok": h0tok}))
    k.finish()
    k.close()
    return nc

_CACHE = {}

def _post_inputs(d, L_, NT_):
    im = {"w_out": (d['ab_w_out'][0] if L_ == 0 else d['cd_w_out'][0]),
          "ln": np.stack([d['ln1_g'][L_], d['ln1_b'][L_], d['ln2_g'][L_], d['ln2_b'][L_]]),
          "wr": np.ascontiguousarray(np.concatenate([d['moe_wg'][L_], d['moe_we'][L_].transpose(1, 0, 2).reshape(1024, 32)], 1)),
          "br": np.concatenate([d['moe_bg'][L_], d['moe_be'][L_].reshape(32)])[None, :],
          "w1r": np.ascontiguousarray(d['moe_w1'][L_].reshape(32, 8, 128, 512).transpose(0, 2, 1, 3)).reshape(4096, 4096),
          "w3r": np.ascontiguousarray(d['moe_w3'][L_].reshape(32, 8, 128, 512).transpose(0, 2, 1, 3)).reshape(4096, 4096),
          "w2r": np.ascontiguousarray(d['moe_w2'][L_].reshape(32, 4, 128, 1024).transpose(0, 2, 1, 3)).reshape(4096, 4096)}
    im.update(post_consts(NT_))
    if L_ == 1:
        im["glu_w"] = d['s5_glu_w'][0]
        im["glu_b"] = np.ascontiguousarray(d["s5_glu_b"][0].reshape(4, 128).T)
    return im

def kernel(**inputs):
    d = {k_: np.asarray(v, dtype=np.float32) for k_, v in inputs.items()}
    x = d['x']
    B, L, D = x.shape
    NC = 8
    NT = B * L // NC
    cores = list(range(NC))
    if 'fused' not in _CACHE:
        _CACHE['fused'] = build_fused(L, NT)
    nc = _CACHE['fused']
    xT = [np.ascontiguousarray(x[b].T) for b in range(B)]
    xflat = x.reshape(B * L, D)
    p0 = _post_inputs(d, 0, NT); p1 = _post_inputs(d, 1, NT)
    ims = []
    for c in cores:
        b, j = c // 4, c % 4
        im = {}
        for n_, v_ in mix0_inputs(d, xT[b], j).items():
            im['m0_' + n_] = v_
        for n_, v_ in mix1_inputs(d, None, j).items():
            if n_ != 'hT':
                im['m1_' + n_] = v_
        for n_, v_ in p0.items():
            im['p0_' + n_] = v_
        for n_, v_ in p1.items():
            im['p1_' + n_] = v_
        im['p0_h_tok'] = np.ascontiguousarray(xflat[c * NT:(c + 1) * NT])
        ims.append(im)
    res = run_bass_kernel_spmd(nc, ims, core_ids=cores).results
    out = np.concatenate([res[c]["p1_out"] for c in cores], 0).reshape(B, L, D)
    return out.astype(np.float32)
```

```python
import numpy as np
from contextlib import ExitStack
import concourse.bass as bass
import concourse.mybir as mybir
from concourse.bass_utils import run_bass_kernel_spmd

F32 = mybir.dt.float32
BF16 = mybir.dt.bfloat16
I32 = mybir.dt.int32
AF = mybir.ActivationFunctionType
ALU = mybir.AluOpType
AX = mybir.AxisListType

class Buf:
    def __init__(self, t, name):
        self.t = t; self.name = name
        self.w = None
        self.r = {}
    def __getitem__(self, key):
        return self.t[key]

class KB:
    def __init__(self, nc, n_dma_sems=32):
        self.nc = nc
        self.es = ExitStack()
        self.scopes = [self.es]
        self.eng = {'pe': nc.tensor, 'act': nc.scalar, 'dve': nc.vector, 'pool': nc.gpsimd, 'sp': nc.sync}
        self.sem = {}
        self.cnt = {}
        for e in self.eng:
            self.sem[e] = self.es.enter_context(nc.semaphore('s_' + e))
            self.cnt[e] = 0
        self.nd = n_dma_sems
        for i in range(n_dma_sems):
            self.sem['d%d' % i] = self.es.enter_context(nc.semaphore('s_d%d' % i))
            self.cnt['d%d' % i] = 0
        self.dma_rr = 0
        self.seen = {e: {} for e in self.eng}
        self.nbuf = 0
    def sb(self, shape, dtype=F32, name=None):
        self.nbuf += 1
        name = ('%s_%d' % (name, self.nbuf)) if name else ('b%d' % self.nbuf)
        t = self.scopes[-1].enter_context(self.nc.sbuf_tensor(name, list(shape), dtype))
        return Buf(t, name)
    def ps(self, shape, dtype=F32, name=None):
        self.nbuf += 1
        name = ('%s_%d' % (name, self.nbuf)) if name else ('p%d' % self.nbuf)
        t = self.scopes[-1].enter_context(self.nc.psum_tensor(name, list(shape), dtype))
        return Buf(t, name)
    def _need(self, e, key, val):
        if e == 'pe' and key == 'pe':
            return
        if val <= self.seen[e].get(key, 0):
            return
        self.eng[e].wait_ge(self.sem[key], val)
        self.seen[e][key] = val
    def _deps(self, e, r, w):
        for b in r:
            if b.w is not None:
                self._need(e, *b.w)
        for b in w:
            if b.w is not None:
                self._need(e, *b.w)
            for key, val in b.r.items():
                self._need(e, key, val)
    def op(self, e, ins_fn, r=(), w=()):
        self._deps(e, r, w)
        ins = ins_fn(self.eng[e])
        self.cnt[e] += 1
        ins.then_inc(self.sem[e], 1)
        v = self.cnt[e]
        for b in r:
            b.r[e] = v
        for b in w:
            b.w = (e, v); b.r = {}
        return ins
    def dma(self, out_ap, in_ap, r=(), w=(), q='sp', **kw):
        self._deps(q, r, w)
        key = 'd%d' % self.dma_rr
        self.dma_rr = (self.dma_rr + 1) % self.nd
        if self.cnt[key] > 0:
            self._need(q, key, self.cnt[key])
        ins = self.eng[q].dma_start(out=out_ap, in_=in_ap, **kw)
        self.cnt[key] += 16
        ins.then_inc(self.sem[key], 16)
        v = self.cnt[key]
        for b in r:
            b.r[key] = v
        for b in w:
            b.w = (key, v); b.r = {}
        return ins
    def push(self):
        self.scopes.append(ExitStack())
    def pop(self):
        self.barrier()
        self.scopes.pop().close()
    def allgather(self, src_ap, dst_ap, dstbuf, groups):
        for key in list(self.cnt):
            if key.startswith('d') and self.cnt[key] > 0:
                self._need('pool', key, self.cnt[key])
        for key, val in dstbuf.r.items():
            self._need('pool', key, val)
        key = 'cc%d' % len([x for x in self.sem if x.startswith('cc')])
        sem = self.es.enter_context(self.nc.semaphore('s_' + key))
        ins = self.nc.gpsimd.collective_compute("AllGather", ALU.bypass, replica_groups=groups, ins=[src_ap.opt()], outs=[dst_ap.opt()])
        ins.then_inc(sem)
        self.sem[key] = sem; self.cnt[key] = 1
        dstbuf.w = (key, 1); dstbuf.r = {}
    def ind(self, out, out_off, in_, in_off, r=(), w=(), bounds=None):
        self._deps('pool', r, w)
        key = 'd%d' % self.dma_rr
        self.dma_rr = (self.dma_rr + 1) % self.nd
        if self.cnt[key] > 0:
            self._need('pool', key, self.cnt[key])
        kw = {}
        if bounds is not None:
            if not hasattr(self, '_breg'):
                self._breg = {}
            if bounds not in self._breg:
                self._breg[bounds] = self.nc.gpsimd.alloc_register('bnd%d' % len(self._breg))
            self.nc.gpsimd.reg_mov(self._breg[bounds], int(bounds))
            kw['bounds_check'] = self._breg[bounds]; kw['oob_is_err'] = False
        ins = self.nc.gpsimd.indirect_dma_start(out=out, out_offset=out_off, in_=in_, in_offset=in_off, **kw)
        self.cnt[key] += 16
        ins.then_inc(self.sem[key], 16)
        v = self.cnt[key]
        for b in r:
            b.r[key] = v
        for b in w:
            b.w = (key, v); b.r = {}
        return ins
    def finish(self):
        for i in range(self.nd):
            key = 'd%d' % i
            if self.cnt[key] > 0:
                self._need('sp', key, self.cnt[key])
    def close(self):
        self.es.close()

def _mm(self, out, lhsT, rhs, start=True, stop=True, r=(), w=()):
    return self.op('pe', lambda e: e.matmul(out, lhsT=lhsT, rhs=rhs, start=start, stop=stop), r=r, w=w)
def _tr(self, out, in_, ident, r=(), w=()):
    return self.op('pe', lambda e: e.transpose(out=out, in_=in_, identity=ident), r=r, w=w)
def _act(self, out, in_, func, bias=None, scale=1.0, accum_out=None, r=(), w=()):
    kw = {}
    if bias is not None: kw['bias'] = bias
    if accum_out is not None: kw['accum_out'] = accum_out
    return self.op('act', lambda e: e.activation(out=out, in_=in_, func=func, scale=scale, **kw), r=r, w=w)
def _tt(self, out, in0, in1, op, r=(), w=(), e='dve'):
    return self.op(e, lambda g: g.tensor_tensor(out=out, in0=in0, in1=in1, op=op), r=r, w=w)
def _ts(self, out, in0, s1, s2=None, op0=ALU.mult, op1=None, r=(), w=(), e='dve', accum_out=None):
    kw = {}
    if op1 is not None: kw['op1'] = op1
    if accum_out is not None: kw['accum_out'] = accum_out
    return self.op(e, lambda g: g.tensor_scalar(out=out, in0=in0, scalar1=s1, scalar2=s2, op0=op0, **kw), r=r, w=w)
def _stt(self, out, in0, scalar, in1, op0, op1, r=(), w=()):
    return self.op('dve', lambda g: g.scalar_tensor_tensor(out=out, in0=in0, scalar=scalar, in1=in1, op0=op0, op1=op1), r=r, w=w)
def _cp(self, out, in_, r=(), w=(), e='dve'):
    if e == 'act':
        return self.op('act', lambda g: g.activation(out=out, in_=in_, func=AF.Copy), r=r, w=w)
    return self.op(e, lambda g: g.tensor_copy(out=out, in_=in_), r=r, w=w)
def _barrier(self):
    for e in self.eng:
        for key in self.cnt:
            if key != e and self.cnt[key] > 0:
                self._need(e, key, self.cnt[key])
KB.mm = _mm; KB.tr = _tr; KB.act = _act; KB.tt = _tt; KB.ts = _ts; KB.stt = _stt; KB.cp = _cp; KB.barrier = _barrier


ALPHA = 4.0 ** 0.25
DEBUG = False
GBLK = 512
EPS = 1e-5

def layer_norm(k, src, dst, gbc, bbc, tmp):
    st, mv, rs = tmp
    k.op('dve', lambda e: e.bn_stats(out=st[:, 0:6], in_=src[:, 0:512]), r=[src], w=[st])
    k.op('dve', lambda e: e.bn_stats(out=st[:, 6:12], in_=src[:, 512:1024]), r=[src], w=[st])
    k.op('dve', lambda e: e.bn_aggr(out=mv[:, 0:2], in_=st[:, 0:12]), r=[st], w=[mv])
    k.act(rs[:, 0:1], mv[:, 1:2], AF.Sqrt, bias=k.eps_ap, r=[mv], w=[rs])
    k.op('dve', lambda e: e.reciprocal(out=rs[:, 1:2], in_=rs[:, 0:1]), r=[rs], w=[rs])
    k.stt(dst[:, :], src[:, :], mv[:, 0:1], gbc[:, :], ALU.subtract, ALU.mult, r=[src, mv, gbc], w=[dst])
    k.stt(dst[:, :], dst[:, :], rs[:, 1:2], bbc[:, :], ALU.mult, ALU.add, r=[dst, rs, bbc], w=[dst])

def post_consts(NT):
    G = min(GBLK, NT); J = max(1, NT // G); NOV = (2 * NT) // G - 1 if J > 1 else 0; NBLK = 32 + NOV
    c = {}
    c["ident"] = np.eye(128, dtype=np.float32)
    c["ut"] = np.triu(np.ones((128, 128), np.float32), 1)
    c["ones"] = np.ones((128, 128), np.float32)
    c["thr"] = np.tile(((np.arange(max(J - 1, 1), dtype=np.float32) + 1) * G)[None, None, :], (128, 32, 1)).reshape(128, 32 * max(J - 1, 1))
    c["biota"] = np.tile(np.arange(max(NOV, 1), dtype=np.float32)[None, :, None], (128, 1, 32)).reshape(128, max(NOV, 1) * 32)
    c["eiota"] = np.tile((np.arange(32, dtype=np.float32) * G)[None, :], (128, 1))
    c["iotap"] = np.arange(128, dtype=np.float32)[:, None].copy()
    return c

def build_post(NT, layer1, NE=32, ctx=None):
    own = ctx is None
    nc = bass.Bass("TRN2", target_bir_lowering=False) if own else ctx['nc']
    pfx = '' if own else ctx['pfx']
    over = {} if own else ctx['over']
    D = 1024
    ntile = NT // 128
    TG = min(GBLK, NT); ntg = NT // TG
    def din(name, shape):
        if name in over:
            return over[name]
        return nc.dram_tensor(pfx + name, shape, F32, kind="ExternalInput").ap()
    h_tok = din("h_tok", [NT, D]); yT = din("yT", [D, NT]) if 'yT_src' not in over else None; w_out = din("w_out", [D, D])
    ln = din("ln", [4, D])
    wr = din("wr", [D, 36]); br = din("br", [1, 36])
    w1r = din("w1r", [4096, 4096]); w3r = din("w3r", [4096, 4096]); w2r = din("w2r", [4096, 4096])
    G = min(GBLK, NT); J = max(1, NT // G); NOV = (2 * NT) // G - 1 if J > 1 else 0; NBLK = 32 + NOV; NSUB = G // 128; NROWS = NBLK * G
    JT = max(J - 1, 1); NOVA = max(NOV, 1)
    cd = {n: din(n, list(v.shape)) for n, v in post_consts(NT).items()}
    ident_d = cd["ident"]
    pfx_ = 'L1' if layer1 else 'L0'
    kind_ = "ExternalOutput" if DEBUG else "Internal"
    h1f_d = nc.dram_tensor(pfx_ + "h1f_d", [NT, D], F32, kind=kind_).ap(); h1b_d = nc.dram_tensor(pfx_ + "h1b_d", [NT, D], BF16, kind=kind_).ap()
    xs_d = nc.dram_tensor(pfx_ + "xs_d", [NROWS + 1, D], BF16, kind=kind_).ap(); yrows_d = nc.dram_tensor(pfx_ + "yrows_d", [NROWS, D], BF16, kind=kind_).ap()
    if layer1:
        glu_w = din("glu_w", [512, 512]); glu_b = din("glu_b", [128, 4])
    out = over["out"] if "out" in over else nc.dram_tensor(pfx + "out", [NT, D], F32, kind="ExternalOutput").ap()
    k = KB(nc) if own else ctx['k']
    if not own:
        k.push()
    ident = k.sb([128, 128]); k.dma(ident[:], ident_d, w=[ident])
    epsb = k.sb([128, 1]); k.op('pool', lambda e: e.memset(epsb[:], EPS), w=[epsb]); k.eps_ap = epsb[:, 0:1]
    cgate = [k.sb([128, 32], name='cg%d' % i) for i in range(ntile)]
    CC = {}
    for n_ in ["ut", "ones", "thr", "biota", "iotap", "eiota"]:
        CC[n_] = k.sb(list(post_consts(NT)[n_].shape), name='pc_' + n_); k.dma(CC[n_][:], cd[n_], w=[CC[n_]])
    identb = k.sb([128, 128], BF16); k.cp(identb[:], ident[:], r=[ident], w=[identb])
    ln2g = k.sb([128, D]); ln2b = k.sb([128, D])
    k.dma(ln2g[:], ln[2:3, :].partition_broadcast(128), w=[ln2g]); k.dma(ln2b[:], ln[3:4, :].partition_broadcast(128), w=[ln2b])
    st = k.sb([128, 12]); mv = k.sb([128, 2]); rs = k.sb([128, 2]); lntmp = (st, mv, rs)
    k.push()
    woutb = k.sb([128, 8, D], BF16); k.dma(woutb[:], w_out.rearrange("(kc p) d -> p kc d", p=128), w=[woutb], q='pool')
    ln1g = k.sb([128, D]); ln1b = k.sb([128, D])
    k.dma(ln1g[:], ln[0:1, :].partition_broadcast(128), w=[ln1g]); k.dma(ln1b[:], ln[1:2, :].partition_broadcast(128), w=[ln1b])
    wrs = k.sb([128, 8, 36]); k.dma(wrs[:], wr.rearrange("(kc p) n -> p kc n", p=128), w=[wrs])
    brb = k.sb([128, 36]); k.dma(brb[:], br.partition_broadcast(128), w=[brb])
    if layer1:
        glub = k.sb([128, 4, 512], BF16); k.dma(glub[:], glu_w.rearrange("(kc p) n -> p kc n", p=128), w=[glub], q='pool')
        glubias = k.sb([128, 4]); k.dma(glubias[:], glu_b, w=[glubias])
    htile = [k.sb([128, D]) for _ in range(2)]
    ytb = [k.sb([128, 8, 128], BF16) for _ in range(2)]
    ytfA = [k.sb([128, 4, 128]) for _ in range(2)]; ytfB = [k.sb([128, 4, 128]) for _ in range(2)]
    rbuf = [k.sb([128, D]) for _ in range(2)]
    h1 = [k.sb([128, D]) for _ in range(2)]
    h1bt = [k.sb([128, D], BF16) for _ in range(2)]
    h1Tf = [k.sb([128, 8, 128]) for _ in range(2)]
    pmix = k.ps([128, D]); pT = k.ps([128, D]); plg = k.ps([128, 36])
    pglu = k.ps([128, 512]) if layer1 else None
    sm = {n: k.sb([128, w_], name='sm_' + n) for n, w_ in [('lgs', 36), ('negm', 1), ('oh', 4), ('e4', 4), ('s4', 1), ('pg', 1), ('fs', 8), ('t8', 8), ('sel', 8), ('negm1', 1), ('ex', 8), ('e2', 1), ('coef', 1), ('cf', 8)]}
    if layer1:
        ysbs = [k.sb([128, 4, 128], BF16) for _ in range(2)]; zss = [k.sb([128, 4, 128]) for _ in range(2)]
    yT3 = yT.rearrange("(kc p) t -> p kc t", p=128) if yT is not None else None
    def load_yT(i, b):
        tsl_ = slice(i * 128, (i + 1) * 128)
        if yT3 is not None:
            k.dma(ytfA[b][:], yT3[:, 0:4, tsl_], w=[ytfA[b]])
            k.dma(ytfB[b][:], yT3[:, 4:8, tsl_], w=[ytfB[b]])
        else:
            if i == 0:
                over['yT_src'].pre()
            srcA, srcB, bufs = over['yT_src'](i)
            k.dma(ytfA[b][:], srcA, r=bufs, w=[ytfA[b]])
            k.dma(ytfB[b][:], srcB, r=bufs, w=[ytfB[b]])
    for i in range(ntile):
        b = i % 2
        tsl = slice(i * 128, (i + 1) * 128)
        k.dma(htile[b][:], h_tok[tsl, :], w=[htile[b]])
        if layer1:
            load_yT(i, b)
            k.cp(ytb[b][:, 4:8, :], ytfB[b][:], r=[ytfB[b]], w=[ytb[b]], e='act')
            ysb = ysbs[b]; zs = zss[b]
            k.cp(ysb[:], ytfA[b][:], r=[ytfA[b]], w=[ysb], e='act')
            for oc in range(4):
                for kc in range(4):
                    k.mm(pglu[:, oc * 128:(oc + 1) * 128], glub[:, kc, oc * 128:(oc + 1) * 128], ysb[:, kc, :], start=(kc == 0), stop=(kc == 3), r=[glub, ysb], w=[pglu])
            for oc in range(4):
                k.act(zs[:, oc, :], pglu[:, oc * 128:(oc + 1) * 128], AF.Sigmoid, bias=glubias[:, oc:oc + 1], r=[pglu, glubias], w=[zs])
            k.tt(ytb[b][:, 0:4, :], zs[:], ytfA[b][:], ALU.mult, r=[zs, ytfA[b]], w=[ytb[b]])
        else:
            load_yT(i, b)
            k.cp(ytb[b][:, 0:4, :], ytfA[b][:], r=[ytfA[b]], w=[ytb[b]], e='dve')
            k.cp(ytb[b][:, 4:8, :], ytfB[b][:], r=[ytfB[b]], w=[ytb[b]], e='act')
        for half in range(2):
            for kc in range(8):
                k.mm(pmix[:, half * 512:(half + 1) * 512], ytb[b][:, kc, :], woutb[:, kc, half * 512:(half + 1) * 512], start=(kc == 0), stop=(kc == 7), r=[ytb[b], woutb], w=[pmix])
        k.stt(rbuf[b][:, :], htile[b][:, :], ALPHA, pmix[:, :], ALU.mult, ALU.add, r=[htile[b], pmix], w=[rbuf[b]])
        layer_norm(k, rbuf[b], h1[b], ln1g, ln1b, lntmp)
        k.dma(h1f_d[tsl, :], h1[b][:, :], r=[h1[b]])
        k.cp(h1bt[b][:, :], h1[b][:, :], r=[h1[b]], w=[h1bt[b]], e='act')
        k.dma(h1b_d[tsl, :], h1bt[b][:, :], r=[h1bt[b]])
        for kc in range(8):
            k.tr(pT[:, kc * 128:(kc + 1) * 128], h1[b][:, kc * 128:(kc + 1) * 128], ident[:], r=[h1[b], ident], w=[pT])
        k.cp(h1Tf[b][:], pT[:, :].rearrange("p (kc t) -> p kc t", t=128), r=[pT], w=[h1Tf[b]], e='act')
        for kc in range(8):
            k.mm(plg[:, :], h1Tf[b][:, kc, :], wrs[:, kc, :], start=(kc == 0), stop=(kc == 7), r=[h1Tf[b], wrs], w=[plg])
        S = sm
        k.tt(S['lgs'][:], plg[:], brb[:], ALU.add, r=[plg, brb], w=[S['lgs']])
        k.op('dve', lambda e: e.reduce_max(out=S['negm'][:], in_=S['lgs'][:, 0:4], axis=AX.X), r=[S['lgs']], w=[S['negm']])
        k.ts(S['negm'][:], S['negm'][:], -1.0, None, op0=ALU.mult, r=[S['negm']], w=[S['negm']])
        k.ts(S['oh'][:], S['lgs'][:, 0:4], S['negm'][:, 0:1], 0.0, op0=ALU.add, op1=ALU.is_ge, r=[S['lgs'], S['negm']], w=[S['oh']])
        k.act(S['e4'][:], S['lgs'][:, 0:4], AF.Exp, bias=S['negm'][:, 0:1], accum_out=S['s4'][:, 0:1], r=[S['lgs'], S['negm']], w=[S['e4'], S['s4']])
        k.op('dve', lambda e: e.reciprocal(out=S['pg'][:], in_=S['s4'][:]), r=[S['s4']], w=[S['pg']])
        k.ts(S['fs'][:], S['lgs'][:, 4:12], S['oh'][:, 0:1], None, op0=ALU.mult, r=[S['lgs'], S['oh']], w=[S['fs']])
        for g in range(1, 4):
            k.stt(S['fs'][:], S['lgs'][:, 4 + 8 * g:12 + 8 * g], S['oh'][:, g:g + 1], S['fs'][:], ALU.mult, ALU.add, r=[S['lgs'], S['oh'], S['fs']], w=[S['fs']])
        k.op('dve', lambda e: e.max(out=S['t8'][:], in_=S['fs'][:]), r=[S['fs']], w=[S['t8']])
        k.ts(S['sel'][:], S['fs'][:], S['t8'][:, 1:2], None, op0=ALU.is_ge, r=[S['fs'], S['t8']], w=[S['sel']])
        k.ts(S['negm1'][:], S['t8'][:, 0:1], -1.0, None, op0=ALU.mult, r=[S['t8']], w=[S['negm1']])
        k.act(S['ex'][:], S['fs'][:], AF.Exp, bias=S['negm1'][:, 0:1], r=[S['fs'], S['negm1']], w=[S['ex']])
        k.act(S['e2'][:], S['t8'][:, 1:2], AF.Exp, bias=S['negm1'][:, 0:1], r=[S['t8'], S['negm1']], w=[S['e2']])
        k.ts(S['e2'][:], S['e2'][:], 1.0, None, op0=ALU.add, r=[S['e2']], w=[S['e2']])
        k.op('dve', lambda e: e.reciprocal(out=S['coef'][:], in_=S['e2'][:]), r=[S['e2']], w=[S['coef']])
        k.tt(S['coef'][:], S['coef'][:], S['pg'][:], ALU.mult, r=[S['coef'], S['pg']], w=[S['coef']])
        k.tt(S['cf'][:], S['ex'][:], S['sel'][:], ALU.mult, r=[S['ex'], S['sel']], w=[S['cf']])
        k.ts(S['cf'][:], S['cf'][:], S['coef'][:, 0:1], None, op0=ALU.mult, r=[S['cf'], S['coef']], w=[S['cf']])
        for g in range(4):
            k.ts(cgate[i][:, 8 * g:8 * g + 8], S['cf'][:], S['oh'][:, g:g + 1], None, op0=ALU.mult, r=[S['cf'], S['oh']], w=[cgate[i]])
    k.pop()
    didx = [k.sb([128, 4], I32, name='didx%d' % i) for i in range(ntile)]
    gts = [k.sb([128, 2], name='gts%d' % i) for i in range(ntile)]
    k.push()
    runb = k.sb([128, 32]); k.op('pool', lambda e: e.memset(runb[:], 0.0), w=[runb])
    mm_ = k.sb([128, 32]); ranks = [k.sb([128, 32], name='rank%d' % i) for i in range(ntile)]
    ppre = k.ps([128, 512], name='ppre_pAT')
    for i in range(ntile):
        k.ts(mm_[:], cgate[i][:], 0.0, None, op0=ALU.is_gt, r=[cgate[i]], w=[mm_])
        k.mm(ppre[:, 0:32], CC['ut'][:], mm_[:], r=[CC['ut'], mm_], w=[ppre])
        k.mm(ppre[:, 32:64], CC['ones'][:], mm_[:], r=[CC['ones'], mm_], w=[ppre])
        k.tt(ranks[i][:], ppre[:, 0:32], runb[:], ALU.add, r=[ppre, runb], w=[ranks[i]])
        k.tt(runb[:], ppre[:, 32:64], runb[:], ALU.add, r=[ppre, runb], w=[runb])
    cmp1 = k.sb([128, 32, JT]); nb = k.sb([128, 32]); pend = k.sb([128, 32]); delta = k.sb([128, 32]); ones32 = k.sb([128, 32])
    k.op('pool', lambda e: e.memset(ones32[:], 1.0), w=[ones32])
    if J > 1:
        k.tt(cmp1[:], runb[:].unsqueeze(2).to_broadcast([128, 32, JT]), CC['thr'][:].rearrange("p (e j) -> p e j", j=JT), ALU.is_gt, r=[runb, CC['thr']], w=[cmp1])
        k.op('dve', lambda e: e.reduce_sum(out=nb[:], in_=cmp1[:], axis=AX.X), r=[cmp1], w=[nb])
    else:
        k.op('pool', lambda e: e.memset(nb[:], 0.0), w=[nb])
    k.op('dve', lambda e: e.tensor_tensor_scan(out=pend[:], data0=ones32[:], data1=nb[:], initial=0.0, op0=ALU.mult, op1=ALU.add), r=[ones32, nb], w=[pend])
    k.tt(delta[:], pend[:], nb[:], ALU.subtract, r=[pend, nb], w=[delta])
    k.ts(delta[:], delta[:], float(G), float(31 * G), op0=ALU.mult, op1=ALU.add, r=[delta], w=[delta])
    k.tt(delta[:], delta[:], CC['eiota'][:], ALU.subtract, r=[delta, CC['eiota']], w=[delta])
    cmp2 = k.sb([128, NOVA, 32]); bef = k.sb([128, NOVA]); widx = k.sb([128, NOVA], I32)
    k.tt(cmp2[:], pend[:].unsqueeze(1).to_broadcast([128, NOVA, 32]), CC['biota'][:].rearrange("p (b e) -> p b e", e=32), ALU.is_le, r=[pend, CC['biota']], w=[cmp2])
    k.op('dve', lambda e: e.reduce_sum(out=bef[:], in_=cmp2[:], axis=AX.X), r=[cmp2], w=[bef])
    k.ts(bef[:], bef[:], 31.0, 128.0, op0=ALU.min, op1=ALU.mult, r=[bef], w=[bef])
    k.ts(bef[:], bef[:], CC['iotap'][:, 0:1], None, op0=ALU.add, r=[bef, CC['iotap']], w=[bef])
    k.cp(widx[:], bef[:], r=[bef], w=[widx])
    selb = k.sb([128, 32])
    Dm = k.sb([128, 32]); eq = k.sb([128, 32]); dd = k.sb([128, 4]); df = k.sb([128, 4]); dneg = k.sb([128, 2])
    for i in range(ntile):
        k.ts(mm_[:], cgate[i][:], 0.0, None, op0=ALU.is_gt, r=[cgate[i]], w=[mm_])
        k.ts(selb[:], ranks[i][:], float(G), None, op0=ALU.is_ge, r=[ranks[i]], w=[selb])
        k.tt(selb[:], selb[:], delta[:], ALU.mult, r=[selb, delta], w=[selb])
        k.tt(Dm[:], ranks[i][:], CC['eiota'][:], ALU.add, r=[ranks[i], CC['eiota']], w=[Dm])
        k.tt(Dm[:], Dm[:], selb[:], ALU.add, r=[Dm, selb], w=[Dm])
        k.stt(Dm[:], Dm[:], 1.0, mm_[:], ALU.add, ALU.mult, r=[Dm, mm_], w=[Dm])
        k.op('dve', lambda e: e.reduce_max(out=dd[:, 0:1], in_=Dm[:], axis=AX.X), r=[Dm], w=[dd])
        k.op('dve', lambda e: e.reduce_sum(out=dd[:, 1:2], in_=Dm[:], axis=AX.X), r=[Dm], w=[dd])
        k.ts(df[:, 0:1], dd[:, 0:1], -1.0, None, op0=ALU.add, r=[dd], w=[df])
        k.stt(df[:, 1:2], dd[:, 1:2], -1.0, dd[:, 0:1], ALU.add, ALU.subtract, r=[dd], w=[df])
        k.ts(dneg[:], df[:, 0:2], 0.0, None, op0=ALU.is_lt, r=[df], w=[dneg])
        k.ts(df[:, 2:4], df[:, 0:2], 0.0, None, op0=ALU.max, r=[df], w=[df])
        k.stt(df[:, 0:2], dneg[:], float(NROWS + 1), df[:, 0:2], ALU.mult, ALU.add, r=[dneg, df], w=[df])
        k.cp(didx[i][:], df[:], r=[df], w=[didx[i]])
        k.ts(eq[:], Dm[:], dd[:, 0:1], None, op0=ALU.is_equal, r=[Dm, dd], w=[eq])
        k.tt(eq[:], eq[:], cgate[i][:], ALU.mult, r=[eq, cgate[i]], w=[eq])
        k.op('dve', lambda e: e.reduce_sum(out=gts[i][:, 0:1], in_=eq[:], axis=AX.X), r=[eq], w=[gts[i]])
        k.op('dve', lambda e: e.reduce_sum(out=dd[:, 2:3], in_=cgate[i][:], axis=AX.X), r=[cgate[i]], w=[dd])
        k.tt(gts[i][:, 1:2], dd[:, 2:3], gts[i][:, 0:1], ALU.subtract, r=[dd, gts[i]], w=[gts[i]])
    hbt = [k.sb([128, D], BF16) for _ in range(2)]
    XS = Buf(None, 'xs')
    for i in range(ntile):
        b = i % 2
        k.dma(hbt[b][:], h1b_d[i * 128:(i + 1) * 128, :], w=[hbt[b]])
        k.ind(xs_d, bass.IndirectOffsetOnAxis(ap=didx[i][:, 0:1], axis=0), hbt[b][:], None, r=[didx[i], hbt[b]], bounds=NROWS)
        k.ind(xs_d, bass.IndirectOffsetOnAxis(ap=didx[i][:, 1:2], axis=0), hbt[b][:], None, r=[didx[i], hbt[b]], bounds=NROWS)
    if DEBUG:
        dbg_cg = nc.dram_tensor("dbg_cg", [NT, 32], F32, kind="ExternalOutput").ap()
        dbg_di = nc.dram_tensor("dbg_di", [NT, 2], I32, kind="ExternalOutput").ap()
        dbg_gt = nc.dram_tensor("dbg_gt", [NT, 2], F32, kind="ExternalOutput").ap()
        dbg_wi = nc.dram_tensor("dbg_wi", [128, NOVA], I32, kind="ExternalOutput").ap()
        dbg_rk = nc.dram_tensor("dbg_rk", [NT, 32], F32, kind="ExternalOutput").ap()
        for i in range(ntile):
            k.dma(dbg_cg[i * 128:(i + 1) * 128, :], cgate[i][:], r=[cgate[i]])
            k.dma(dbg_di[i * 128:(i + 1) * 128, :], didx[i][:, 0:2], r=[didx[i]])
            k.dma(dbg_gt[i * 128:(i + 1) * 128, :], gts[i][:], r=[gts[i]])
            k.dma(dbg_rk[i * 128:(i + 1) * 128, :], ranks[i][:], r=[ranks[i]])
        k.dma(dbg_wi, widx[:], r=[widx])
    k.barrier()
    w1s = [k.sb([128, 4096])] * 2; w3s = [k.sb([128, 4096])] * 2; w2s = [k.sb([128, 4096])] * 2
    w1b = [k.sb([128, 4096], BF16) for _ in range(2)]; w3b = [k.sb([128, 4096], BF16) for _ in range(2)]; w2b = [k.sb([128, 4096], BF16) for _ in range(2)]
    xt = [k.sb([128, NSUB, D], BF16) for _ in range(2)]
    XT = [k.sb([128, 8, 128], BF16) for _ in range(2)]
    sl = [k.sb([128, 512]) for _ in range(2)]; actb = [k.sb([128, 512], BF16) for _ in range(2)]
    actT = [k.sb([128, 4, 128], BF16) for _ in range(2)]
    yrow = [k.sb([128, D], BF16) for _ in range(2)]
    pXT = k.ps([128, D], BF16); ph1 = k.ps([128, 512]); ph3 = k.ps([128, 512]); pATv = ppre[:, :].bitcast(BF16)
    py = [k.ps([128, 512]) for _ in range(2)]
    def load_blk(bk):
        sb_ = bk % 2
        if bk < 32:
            k.dma(w1s[sb_][:], w1r[bk * 128:(bk + 1) * 128, :], w=[w1s[sb_]])
            k.dma(w3s[sb_][:], w3r[bk * 128:(bk + 1) * 128, :], w=[w3s[sb_]], q='act')
            k.dma(w2s[sb_][:], w2r[bk * 128:(bk + 1) * 128, :], w=[w2s[sb_]])
        else:
            off = bass.IndirectOffsetOnAxis(ap=widx[:, bk - 32:bk - 31], axis=0)
            k.ind(w1s[sb_][:], None, w1r, off, r=[widx], w=[w1s[sb_]], bounds=4095)
            k.ind(w3s[sb_][:], None, w3r, off, r=[widx], w=[w3s[sb_]], bounds=4095)
            k.ind(w2s[sb_][:], None, w2r, off, r=[widx], w=[w2s[sb_]], bounds=4095)
        k.dma(xt[sb_][:], xs_d[bk * G:(bk + 1) * G, :].rearrange("(s p) d -> p s d", p=128), w=[xt[sb_]])
    ph1b = [ph1, k.ps([128, 512])]; ph3b = [ph3, k.ps([128, 512])]
    U = NBLK * NSUB
    def casts(bk):
        sb_ = bk % 2
        k.cp(w1b[sb_][:], w1s[sb_][:], r=[w1s[sb_]], w=[w1b[sb_]], e='act')
        k.cp(w3b[sb_][:], w3s[sb_][:], r=[w3s[sb_]], w=[w3b[sb_]], e='dve')
        k.cp(w2b[sb_][:, 0:2048], w2s[sb_][:, 0:2048], r=[w2s[sb_]], w=[w2b[sb_]], e='act')
        k.cp(w2b[sb_][:, 2048:4096], w2s[sb_][:, 2048:4096], r=[w2s[sb_]], w=[w2b[sb_]], e='dve')
        if bk + 1 < NBLK:
            load_blk(bk + 1)
    def S1(u):
        bk, s_ = divmod(u, NSUB); q = u % 2
        for kc in range(8):
            k.tr(pXT[:, kc * 128:(kc + 1) * 128], xt[bk % 2][:, s_, kc * 128:(kc + 1) * 128], identb[:], r=[xt[bk % 2], identb], w=[pXT])
        k.cp(XT[q][:], pXT[:, :].rearrange("p (kc t) -> p kc t", t=128), r=[pXT], w=[XT[q]], e='act')
    def S2(u):
        bk, s_ = divmod(u, NSUB); q = u % 2; sb_ = bk % 2
        if s_ == 0:
            casts(bk)
        for kc in range(8):
            k.mm(ph1b[q][:, :], XT[q][:, kc, :], w1b[sb_][:, kc * 512:(kc + 1) * 512], start=(kc == 0), stop=(kc == 7), r=[XT[q], w1b[sb_]], w=[ph1b[q]])
        for kc in range(8):
            k.mm(ph3b[q][:, :], XT[q][:, kc, :], w3b[sb_][:, kc * 512:(kc + 1) * 512], start=(kc == 0), stop=(kc == 7), r=[XT[q], w3b[sb_]], w=[ph3b[q]])
        k.act(sl[q][:, :], ph1b[q][:, :], AF.Silu, r=[ph1b[q]], w=[sl[q]])
        k.tt(actb[q][:, :], sl[q][:, :], ph3b[q][:, :], ALU.mult, r=[sl[q], ph3b[q]], w=[actb[q]])
    def S3a(u):
        q = u % 2
        for hc in range(4):
            k.tr(pATv[:, hc * 128:(hc + 1) * 128], actb[q][:, hc * 128:(hc + 1) * 128], identb[:], r=[actb[q], identb], w=[ppre])
        k.cp(actT[q][:], pATv[:, 0:512].rearrange("p (hc t) -> p hc t", t=128), r=[ppre], w=[actT[q]], e='dve')
    def S3b(u):
        bk, s_ = divmod(u, NSUB); q = u % 2; sb_ = bk % 2
        for half in range(2):
            for hc in range(4):
                k.mm(py[half][:, :], actT[q][:, hc, :], w2b[sb_][:, hc * 1024 + half * 512:hc * 1024 + (half + 1) * 512], start=(hc == 0), stop=(hc == 3), r=[actT[q], w2b[sb_]], w=[py[half]])
        k.cp(yrow[q][:, 0:512], py[0][:, :], r=[py[0]], w=[yrow[q]], e='act')
        k.cp(yrow[q][:, 512:1024], py[1][:, :], r=[py[1]], w=[yrow[q]], e='dve')
        r0 = bk * G + s_ * 128
        k.dma(yrows_d[r0:r0 + 128, :], yrow[q][:, :], r=[yrow[q]])
    load_blk(0)
    S1(0)
    for step in range(U + 1):
        if step - 1 >= 0:
            S3a(step - 1)
        if step + 1 < U:
            S1(step + 1)
        if step < U:
            S2(step)
        if step - 1 >= 0:
            S3b(step - 1)
    k.pop()
    ob = [k.sb([128, D]) for _ in range(2)]
    hT_out = over.get('hT_dst')
    if hT_out is not None:
        pT3 = k.ps([128, D]); obT = [k.sb([128, 8, 128], BF16) for _ in range(2)]
    NB3 = 2
    hf = [k.sb([128, D]) for _ in range(NB3)]; rhi = [k.sb([128, D], BF16) for _ in range(NB3)]; rlo = [k.sb([128, D], BF16) for _ in range(NB3)]
    YR = Buf(None, 'yrows')
    for i in range(ntile):
        b = i % NB3
        k.dma(hf[b][:], h1f_d[i * 128:(i + 1) * 128, :], w=[hf[b]])
        k.ind(rhi[b][:], None, yrows_d, bass.IndirectOffsetOnAxis(ap=didx[i][:, 2:3], axis=0), r=[didx[i]], w=[rhi[b]], bounds=NROWS - 1)
        k.ind(rlo[b][:], None, yrows_d, bass.IndirectOffsetOnAxis(ap=didx[i][:, 3:4], axis=0), r=[didx[i]], w=[rlo[b]], bounds=NROWS - 1)
        if DEBUG:
            if i == 0:
                dbg_rhi = nc.dram_tensor("dbg_rhi", [NT, D], BF16, kind="ExternalOutput").ap(); dbg_pre = nc.dram_tensor("dbg_pre", [NT, D], F32, kind="ExternalOutput").ap()
            k.dma(dbg_rhi[i * 128:(i + 1) * 128, :], rhi[b][:, :], r=[rhi[b]])
        k.act(hf[b][:, :], hf[b][:, :], AF.Copy, scale=ALPHA, r=[hf[b]], w=[hf[b]])
        k.stt(hf[b][:, :], rhi[b][:, :], gts[i][:, 0:1], hf[b][:, :], ALU.mult, ALU.add, r=[rhi[b], gts[i], hf[b]], w=[hf[b]])
        k.stt(hf[b][:, :], rlo[b][:, :], gts[i][:, 1:2], hf[b][:, :], ALU.mult, ALU.add, r=[rlo[b], gts[i], hf[b]], w=[hf[b]])
        if DEBUG:
            k.dma(dbg_pre[i * 128:(i + 1) * 128, :], hf[b][:, :], r=[hf[b]])
        layer_norm(k, hf[b], ob[i % 2], ln2g, ln2b, lntmp)
        k.dma(out[i * 128:(i + 1) * 128, :], ob[i % 2][:, :], r=[ob[i % 2]])
        if hT_out is not None:
            for kc in range(8):
                k.tr(pT3[:, kc * 128:(kc + 1) * 128], ob[i % 2][:, kc * 128:(kc + 1) * 128], ident[:], r=[ob[i % 2], ident], w=[pT3])
            k.cp(obT[i % 2][:], pT3[:, :].rearrange("p (kc t) -> p kc t", t=128), r=[pT3], w=[obT[i % 2]], e='act')
            k.dma(hT_out(i), obT[i % 2][:], r=[obT[i % 2]])
            over['after_tile'](i)
    if own:
        k.finish()
        k.close()
        return nc
    k.pop()


TB = 512
NCH = 8
GN_EPS = 64e-5
NORM_EPS = 1e-5

def consts0():
    c = {}
    c["ident"] = np.eye(128, dtype=np.float32)
    bo = np.zeros((128, 128), np.float32); bo[:64, :64] = 1; bo[64:, 64:] = 1
    c["blockones"] = bo
    c["ones"] = np.ones((128, 128), np.float32)
    p = np.arange(128)[:, None] % 64; q = np.arange(512)[None, :] % 64
    c["m_us"] = (q > p).astype(np.float32)
    c["m_ui"] = (q >= p).astype(np.float32)
    c["m_ls"] = (p > q).astype(np.float32)
    c["ident4"] = np.tile(np.eye(128, dtype=np.float32), (1, 4))
    hm = np.zeros((128, 4), np.float32); hm[:64, 0] = 1; hm[64:, 1] = 1; hm[:64, 2] = -1; hm[64:, 3] = -1
    c["hm"] = hm
    rm = np.ones((128, 512), np.float32); rm[:, ::64] = 0
    c["resetm"] = rm
    return c

def build_mix0(L, ctx=None):
    own = ctx is None
    nc = bass.Bass("TRN2", target_bir_lowering=False) if own else ctx['nc']
    pfx = '' if own else ctx['pfx']
    over = {} if own else ctx['over']
    nblk = L // TB
    def din(name, shape):
        if name in over:
            return over[name]
        return nc.dram_tensor(pfx + name, shape, F32, kind="ExternalInput").ap()
    hT = din("hT", [1024, L])
    w_rw = din("w_rw", [1024, 640]); w_gla = din("w_gla", [1024, 400])
    rwvec = din("rwvec", [128, 16])
    lora_wa = din("lora_wa", [128, 128]); g2c = din("g2c", [128, 128])
    gk_w2 = din("gk_w2", [16, 64]); glavec = din("glavec", [128, 2])
    cd = {n: din(n, list(v.shape)) for n, v in consts0().items()}
    y_rw = None if "y_dst" in over else nc.dram_tensor("y_rw", [128, L], F32, kind="ExternalOutput").ap()
    y_gla = None if "y_dst" in over else nc.dram_tensor("y_gla", [128, L], F32, kind="ExternalOutput").ap()
    k = KB(nc) if own else ctx['k']
    if not own:
        k.push()
    C = {}
    for n, v in consts0().items():
        C[n] = k.sb(list(v.shape), name='c_' + n); k.dma(C[n][:], cd[n], w=[C[n]])
    vec = k.sb([128, 16]); k.dma(vec[:], rwvec, w=[vec])
    MU = lambda i: vec[:, i:i + 1]
    W0, A0, KK, KA, RK, GNG, GNB = [vec[:, 5 + i:6 + i] for i in range(7)]
    vx = k.sb([128, 4])
    k.ts(vx[:, 0:1], KA, -1.0, 1.0, op0=ALU.mult, op1=ALU.add, r=[vec], w=[vx])
    k.op('pool', lambda e: e.memset(vx[:, 1:2], GN_EPS), w=[vx])
    k.op('pool', lambda e: e.memset(vx[:, 2:3], NORM_EPS), w=[vx])
    gv = k.sb([128, 2]); k.dma(gv[:], glavec, w=[gv])
    k.ts(vx[:, 3:4], gv[:, 0:1], -1.0, None, op0=ALU.mult, r=[gv, vx], w=[vx])
    lwa = k.sb([128, 128]); k.dma(lwa[:], lora_wa, w=[lwa])
    g2s = k.sb([128, 128]); k.dma(g2s[:], g2c, w=[g2s])
    gkw = k.sb([16, 64]); k.dma(gkw[:], gk_w2, w=[gkw])
    wrwb = k.sb([128, 8, 640], BF16); k.dma(wrwb[:], w_rw.rearrange("(kc p) n -> p kc n", p=128), w=[wrwb], q='pool')
    wglb = k.sb([128, 8, 400], BF16); k.dma(wglb[:], w_gla.rearrange("(kc p) n -> p kc n", p=128), w=[wglb], q='pool')
    hTb = [k.sb([128, 8, TB], BF16) for _ in range(2)]
    hT3 = hT.rearrange("(kc p) t -> p kc t", p=128)
    pp = k.ps([128, 512]); pA = k.ps([128, 512])
    pM = k.ps([128, 512]); pN = k.ps([128, 512]); pP = k.ps([128, 512])
    pWUS = k.ps([128, 512]); pY = k.ps([128, 512]); pO = k.ps([128, 512])
    T = lambda name: k.sb([128, TB], name=name)
    psh = [k.sb([128, TB + 1], name='psh%d' % i) for i in range(5)]
    for b_ in psh:
        k.op('pool', lambda e: e.memset(b_[:, 0:1], 0.0), w=[b_])
    xs = [T('xs%d' % i) for i in range(5)]
    tmp = T('tmp'); tmp2 = T('tmp2')
    ld = T('ld'); a_sb = T('a_sb'); g_sb = T('g_sb'); kkn = T('kkn'); k2 = T('k2'); bvec = T('bvec'); bonus = T('bonus')
    bcum = T('bcum'); eb = T('eb'); enb = T('enb'); ebx = T('ebx')
    BD = lambda name: k.sb([128, NCH, 2, 64], name=name)
    r_bd, k_bd, b_bd, a_bd, kd_bd, bd_bd, v_bd = [BD(n) for n in ['r_bd', 'k_bd', 'b_bd', 'a_bd', 'kd_bd', 'bd_bd', 'v_bd']]
    AM = lambda name: k.sb([128, NCH, 128], name=name)
    AakT, ArbT, ArkT, Vtok, Kdt, Bdt = [AM(n) for n in ['AakT', 'ArbT', 'ArkT', 'Vtok', 'Kdt', 'Bdt']]
    NtA, MA, Pfin = [k.sb([128, NCH, 128], BF16, name=n) for n in ['NtA', 'MA', 'Pfin']]
    Ntb = [k.sb([128, 4, 128], BF16, name='Ntb%d' % i) for i in range(4)]
    Mb = [k.sb([128, 4, 128], BF16, name='Mb%d' % i) for i in range(4)]
    Pb = [k.sb([128, 4, 128], BF16, name='Pb%d' % i) for i in range(4)]
    ident4b = k.sb([128, 512], BF16, name='ident4b'); k.cp(ident4b[:], C['ident4'][:], r=[C['ident4']], w=[ident4b])
    Wsb = k.sb([128, 128], BF16, name='Wsb'); Usb = k.sb([128, 128], name='Usb')
    Sbd = [k.sb([128, 128], name='Sbd%d' % i) for i in range(2)]
    k.op('pool', lambda e: e.memset(Sbd[0][:], 0.0), w=[Sbd[0]])
    yT = T('yT'); yo = T('yo')
    gq = k.sb([64, TB], name='gq'); gk = k.sb([64, TB], name='gk'); ggate = T('ggate'); gkl = k.sb([16, TB], name='gkl')
    gvt = k.sb([64, NCH, 128], name='gvt')
    gsp = k.sb([64, TB], name='gsp'); gb = k.sb([64, TB], name='gb'); geb = k.sb([64, TB], name='geb'); genb = k.sb([64, TB], name='genb')
    gqt = k.sb([64, TB], name='gqt'); gkt = k.sb([64, TB], name='gkt'); gkd = k.sb([64, TB], name='gkd')
    gAT = k.sb([64, NCH, 64], name='gAT'); gkdt = k.sb([64, NCH, 64], name='gkdt')
    gS = [k.sb([64, 128], name='gS%d' % i) for i in range(2)]
    k.op('pool', lambda e: e.memset(gS[0][:], 0.0), w=[gS[0]])
    goT = yT; gsq = tmp2; gout = tmp
    sidx = 0; gsidx = 0
    c3 = lambda buf: buf[:, :].rearrange("p (c t) -> p c t", t=64)
    for blk in range(nblk):
        hb = hTb[blk % 2]
        cs = slice(blk * TB, (blk + 1) * TB)
        k.dma(hb[:], hT3[:, :, cs], w=[hb], q='pool')
        for ct in range(5):
            for kc in range(8):
                k.mm(pp[:, :], wrwb[:, kc, ct * 128:(ct + 1) * 128], hb[:, kc, :], start=(kc == 0), stop=(kc == 7), r=[wrwb, hb], w=[pp])
            k.cp(psh[ct][:, 1:TB + 1], pp[:, :], r=[pp], w=[psh[ct]], e='act')
            k.tt(tmp[:, :], psh[ct][:, 0:TB], psh[ct][:, 1:TB + 1], ALU.subtract, r=[psh[ct]], w=[tmp])
            k.stt(xs[ct][:, :], tmp[:, :], MU(ct), psh[ct][:, 1:TB + 1], ALU.mult, ALU.add, r=[tmp, psh[ct], vec], w=[xs[ct]])
            k.cp(psh[ct][:, 0:1], psh[ct][:, TB:TB + 1], r=[psh[ct]], w=[psh[ct]], e='pool')
        xr, xk, xv, xwa, xgl = xs
        k.act(xwa[0:64, :], xwa[0:64, :], AF.Tanh, r=[xwa], w=[xwa])
        k.mm(pp[:, :], lwa[0:64, :], xwa[0:64, :], r=[lwa, xwa], w=[pp])
        k.act(tmp[:, :], pp[:, :], AF.Sigmoid, bias=W0, r=[pp, vec], w=[tmp])
        k.ts(ld[:, :], tmp[:, :], -0.6065306597126334, None, op0=ALU.mult, r=[tmp], w=[ld])
        k.mm(pA[:, :], lwa[64:128, :], xwa[64:128, :], r=[lwa, xwa], w=[pA])
        k.act(a_sb[:, :], pA[:, :], AF.Sigmoid, bias=A0, r=[pA, vec], w=[a_sb])
        k.act(tmp[:, :], xgl[:, :], AF.Sigmoid, r=[xgl], w=[tmp])
        k.mm(pp[:, :], g2s[:, :], tmp[:, :], r=[g2s, tmp], w=[pp])
        k.cp(g_sb[:, :], pp[:, :], r=[pp], w=[g_sb], e='act')
        k.ts(kkn[:, :], xk[:, :], KK, None, op0=ALU.mult, r=[xk, vec], w=[kkn])
        k.tt(tmp[:, :], kkn[:, :], kkn[:, :], ALU.mult, r=[kkn], w=[tmp])
        k.mm(pA[:, :], C['blockones'][:, :], tmp[:, :], r=[C['blockones'], tmp], w=[pA])
        k.act(tmp2[:, :], pA[:, :], AF.Sqrt, r=[pA], w=[tmp2])
        k.ts(tmp2[:, :], tmp2[:, :], 1e-12, None, op0=ALU.max, r=[tmp2], w=[tmp2])
        k.op('dve', lambda e: e.reciprocal(out=tmp2[:, :], in_=tmp2[:, :]), r=[tmp2], w=[tmp2])
        k.tt(kkn[:, :], kkn[:, :], tmp2[:, :], ALU.mult, r=[kkn, tmp2], w=[kkn])
        k.ts(tmp[:, :], a_sb[:, :], KA, vx[:, 0:1], op0=ALU.mult, op1=ALU.add, r=[a_sb, vec, vx], w=[tmp])
        k.tt(k2[:, :], xk[:, :], tmp[:, :], ALU.mult, r=[xk, tmp], w=[k2])
        k.tt(bvec[:, :], kkn[:, :], a_sb[:, :], ALU.mult, r=[kkn, a_sb], w=[bvec])
        k.stt(tmp[:, :], xr[:, :], RK, k2[:, :], ALU.mult, ALU.mult, r=[xr, vec, k2], w=[tmp])
        k.mm(pp[:, :], C['blockones'][:, :], tmp[:, :], r=[C['blockones'], tmp], w=[pp])
        k.tt(bonus[:, :], pp[:, :], xv[:, :], ALU.mult, r=[pp, xv], w=[bonus])
        k.op('dve', lambda e: e.tensor_tensor_scan(out=bcum[:, :], data0=C['resetm'][:, :], data1=ld[:, :], initial=0.0, op0=ALU.mult, op1=ALU.add), r=[C['resetm'], ld], w=[bcum])
        k.act(eb[:, :], bcum[:, :], AF.Exp, r=[bcum], w=[eb])
        k.act(enb[:, :], bcum[:, :], AF.Exp, scale=-1.0, r=[bcum], w=[enb])
        k.tt(tmp[:, :], bcum[:, :], ld[:, :], ALU.subtract, r=[bcum, ld], w=[tmp])
        k.act(ebx[:, :], tmp[:, :], AF.Exp, r=[tmp], w=[ebx])
        gC = c3(eb)[:, :, 63:64].to_broadcast([128, NCH, 64])
        for h in range(2):
            hm = C['hm'][:, h:h + 1]; hmn = C['hm'][:, 2 + h:3 + h]
            k.stt(r_bd[:, :, h, :], c3(xr), hm, c3(eb), ALU.mult, ALU.mult, r=[xr, eb, C['hm']], w=[r_bd])
            k.stt(k_bd[:, :, h, :], c3(k2), hm, c3(enb), ALU.mult, ALU.mult, r=[k2, enb, C['hm']], w=[k_bd])
            k.stt(b_bd[:, :, h, :], c3(bvec), hm, c3(enb), ALU.mult, ALU.mult, r=[bvec, enb, C['hm']], w=[b_bd])
            k.stt(a_bd[:, :, h, :], c3(kkn), hmn, c3(ebx), ALU.mult, ALU.mult, r=[kkn, ebx, C['hm']], w=[a_bd])
            k.tt(kd_bd[:, :, h, :], k_bd[:, :, h, :], gC, ALU.mult, r=[k_bd, eb], w=[kd_bd])
            k.tt(bd_bd[:, :, h, :], b_bd[:, :, h, :], gC, ALU.mult, r=[b_bd, eb], w=[bd_bd])
            k.ts(v_bd[:, :, h, :], c3(xv), hm, None, op0=ALU.mult, r=[xv, C['hm']], w=[v_bd], e='pool')
        f2 = lambda bd, c: bd[:, c, :, :].rearrange("p h t -> p (h t)")
        def amat(dst, lbd, rbd, mask):
            for g4 in range(2):
                for cc in range(4):
                    c = g4 * 4 + cc
                    k.mm(pA[:, cc * 128:(cc + 1) * 128], f2(lbd, c), f2(rbd, c), r=[lbd, rbd], w=[pA])
                if mask is None:
                    k.cp(dst[:, g4 * 4:(g4 + 1) * 4, :], pA[:, :].rearrange("p (c t) -> p c t", t=128), r=[pA], w=[dst], e='act')
                else:
                    k.tt(dst[:, g4 * 4:(g4 + 1) * 4, :], pA[:, :].rearrange("p (c t) -> p c t", t=128), mask[:, :].rearrange("p (c t) -> p c t", t=128), ALU.mult, r=[pA, mask], w=[dst])
        amat(NtA, b_bd, a_bd, C['m_us'])
        amat(MA, a_bd, b_bd, C['m_ls'])
        amat(AakT, k_bd, a_bd, C['m_us'])
        amat(ArbT, b_bd, r_bd, C['m_ui'])
        amat(ArkT, k_bd, r_bd, C['m_ui'])
        def tmat(dst, src):
            for g4 in range(2):
                for cc in range(4):
                    c = g4 * 4 + cc
                    k.tr(pA[:, cc * 128:(cc + 1) * 128], f2(src, c), C['ident'][:, :], r=[src, C['ident']], w=[pA])
                k.cp(dst[:, g4 * 4:(g4 + 1) * 4, :], pA[:, :].rearrange("p (c t) -> p c t", t=128), r=[pA], w=[dst], e='act')
        tmat(Vtok, v_bd); tmat(Kdt, kd_bd); tmat(Bdt, bd_bd)
        def A3(buf):
            return buf[:, :, :] if len(buf.t.shape) == 3 else buf[:, :].rearrange("p (c t) -> p c t", t=128)
        sets = [dict(pM=pM, pN=pN, pP=pP, N=Ntb[0:2], M=Mb[0:2], P=Pb[0:2]),
                dict(pM=pp, pN=pA, pP=pO, N=Ntb[2:4], M=Mb[2:4], P=Pb[2:4])]
        stt_ = []
        i4 = ident4b[:, :].rearrange("p (c t) -> p c t", t=128)
        for g4 in range(2):
            S_ = sets[g4]; gs = slice(g4 * 4, (g4 + 1) * 4)
            k.tt(A3(S_['P'][0]), NtA[:, gs, :], i4, ALU.add, r=[NtA, ident4b], w=[S_['P'][0]])
            stt_.append(dict(N=None, M=None, pi=0))
        def Nsl(g4, c):
            s_ = stt_[g4]
            return (NtA[:, g4 * 4 + c, :], NtA) if s_['N'] is None else (A3(s_['N'])[:, c, :], s_['N'])
        def Msl(g4, c):
            s_ = stt_[g4]
            return (MA[:, g4 * 4 + c, :], MA) if s_['M'] is None else (A3(s_['M'])[:, c, :], s_['M'])
        for lvl in range(1, 6):
            for g4 in range(2):
                S_ = sets[g4]
                for c in range(4):
                    (na, nb_), (ma, mb_) = Nsl(g4, c), Msl(g4, c)
                    k.mm(S_['pM'][:, c * 128:(c + 1) * 128], na, ma, r=[nb_, mb_], w=[S_['pM']])
                if lvl < 5:
                    for c in range(4):
                        (na, nb_), (ma, mb_) = Nsl(g4, c), Msl(g4, c)
                        k.mm(S_['pN'][:, c * 128:(c + 1) * 128], ma, na, r=[nb_, mb_], w=[S_['pN']])
            for g4 in range(2):
                S_ = sets[g4]
                Mn = S_['M'][lvl % 2]; Nn = S_['N'][lvl % 2]
                k.cp(A3(Mn), S_['pM'][:, :].rearrange("p (c t) -> p c t", t=128), r=[S_['pM']], w=[Mn], e='act')
                if lvl < 5:
                    k.cp(A3(Nn), S_['pN'][:, :].rearrange("p (c t) -> p c t", t=128), r=[S_['pN']], w=[Nn], e='act')
            for g4 in range(2):
                S_ = sets[g4]; Mn = S_['M'][lvl % 2]; Pc = S_['P'][stt_[g4]['pi']]
                for c in range(4):
                    k.mm(S_['pP'][:, c * 128:(c + 1) * 128], A3(Mn)[:, c, :], A3(Pc)[:, c, :], r=[Mn, Pc], w=[S_['pP']])
            for g4 in range(2):
                S_ = sets[g4]; s_ = stt_[g4]; gs = slice(g4 * 4, (g4 + 1) * 4)
                Pc = S_['P'][s_['pi']]
                if lvl < 5:
                    Pn = S_['P'][1 - s_['pi']]
                    k.tt(A3(Pn), S_['pP'][:, :].rearrange("p (c t) -> p c t", t=128), A3(Pc), ALU.add, r=[S_['pP'], Pc], w=[Pn])
                    s_['pi'] = 1 - s_['pi']
                else:
                    k.tt(Pfin[:, gs, :], S_['pP'][:, :].rearrange("p (c t) -> p c t", t=128), A3(Pc), ALU.add, r=[S_['pP'], Pc], w=[Pfin])
                s_['M'] = S_['M'][lvl % 2]
                if lvl < 5:
                    s_['N'] = S_['N'][lvl % 2]
        for c in range(NCH):
            S = Sbd[sidx % 2]; Sn = Sbd[(sidx + 1) % 2]; sidx += 1
            k.mm(pWUS[:, 0:128], AakT[:, c, :], Vtok[:, c, :], start=True, stop=False, r=[AakT, Vtok], w=[pWUS])
            k.mm(pWUS[:, 0:128], f2(a_bd, c), S[:, :], start=False, stop=True, r=[a_bd, S], w=[pWUS])
            k.cp(Wsb[:, :], pWUS[:, 0:128], r=[pWUS], w=[Wsb], e='act')
            k.mm(pWUS[:, 128:256], Pfin[:, c, :], Wsb[:, :], r=[Pfin, Wsb], w=[pWUS])
            k.cp(Usb[:, :], pWUS[:, 128:256], r=[pWUS], w=[Usb], e='dve')
            cc = c % 4
            k.mm(pY[:, cc * 128:(cc + 1) * 128], Vtok[:, c, :], ArkT[:, c, :], start=True, stop=False, r=[Vtok, ArkT], w=[pY])
            k.mm(pY[:, cc * 128:(cc + 1) * 128], S[:, :], f2(r_bd, c), start=False, stop=False, r=[S, r_bd], w=[pY])
            k.mm(pY[:, cc * 128:(cc + 1) * 128], Usb[:, :], ArbT[:, c, :], start=False, stop=True, r=[Usb, ArbT], w=[pY])
            k.mm(pWUS[:, 256:384], Kdt[:, c, :], Vtok[:, c, :], start=True, stop=False, r=[Kdt, Vtok], w=[pWUS])
            k.mm(pWUS[:, 256:384], Bdt[:, c, :], Usb[:, :], start=False, stop=True, r=[Bdt, Usb], w=[pWUS])
            k.stt(Sn[:, :], S[:, :], eb[:, c * 64 + 63:c * 64 + 64], pWUS[:, 256:384], ALU.mult, ALU.add, r=[S, eb, pWUS], w=[Sn])
            if cc == 3:
                g4 = c // 4
                pv = pY[:, :].rearrange("p (c h t) -> p c h t", h=2, t=64)
                k.cp(yT[0:64, g4 * 256:(g4 + 1) * 256].rearrange("p (c t) -> p c t", t=64), pv[0:64, :, 0, :], r=[pY], w=[yT], e='act')
                k.cp(yT[64:128, g4 * 256:(g4 + 1) * 256].rearrange("p (c t) -> p c t", t=64), pv[64:128, :, 1, :], r=[pY], w=[yT], e='act')
        k.mm(pp[:, :], C['blockones'][:, :], yT[:, :], r=[C['blockones'], yT], w=[pp])
        k.stt(tmp[:, :], pp[:, :], -1.0 / 64, yT[:, :], ALU.mult, ALU.add, r=[pp, yT], w=[tmp])
        k.tt(tmp2[:, :], tmp[:, :], tmp[:, :], ALU.mult, r=[tmp], w=[tmp2])
        k.mm(pp[:, :], C['blockones'][:, :], tmp2[:, :], r=[C['blockones'], tmp2], w=[pp])
        k.act(tmp2[:, :], pp[:, :], AF.Sqrt, bias=vx[:, 1:2], scale=1.0 / 64, r=[pp, vx], w=[tmp2])
        k.op('dve', lambda e: e.reciprocal(out=tmp2[:, :], in_=tmp2[:, :]), r=[tmp2], w=[tmp2])
        k.tt(tmp[:, :], tmp[:, :], tmp2[:, :], ALU.mult, r=[tmp, tmp2], w=[tmp])
        k.ts(tmp[:, :], tmp[:, :], GNG, GNB, op0=ALU.mult, op1=ALU.add, r=[tmp, vec], w=[tmp])
        k.tt(tmp[:, :], tmp[:, :], bonus[:, :], ALU.add, r=[tmp, bonus], w=[tmp])
        k.tt(yo[:, :], tmp[:, :], g_sb[:, :], ALU.mult, r=[tmp, g_sb], w=[yo])
        k.dma(over['y_dst'](0, blk) if 'y_dst' in over else y_rw[:, cs], yo[:, :], r=[yo])
        for (dst, c0, c1) in [(gq, 0, 64), (gk, 64, 128), (ggate, 256, 384), (gkl, 384, 400)]:
            m = c1 - c0
            for kc in range(8):
                k.mm(pp[0:m, :], wglb[:, kc, c0:c1], hb[:, kc, :], start=(kc == 0), stop=(kc == 7), r=[wglb, hb], w=[pp])
            k.cp(dst[0:m, :], pp[0:m, :], r=[pp], w=[dst], e='act')
        for g4 in range(2):
            for cc in range(4):
                c = g4 * 4 + cc
                for kc in range(8):
                    k.mm(pA[0:64, cc * 128:(cc + 1) * 128], hb[:, kc, c * 64:(c + 1) * 64], wglb[:, kc, 128:256], start=(kc == 0), stop=(kc == 7), r=[wglb, hb], w=[pA])
            k.cp(gvt[:, g4 * 4:(g4 + 1) * 4, :], pA[0:64, :].rearrange("p (c t) -> p c t", t=128), r=[pA], w=[gvt], e='act')
        k.mm(pp[0:64, :], gkw[:, :], gkl[:, :], r=[gkw, gkl], w=[pp])
        k.act(gsp[:, :], pp[0:64, :], AF.Exp, bias=vx[0:64, 3:4], scale=-1.0, r=[pp, vx], w=[gsp])
        k.act(gsp[:, :], gsp[:, :], AF.Ln, bias=1.0, r=[gsp], w=[gsp])
        k.op('dve', lambda e: e.tensor_tensor_scan(out=gb[:, :], data0=C['resetm'][0:64, :], data1=gsp[:, :], initial=0.0, op0=ALU.mult, op1=ALU.add), r=[C['resetm'], gsp], w=[gb])
        k.act(geb[:, :], gb[:, :], AF.Exp, scale=-1.0 / 16, r=[gb], w=[geb])
        k.act(genb[:, :], gb[:, :], AF.Exp, scale=1.0 / 16, r=[gb], w=[genb])
        k.stt(gqt[:, :], gq[:, :], 0.125, geb[:, :], ALU.mult, ALU.mult, r=[gq, geb], w=[gqt])
        k.tt(gkt[:, :], gk[:, :], genb[:, :], ALU.mult, r=[gk, genb], w=[gkt])
        g3 = lambda buf: buf[:, :].rearrange("p (c t) -> p c t", t=64)
        k.tt(g3(gkd), g3(gkt), g3(geb)[:, :, 63:64].to_broadcast([64, NCH, 64]), ALU.mult, r=[gkt, geb], w=[gkd])
        for c in range(NCH):
            k.mm(pA[0:64, c * 64:(c + 1) * 64], gkt[:, c * 64:(c + 1) * 64], gqt[:, c * 64:(c + 1) * 64], r=[gkt, gqt], w=[pA])
        k.tt(gAT[:, :, :], pA[0:64, :].rearrange("p (c t) -> p c t", t=64), C['m_ui'][0:64, :].rearrange("p (c t) -> p c t", t=64), ALU.mult, r=[pA, C['m_ui']], w=[gAT])
        for c in range(NCH):
            k.tr(pA[0:64, c * 64:(c + 1) * 64], gkd[:, c * 64:(c + 1) * 64], C['ident'][0:64, 0:64], r=[gkd, C['ident']], w=[pA])
        k.cp(gkdt[:, :, :], pA[0:64, :].rearrange("p (c t) -> p c t", t=64), r=[pA], w=[gkdt], e='act')
        for c in range(NCH):
            S = gS[gsidx % 2]; Sn = gS[(gsidx + 1) % 2]; gsidx += 1
            k.mm(pO[:, c * 64:(c + 1) * 64], gvt[:, c, :], gAT[:, c, :], start=True, stop=False, r=[gvt, gAT], w=[pO])
            k.mm(pO[:, c * 64:(c + 1) * 64], S[:, :], gqt[:, c * 64:(c + 1) * 64], start=False, stop=True, r=[S, gqt], w=[pO])
            k.mm(pWUS[0:64, 384:512], gkdt[:, c, :], gvt[:, c, :], r=[gkdt, gvt], w=[pWUS])
            k.stt(Sn[:, :], S[:, :], geb[:, c * 64 + 63:c * 64 + 64], pWUS[0:64, 384:512], ALU.mult, ALU.add, r=[S, geb, pWUS], w=[Sn])
        k.cp(goT[:, :], pO[:, :], r=[pO], w=[goT], e='act')
        k.tt(gsq[:, :], goT[:, :], goT[:, :], ALU.mult, r=[goT], w=[gsq])
        k.mm(pp[:, :], C['ones'][:, :], gsq[:, :], r=[C['ones'], gsq], w=[pp])
        k.act(gsq[:, :], pp[:, :], AF.Sqrt, bias=vx[:, 2:3], scale=1.0 / 128, r=[pp, vx], w=[gsq])
        k.op('dve', lambda e: e.reciprocal(out=gsq[:, :], in_=gsq[:, :]), r=[gsq], w=[gsq])
        k.stt(goT[:, :], goT[:, :], gv[:, 1:2], gsq[:, :], ALU.mult, ALU.mult, r=[goT, gv, gsq], w=[goT])
        k.act(gsq[:, :], ggate[:, :], AF.Silu, r=[ggate], w=[gsq])
        k.tt(gout[:, :], goT[:, :], gsq[:, :], ALU.mult, r=[goT, gsq], w=[gout])
        k.dma(over['y_dst'](1, blk) if 'y_dst' in over else y_gla[:, cs], gout[:, :], r=[gout])
        if 'after_blk' in over:
            over['after_blk'](blk)
    if own:
        k.finish()
        k.close()
        return nc
    k.pop()

def mix0_inputs(d, hT_b, j):
    W = d['ab_w_in'][0]
    ch = slice(128 * j, 128 * j + 128)
    RW = 512
    o_r, o_wl, o_k, o_v, o_al, o_gl = 0, 512, 576, 1088, 1600, 1664
    cols = np.concatenate([np.arange(o_r + 128 * j, o_r + 128 * j + 128), np.arange(o_k + 128 * j, o_k + 128 * j + 128),
                           np.arange(o_v + 128 * j, o_v + 128 * j + 128), np.arange(o_wl, o_wl + 64), np.arange(o_al, o_al + 64),
                           np.arange(o_gl, o_gl + 128)])
    w_rw = np.ascontiguousarray(W[:, cols])
    mu = d['rw_mu'][0][cols].reshape(5, 128).T
    G0 = 1792
    gq, gk, gv, gkl, gg = G0, G0 + 256, G0 + 512, G0 + 1024, G0 + 1040
    gcols = np.concatenate([np.arange(gq + 64 * j, gq + 64 * j + 64), np.arange(gk + 64 * j, gk + 64 * j + 64),
                            np.arange(gv + 128 * j, gv + 128 * j + 128), np.arange(gg + 128 * j, gg + 128 * j + 128), np.arange(gkl, gkl + 16)])
    w_gla = np.ascontiguousarray(W[:, gcols])
    rwvec = np.zeros((128, 16), np.float32)
    rwvec[:, 0:5] = mu
    for i, n in enumerate(['rw_w0', 'rw_a0', 'rw_k_k', 'rw_k_a']):
        rwvec[:, 5 + i] = d[n][0][ch]
    rwvec[:, 9] = d['rw_r_k'][0].reshape(512)[ch]
    rwvec[:, 10] = d['rw_gn_g'][0][ch]; rwvec[:, 11] = d['rw_gn_b'][0][ch]
    lora_wa = np.concatenate([d['rw_w2'][0][:, ch], d['rw_a2'][0][:, ch]], 0)
    g2c = np.ascontiguousarray(d['rw_g2'][0][:, ch])
    gk_w2 = np.ascontiguousarray(d['gla_gk_w2'][0][:, 64 * j:64 * j + 64])
    glavec = np.zeros((128, 2), np.float32)
    glavec[:64, 0] = d['gla_gk_b'][0][64 * j:64 * j + 64]; glavec[:, 1] = d['gla_norm_g'][0]
    im = {"hT": np.ascontiguousarray(hT_b), "w_rw": w_rw, "w_gla": w_gla, "rwvec": rwvec, "lora_wa": np.ascontiguousarray(lora_wa), "g2c": g2c,
          "gk_w2": gk_w2, "glavec": glavec}
    im.update(consts0())
    return im


import math

TB = 512
NCH = 8
NORM_EPS = 1e-5
TWO_PI = 2.0 * math.pi

def consts1():
    c = {}
    c["ident"] = np.eye(128, dtype=np.float32)
    c["ones"] = np.ones((128, 128), np.float32)
    p = np.arange(128)[:, None] % 64; q = np.arange(512)[None, :] % 64
    c["m_ui"] = (q >= p).astype(np.float32)
    rm = np.ones((128, 512), np.float32); rm[:, ::64] = 0
    c["resetm"] = rm
    c["iota"] = np.tile(np.arange(512, dtype=np.float32)[None, :], (128, 1))
    return c

def sincos(k, u, sn, cs, t1, t2, ti, sl):
    def wrap(x):
        k.ts(sl(t2), sl(x), 0.5, None, op0=ALU.is_gt, r=[x], w=[t2])
        k.tt(sl(x), sl(x), sl(t2), ALU.subtract, r=[x, t2], w=[x])
        k.ts(sl(t2), sl(x), -0.5, None, op0=ALU.is_lt, r=[x], w=[t2])
        k.tt(sl(x), sl(x), sl(t2), ALU.add, r=[x, t2], w=[x])
    k.cp(sl(ti), sl(u), r=[u], w=[ti])
    k.cp(sl(t1), sl(ti), r=[ti], w=[t1])
    k.tt(sl(t1), sl(u), sl(t1), ALU.subtract, r=[u, t1], w=[t1])
    wrap(t1)
    k.act(sl(sn), sl(t1), AF.Sin, scale=TWO_PI, r=[t1], w=[sn])
    k.ts(sl(t1), sl(t1), 0.25, None, op0=ALU.add, r=[t1], w=[t1])
    wrap(t1)
    k.act(sl(cs), sl(t1), AF.Sin, scale=TWO_PI, r=[t1], w=[cs])

def build_mix1(L, ctx=None):
    own = ctx is None
    nc = bass.Bass("TRN2", target_bir_lowering=False) if own else ctx['nc']
    pfx = '' if own else ctx['pfx']
    over = {} if own else ctx['over']
    nblk = L // TB
    def din(name, shape):
        if name in over:
            return over[name]
        return nc.dram_tensor(pfx + name, shape, F32, kind="ExternalInput").ap()
    hT = din("hT", [1024, L]) if "hT_blk" not in over else None
    w_cd = din("w_cd", [1024, 640])
    Bre = din("Bre", [4, 128, 128]); Bim = din("Bim", [4, 128, 128]); Cre = din("Cre", [4, 128, 128]); Cim = din("Cim", [4, 128, 128])
    s5vec = din("s5vec", [128, 16]); hgvec = din("hgvec", [128, 4])
    cd = {n: din(n, list(v.shape)) for n, v in consts1().items()}
    y_s5 = None if "y_dst" in over else nc.dram_tensor("y_s5", [128, L], F32, kind="ExternalOutput").ap()
    y_hg = None if "y_dst" in over else nc.dram_tensor("y_hg", [128, L], F32, kind="ExternalOutput").ap()
    k = KB(nc) if own else ctx['k']
    if not own:
        k.push()
    C = {}
    for n, v in consts1().items():
        C[n] = k.sb(list(v.shape), name='c_' + n); k.dma(C[n][:], cd[n], w=[C[n]])
    sv = k.sb([128, 16]); k.dma(sv[:], s5vec, w=[sv])
    hv = k.sb([128, 4]); k.dma(hv[:], hgvec, w=[hv])
    BreS = k.sb([128, 4, 128]); BimS = k.sb([128, 4, 128]); CreS = k.sb([128, 4, 128]); CimS = k.sb([128, 4, 128])
    for (dst, src) in [(BreS, Bre), (BimS, Bim), (CreS, Cre), (CimS, Cim)]:
        k.dma(dst[:], src.rearrange("s p n -> p s n"), w=[dst])
    k.ts(CimS[:, :, :], CimS[:, :, :], -1.0, None, op0=ALU.mult, r=[CimS], w=[CimS])
    wcdb = k.sb([128, 8, 640], BF16); k.dma(wcdb[:], w_cd.rearrange("(kc p) n -> p kc n", p=128), w=[wcdb], q='pool')
    hTb = [k.sb([128, 8, TB], BF16) for _ in range(2)]
    hT3 = hT.rearrange("(kc p) t -> p kc t", p=128) if hT is not None else None
    pp = k.ps([128, 512]); pA = k.ps([128, 512]); pbr = k.ps([128, 512]); pbi = k.ps([128, 512])
    py = k.ps([128, 512]); pO = k.ps([128, 512]); pS = k.ps([128, 512])
    T = lambda name: k.sb([128, TB], name=name)
    S4 = lambda name: k.sb([128, 4], name=name)
    lre, lim, dt, rho, th, f4, sn4, cs4, t4a, t4b, are, aim, nre, den, zre, zim, u512, s512, c512 = [S4(n) for n in
        ['lre', 'lim', 'dt', 'rho', 'th', 'f4', 'sn4', 'cs4', 't4a', 't4b', 'are', 'aim', 'nre', 'den', 'zre', 'zim', 'u512', 's512', 'c512']]
    ti4 = k.sb([128, 4], I32, name='ti4')
    A_ = lambda b: b[:, :]
    k.ts(A_(lre), sv[:, 0:4], -1e-4, None, op0=ALU.min, r=[sv], w=[lre])
    k.cp(A_(lim), sv[:, 4:8], r=[sv], w=[lim])
    k.act(A_(dt), sv[:, 8:12], AF.Exp, r=[sv], w=[dt])
    k.tt(A_(t4a), A_(lre), A_(dt), ALU.mult, r=[lre, dt], w=[t4a])
    k.act(A_(rho), A_(t4a), AF.Exp, r=[t4a], w=[rho])
    k.tt(A_(th), A_(lim), A_(dt), ALU.mult, r=[lim, dt], w=[th])
    k.ts(A_(f4), A_(th), 1.0 / TWO_PI, None, op0=ALU.mult, r=[th], w=[f4])
    sincos(k, f4, sn4, cs4, t4a, t4b, ti4, A_)
    k.tt(A_(are), A_(rho), A_(cs4), ALU.mult, r=[rho, cs4], w=[are])
    k.tt(A_(aim), A_(rho), A_(sn4), ALU.mult, r=[rho, sn4], w=[aim])
    k.ts(A_(nre), A_(are), -1.0, None, op0=ALU.add, r=[are], w=[nre])
    k.tt(A_(den), A_(lre), A_(lre), ALU.mult, r=[lre], w=[den])
    k.tt(A_(t4a), A_(lim), A_(lim), ALU.mult, r=[lim], w=[t4a])
    k.tt(A_(den), A_(den), A_(t4a), ALU.add, r=[den, t4a], w=[den])
    k.op('dve', lambda e: e.reciprocal(out=A_(den), in_=A_(den)), r=[den], w=[den])
    k.tt(A_(zre), A_(nre), A_(lre), ALU.mult, r=[nre, lre], w=[zre])
    k.tt(A_(t4a), A_(aim), A_(lim), ALU.mult, r=[aim, lim], w=[t4a])
    k.tt(A_(zre), A_(zre), A_(t4a), ALU.add, r=[zre, t4a], w=[zre])
    k.tt(A_(zre), A_(zre), A_(den), ALU.mult, r=[zre, den], w=[zre])
    k.tt(A_(zim), A_(aim), A_(lre), ALU.mult, r=[aim, lre], w=[zim])
    k.tt(A_(t4a), A_(nre), A_(lim), ALU.mult, r=[nre, lim], w=[t4a])
    k.tt(A_(zim), A_(zim), A_(t4a), ALU.subtract, r=[zim, t4a], w=[zim])
    k.tt(A_(zim), A_(zim), A_(den), ALU.mult, r=[zim, den], w=[zim])
    k.ts(A_(u512), A_(f4), float(TB), None, op0=ALU.mult, r=[f4], w=[u512])
    sincos(k, u512, s512, c512, t4a, t4b, ti4, A_)
    cosl = [T('cosl%d' % s) for s in range(4)]; sinl = [T('sinl%d' % s) for s in range(4)]
    Ere = [T('Ere%d' % s) for s in range(4)]; Eim = [T('Eim%d' % s) for s in range(4)]
    rhoT = [T('rhoT%d' % s) for s in range(4)]
    tu = T('tu'); tt1 = T('tt1'); tt2 = T('tt2'); tti = k.sb([128, TB], I32, name='tti')
    for s in range(4):
        k.ts(tu[:, :], C['iota'][:, :], f4[:, s:s + 1], None, op0=ALU.mult, r=[C['iota'], f4], w=[tu])
        sincos(k, tu, sinl[s], cosl[s], tt1, tt2, tti, A_)
        k.ts(Ere[s][:, :], cosl[s][:, :], zre[:, s:s + 1], None, op0=ALU.mult, r=[cosl[s], zre], w=[Ere[s]])
        k.stt(Ere[s][:, :], sinl[s][:, :], zim[:, s:s + 1], Ere[s][:, :], ALU.mult, ALU.add, r=[sinl[s], zim, Ere[s]], w=[Ere[s]])
        k.ts(Eim[s][:, :], cosl[s][:, :], zim[:, s:s + 1], None, op0=ALU.mult, r=[cosl[s], zim], w=[Eim[s]])
        k.stt(tt1[:, :], sinl[s][:, :], zre[:, s:s + 1], Eim[s][:, :], ALU.mult, ALU.subtract, r=[sinl[s], zre, Eim[s]], w=[tt1])
        k.ts(Eim[s][:, :], tt1[:, :], -1.0, None, op0=ALU.mult, r=[tt1], w=[Eim[s]])
        k.op('pool', lambda e: e.memset(rhoT[s][:, :], 1.0), w=[rhoT[s]])
        k.ts(rhoT[s][:, :], rhoT[s][:, :], rho[:, s:s + 1], None, op0=ALU.mult, r=[rhoT[s], rho], w=[rhoT[s]])
    carry = [k.sb([128, 2], name='carry%d' % s) for s in range(4)]
    for s in range(4):
        k.op('pool', lambda e: e.memset(carry[s][:, :], 0.0), w=[carry[s]])
    ctmp = k.sb([128, 2], name='ctmp')
    hx = k.sb([128, 4], name='hx')
    k.tt(hx[:, 0:1], hv[:, 1:2], hv[:, 0:1], ALU.subtract, r=[hv], w=[hx])
    k.act(hx[:, 0:1], hx[:, 0:1], AF.Sigmoid, r=[hx], w=[hx])
    k.ts(hx[:, 1:2], hx[:, 0:1], -1.0, 1.0, op0=ALU.mult, op1=ALU.add, r=[hx], w=[hx])
    k.ts(hx[:, 2:3], hx[:, 1:2], -1.0, None, op0=ALU.mult, r=[hx], w=[hx])
    k.op('pool', lambda e: e.memset(hx[:, 3:4], NORM_EPS), w=[hx])
    uT = T('uT'); xre = T('xre'); xim = T('xim'); gre = T('gre'); gim = T('gim'); w1_ = T('w1_'); w2_ = T('w2_'); hre = T('hre'); him = T('him')
    ys = T('ys'); yo = T('yo')
    hq = T('hq'); hf = T('hf'); hgate = T('hgate'); hit = k.sb([64, NCH, 128], name='hit')
    hlg = T('hlg'); hb = T('hb'); heb = T('heb'); henb = T('henb'); hqt = T('hqt'); hkt = T('hkt'); hkd = T('hkd')
    hAT = k.sb([64, NCH, 64], name='hAT'); hkdt = k.sb([64, NCH, 128], name='hkdt')
    hS = [k.sb([128, 128], name='hS%d' % i) for i in range(2)]
    k.op('pool', lambda e: e.memset(hS[0][:], 0.0), w=[hS[0]])
    hoT = T('hoT'); hsq = T('hsq'); hout = T('hout')
    hsidx = 0
    for blk in range(nblk):
        hb_ = hTb[blk % 2]
        cs = slice(blk * TB, (blk + 1) * TB)
        if hT3 is not None:
            k.dma(hb_[:], hT3[:, :, cs], w=[hb_], q='pool')
        else:
            hsrc_, hbufs_ = over['hT_blk'](blk)
            k.dma(hb_[:], hsrc_, r=hbufs_, w=[hb_], q='pool')
        for (dst, c0) in [(uT, 0), (hq, 128), (hf, 256), (hgate, 512)]:
            for kc in range(8):
                k.mm(pp[:, :], wcdb[:, kc, c0:c0 + 128], hb_[:, kc, :], start=(kc == 0), stop=(kc == 7), r=[wcdb, hb_], w=[pp])
            k.cp(dst[:, :], pp[:, :], r=[pp], w=[dst], e='act')
        for g4 in range(2):
            for cc in range(4):
                c = g4 * 4 + cc
                for kc in range(8):
                    k.mm(pA[0:64, cc * 128:(cc + 1) * 128], hb_[:, kc, c * 64:(c + 1) * 64], wcdb[:, kc, 384:512], start=(kc == 0), stop=(kc == 7), r=[wcdb, hb_], w=[pA])
            k.cp(hit[:, g4 * 4:(g4 + 1) * 4, :], pA[0:64, :].rearrange("p (c t) -> p c t", t=128), r=[pA], w=[hit], e='act')
        for s in range(4):
            k.mm(pbr[:, :], BreS[:, s, :], uT[:, :], r=[BreS, uT], w=[pbr])
            k.mm(pbi[:, :], BimS[:, s, :], uT[:, :], r=[BimS, uT], w=[pbi])
            k.tt(xre[:, :], pbr[:, :], Ere[s][:, :], ALU.mult, r=[pbr, Ere[s]], w=[xre])
            k.tt(w1_[:, :], pbi[:, :], Eim[s][:, :], ALU.mult, r=[pbi, Eim[s]], w=[w1_])
            k.tt(xre[:, :], xre[:, :], w1_[:, :], ALU.subtract, r=[xre, w1_], w=[xre])
            k.tt(xim[:, :], pbr[:, :], Eim[s][:, :], ALU.mult, r=[pbr, Eim[s]], w=[xim])
            k.tt(w2_[:, :], pbi[:, :], Ere[s][:, :], ALU.mult, r=[pbi, Ere[s]], w=[w2_])
            k.tt(xim[:, :], xim[:, :], w2_[:, :], ALU.add, r=[xim, w2_], w=[xim])
            k.op('dve', lambda e: e.tensor_tensor_scan(out=gre[:, :], data0=rhoT[s][:, :], data1=xre[:, :], initial=carry[s][:, 0:1], op0=ALU.mult, op1=ALU.add), r=[rhoT[s], xre, carry[s]], w=[gre])
            k.op('dve', lambda e: e.tensor_tensor_scan(out=gim[:, :], data0=rhoT[s][:, :], data1=xim[:, :], initial=carry[s][:, 1:2], op0=ALU.mult, op1=ALU.add), r=[rhoT[s], xim, carry[s]], w=[gim])
            k.ts(ctmp[:, 0:1], gim[:, TB - 1:TB], s512[:, s:s + 1], None, op0=ALU.mult, r=[gim, s512], w=[ctmp])
            k.ts(ctmp[:, 1:2], gim[:, TB - 1:TB], c512[:, s:s + 1], None, op0=ALU.mult, r=[gim, c512], w=[ctmp])
            k.stt(carry[s][:, 0:1], gre[:, TB - 1:TB], c512[:, s:s + 1], ctmp[:, 0:1], ALU.mult, ALU.subtract, r=[gre, c512, ctmp], w=[carry[s]])
            k.stt(carry[s][:, 1:2], gre[:, TB - 1:TB], s512[:, s:s + 1], ctmp[:, 1:2], ALU.mult, ALU.add, r=[gre, s512, ctmp], w=[carry[s]])
            k.tt(hre[:, :], gre[:, :], cosl[s][:, :], ALU.mult, r=[gre, cosl[s]], w=[hre])
            k.tt(w1_[:, :], gim[:, :], sinl[s][:, :], ALU.mult, r=[gim, sinl[s]], w=[w1_])
            k.tt(hre[:, :], hre[:, :], w1_[:, :], ALU.subtract, r=[hre, w1_], w=[hre])
            k.tt(him[:, :], gre[:, :], sinl[s][:, :], ALU.mult, r=[gre, sinl[s]], w=[him])
            k.tt(w2_[:, :], gim[:, :], cosl[s][:, :], ALU.mult, r=[gim, cosl[s]], w=[w2_])
            k.tt(him[:, :], him[:, :], w2_[:, :], ALU.add, r=[him, w2_], w=[him])
            k.mm(py[:, :], CreS[:, s, :], hre[:, :], start=(s == 0), stop=False, r=[CreS, hre], w=[py])
            k.mm(py[:, :], CimS[:, s, :], him[:, :], start=False, stop=(s == 3), r=[CimS, him], w=[py])
        k.stt(ys[:, :], uT[:, :], sv[:, 12:13], py[:, :], ALU.mult, ALU.add, r=[uT, sv, py], w=[ys])
        k.tt(w1_[:, :], ys[:, :], ys[:, :], ALU.mult, r=[ys], w=[w1_])
        k.ts(w1_[:, :], w1_[:, :], 0.044715, 1.0, op0=ALU.mult, op1=ALU.add, r=[w1_], w=[w1_])
        k.tt(w1_[:, :], w1_[:, :], ys[:, :], ALU.mult, r=[w1_, ys], w=[w1_])
        k.act(w1_[:, :], w1_[:, :], AF.Sigmoid, scale=1.5957691216057308, r=[w1_], w=[w1_])
        k.tt(yo[:, :], ys[:, :], w1_[:, :], ALU.mult, r=[ys, w1_], w=[yo])
        k.dma(over['y_dst'](0, blk) if 'y_dst' in over else y_s5[:, cs], yo[:, :], r=[yo])
        k.act(hsq[:, :], hf[:, :], AF.Sigmoid, r=[hf], w=[hsq])
        k.ts(hlg[:, :], hsq[:, :], hx[:, 1:2], hx[:, 0:1], op0=ALU.mult, op1=ALU.add, r=[hsq, hx], w=[hlg])
        k.act(hlg[:, :], hlg[:, :], AF.Ln, r=[hlg], w=[hlg])
        k.ts(hkt[:, :], hsq[:, :], hx[:, 2:3], hx[:, 1:2], op0=ALU.mult, op1=ALU.add, r=[hsq, hx], w=[hkt])
        k.op('dve', lambda e: e.tensor_tensor_scan(out=hb[:, :], data0=C['resetm'][:, :], data1=hlg[:, :], initial=0.0, op0=ALU.mult, op1=ALU.add), r=[C['resetm'], hlg], w=[hb])
        k.act(heb[:, :], hb[:, :], AF.Exp, r=[hb], w=[heb])
        k.act(henb[:, :], hb[:, :], AF.Exp, scale=-1.0, r=[hb], w=[henb])
        k.act(hqt[:, :], hq[:, :], AF.Silu, r=[hq], w=[hqt])
        k.tt(hqt[:, :], hqt[:, :], heb[:, :], ALU.mult, r=[hqt, heb], w=[hqt])
        k.tt(hkt[:, :], hkt[:, :], henb[:, :], ALU.mult, r=[hkt, henb], w=[hkt])
        g3 = lambda buf: buf[:, :].rearrange("p (c t) -> p c t", t=64)
        k.tt(g3(hkd), g3(hkt), g3(heb)[:, :, 63:64].to_broadcast([128, NCH, 64]), ALU.mult, r=[hkt, heb], w=[hkd])
        for c in range(NCH):
            k.mm(pA[0:64, c * 64:(c + 1) * 64], hkt[:, c * 64:(c + 1) * 64], hqt[:, c * 64:(c + 1) * 64], r=[hkt, hqt], w=[pA])
        k.tt(hAT[:, :, :], pA[0:64, :].rearrange("p (c t) -> p c t", t=64), C['m_ui'][0:64, :].rearrange("p (c t) -> p c t", t=64), ALU.mult, r=[pA, C['m_ui']], w=[hAT])
        for g4 in range(2):
            for cc in range(4):
                c = g4 * 4 + cc
                k.tr(pA[0:64, cc * 128:(cc + 1) * 128], hkd[:, c * 64:(c + 1) * 64], C['ident'][:, :], r=[hkd, C['ident']], w=[pA])
            k.cp(hkdt[:, g4 * 4:(g4 + 1) * 4, :], pA[0:64, :].rearrange("p (c t) -> p c t", t=128), r=[pA], w=[hkdt], e='act')
        for c in range(NCH):
            S = hS[hsidx % 2]; Sn = hS[(hsidx + 1) % 2]; hsidx += 1
            k.mm(pO[:, c * 64:(c + 1) * 64], hit[:, c, :], hAT[:, c, :], start=True, stop=False, r=[hit, hAT], w=[pO])
            k.mm(pO[:, c * 64:(c + 1) * 64], S[:, :], hqt[:, c * 64:(c + 1) * 64], start=False, stop=True, r=[S, hqt], w=[pO])
            k.mm(pS[:, 0:128], hkdt[:, c, :], hit[:, c, :], r=[hkdt, hit], w=[pS])
            k.stt(Sn[:, :], S[:, :], heb[:, c * 64 + 63:c * 64 + 64], pS[:, 0:128], ALU.mult, ALU.add, r=[S, heb, pS], w=[Sn])
        k.cp(hoT[:, :], pO[:, :], r=[pO], w=[hoT], e='act')
        k.tt(hsq[:, :], hoT[:, :], hoT[:, :], ALU.mult, r=[hoT], w=[hsq])
        k.mm(pp[:, :], C['ones'][:, :], hsq[:, :], r=[C['ones'], hsq], w=[pp])
        k.act(hsq[:, :], pp[:, :], AF.Sqrt, bias=hx[:, 3:4], scale=1.0 / 128, r=[pp, hx], w=[hsq])
        k.op('dve', lambda e: e.reciprocal(out=hsq[:, :], in_=hsq[:, :]), r=[hsq], w=[hsq])
        k.stt(hoT[:, :], hoT[:, :], hv[:, 2:3], hsq[:, :], ALU.mult, ALU.mult, r=[hoT, hv, hsq], w=[hoT])
        k.act(hsq[:, :], hgate[:, :], AF.Silu, r=[hgate], w=[hsq])
        k.tt(hout[:, :], hoT[:, :], hsq[:, :], ALU.mult, r=[hoT, hsq], w=[hout])
        k.dma(over['y_dst'](1, blk) if 'y_dst' in over else y_hg[:, cs], hout[:, :], r=[hout])
        if 'after_blk' in over:
            over['after_blk'](blk)
    if own:
        k.finish()
        k.close()
        return nc
    k.pop()

def mix1_inputs(d, hT_b, j):
    W = d['cd_w_in'][0]
    cols = np.concatenate([np.arange(128 * j, 128 * j + 128)] + [np.arange(512 * m + 128 * j, 512 * m + 128 * j + 128) for m in (1, 2, 3, 4)])
    w_cd = np.ascontiguousarray(W[:, cols])
    Bre = np.zeros((4, 128, 128), np.float32); Bim = np.zeros_like(Bre); Cre = np.zeros_like(Bre); Cim = np.zeros_like(Bre)
    s5vec = np.zeros((128, 16), np.float32)
    for st in range(4):
        for gl in range(2):
            g = 8 * j + 2 * st + gl
            chs = slice((2 * st + gl) * 16, (2 * st + gl) * 16 + 16); ps = slice(gl * 64, gl * 64 + 64)
            Bre[st, chs, ps] = d['s5_b_re'][0][g].T; Bim[st, chs, ps] = d['s5_b_im'][0][g].T
            Cre[st, ps, chs] = d['s5_c_re'][0][g].T; Cim[st, ps, chs] = d['s5_c_im'][0][g].T
            s5vec[ps, st] = d['s5_a_re'][0][g]; s5vec[ps, 4 + st] = d['s5_a_im'][0][g]; s5vec[ps, 8 + st] = d['s5_log_dt'][0][g]
    s5vec[:, 12] = d['s5_d'][0][128 * j:128 * j + 128]
    hgvec = np.zeros((128, 4), np.float32)
    hgvec[:, 0] = d['hg_lb'][0][128 * j:128 * j + 128]; hgvec[:, 1] = d['hg_lb'][1][128 * j:128 * j + 128]; hgvec[:, 2] = d['hg_norm_g'][0]
    im = {"hT": np.ascontiguousarray(hT_b), "w_cd": w_cd, "Bre": Bre, "Bim": Bim, "Cre": Cre, "Cim": Cim, "s5vec": s5vec, "hgvec": hgvec}
    im.update(consts1())
    return im


GROUPS = [[0, 1, 2, 3], [4, 5, 6, 7]]

def build_fused(L, NT):
    nc = bass.Bass("TRN2", target_bir_lowering=False)
    k = KB(nc)
    D = 1024
    CW = min(1024, NT)
    NQY = L // CW; BPC = CW // TB
    HW_ = 512
    NQH = NT // HW_
    def ydram(tag):
        src = nc.dram_tensor(tag + "src", [NQY, 256, CW], F32).ap(); dst = nc.dram_tensor(tag + "all", [NQY, 1024, CW], F32).ap()
        return src, dst, [Buf(None, tag + 'all%d' % q) for q in range(NQY)]
    y0src, y0all, Y0 = ydram("y0"); y1src, y1all, Y1 = ydram("y1")
    h0tok = nc.dram_tensor("h0tok", [NT, D], F32).ap()
    h0Tsrc = nc.dram_tensor("h0Tsrc", [NQH, D, HW_], BF16).ap(); h0Tall = nc.dram_tensor("h0Tall", [NQH, 4 * D, HW_], BF16).ap()
    H0 = [Buf(None, 'h0Tall%d' % q) for q in range(NQH)]
    pid = nc.sync.partition_id()
    rank = pid % 4
    tag_of = {}
    def y_hooks(ysrc, yall, YB):
        def y_dst(which, blk):
            q = blk // BPC; c0 = (blk % BPC) * TB
            return ysrc[q][which * 128:(which + 1) * 128, c0:c0 + TB]
        def after_blk(blk):
            if blk % BPC == BPC - 1:
                q = blk // BPC
                k.allgather(ysrc[q], yall[q], YB[q], GROUPS)
        NM = NT // CW
        ymine = nc.dram_tensor(tag_of[id(ysrc)] + "mine", [NM, 1024, CW], F32).ap()
        YM = [[Buf(None, 'ym%d_%d' % (m, hh)) for hh in range(2)] for m in range(NM)]
        def pre():
            for m in range(NM):
                qe = rank * NM + m
                for hh in range(2):
                    rs = slice(hh * 512, (hh + 1) * 512)
                    k.dma(ymine[m][rs, :], yall[bass.ds(qe, 1)].rearrange("o r t -> (o r) t")[rs, :], r=YB, w=[YM[m][hh]])
        def yT_src(i):
            m = (i * 128) // CW
            c0 = (i * 128) % CW
            v = ymine[m].rearrange("(r two p) t -> p r two t", two=2, p=128)
            return v[:, :, 0, c0:c0 + 128], v[:, :, 1, c0:c0 + 128], YM[m]
        yT_src.pre = pre
        return y_dst, after_blk, yT_src
    tag_of[id(y0src)] = 'y0'; tag_of[id(y1src)] = 'y1'
    yd0, ab0, ys0 = y_hooks(y0src, y0all, Y0)
    yd1, ab1, ys1 = y_hooks(y1src, y1all, Y1)
    build_mix0(L, ctx=dict(nc=nc, k=k, pfx='m0_', over={"y_dst": yd0, "after_blk": ab0}))
    def hT_dst(i):
        q = (i * 128) // HW_; c0 = (i * 128) % HW_
        return h0Tsrc[q].rearrange("(kc p) t -> p kc t", p=128)[:, :, c0:c0 + 128]
    def after_tile(i):
        if ((i + 1) * 128) % HW_ == 0:
            q = (i * 128) // HW_
            k.allgather(h0Tsrc[q], h0Tall[q], H0[q], GROUPS)
    build_post(NT, False, ctx=dict(nc=nc, k=k, pfx='p0_', over={"yT_src": ys0, "out": h0tok, "hT_dst": hT_dst, "after_tile": after_tile}))
    def hT_blk(blk):
        r = blk // NQH; lb = blk % NQH
        return h0Tall[lb].rearrange("(r kc p) t -> p r kc t", r=4, p=128)[:, r, :, :], [H0[lb]]
    build_mix1(L, ctx=dict(nc=nc, k=k, pfx='m1_', over={"y_dst": yd1, "after_blk": ab1, "hT_blk": hT_blk}))
    build_post(NT, True, ctx=dict(nc=nc, k=k, pfx='p1_', over={"yT_src": ys1, "h_tok": h0tok}))
    k.finish()
    k.close()
    return nc

_CACHE = {}

def _post_inputs(d, L_, NT_):
    im = {"w_out": (d['ab_w_out'][0] if L_ == 0 else d['cd_w_out'][0]),
          "ln": np.stack([d['ln1_g'][L_], d['ln1_b'][L_], d['ln2_g'][L_], d['ln2_b'][L_]]),
          "wr": np.ascontiguousarray(np.concatenate([d['moe_wg'][L_], d['moe_we'][L_].transpose(1, 0, 2).reshape(1024, 32)], 1)),
          "br": np.concatenate([d['moe_bg'][L_], d['moe_be'][L_].reshape(32)])[None, :],
          "w1r": np.ascontiguousarray(d['moe_w1'][L_].reshape(32, 8, 128, 512).transpose(0, 2, 1, 3)).reshape(4096, 4096),
          "w3r": np.ascontiguousarray(d['moe_w3'][L_].reshape(32, 8, 128, 512).transpose(0, 2, 1, 3)).reshape(4096, 4096),
          "w2r": np.ascontiguousarray(d['moe_w2'][L_].reshape(32, 4, 128, 1024).transpose(0, 2, 1, 3)).reshape(4096, 4096)}
    im.update(post_consts(NT_))
    if L_ == 1:
        im["glu_w"] = d['s5_glu_w'][0]
        im["glu_b"] = np.ascontiguousarray(d["s5_glu_b"][0].reshape(4, 128).T)
    return im

def kernel(**inputs):
    d = {k_: np.asarray(v, dtype=np.float32) for k_, v in inputs.items()}
    x = d['x']
    B, L, D = x.shape
    NC = 8
    NT = B * L // NC
    cores = list(range(NC))
    if 'fused' not in _CACHE:
        _CACHE['fused'] = build_fused(L, NT)
    nc = _CACHE['fused']
    xT = [np.ascontiguousarray(x[b].T) for b in range(B)]
    xflat = x.reshape(B * L, D)
    p0 = _post_inputs(d, 0, NT); p1 = _post_inputs(d, 1, NT)
    ims = []
    for c in cores:
        b, j = c // 4, c % 4
        im = {}
        for n_, v_ in mix0_inputs(d, xT[b], j).items():
            im['m0_' + n_] = v_
        for n_, v_ in mix1_inputs(d, None, j).items():
            if n_ != 'hT':
                im['m1_' + n_] = v_
        for n_, v_ in p0.items():
            im['p0_' + n_] = v_
        for n_, v_ in p1.items():
            im['p1_' + n_] = v_
        im['p0_h_tok'] = np.ascontiguousarray(xflat[c * NT:(c + 1) * NT])
        ims.append(im)
    res = run_bass_kernel_spmd(nc, ims, core_ids=cores).results
    out = np.concatenate([res[c]["p1_out"] for c in cores], 0).reshape(B, L, D)
    return out.astype(np.float32)
```

```python
import numpy as np
from contextlib import ExitStack
import concourse.bass as bass
import concourse.mybir as mybir
from concourse.bass_utils import run_bass_kernel_spmd

F32 = mybir.dt.float32
BF16 = mybir.dt.bfloat16
I32 = mybir.dt.int32
AF = mybir.ActivationFunctionType
ALU = mybir.AluOpType
AX = mybir.AxisListType

class Buf:
    def __init__(self, t, name):
        self.t = t; self.name = name
        self.w = None
        self.r = {}
    def __getitem__(self, key):
        return self.t[key]

class KB:
    def __init__(self, nc, n_dma_sems=32):
        self.nc = nc
        self.es = ExitStack()
        self.scopes = [self.es]
        self.eng = {'pe': nc.tensor, 'act': nc.scalar, 'dve': nc.vector, 'pool': nc.gpsimd, 'sp': nc.sync}
        self.sem = {}
        self.cnt = {}
        for e in self.eng:
            self.sem[e] = self.es.enter_context(nc.semaphore('s_' + e))
            self.cnt[e] = 0
        self.nd = n_dma_sems
        for i in range(n_dma_sems):
            self.sem['d%d' % i] = self.es.enter_context(nc.semaphore('s_d%d' % i))
            self.cnt['d%d' % i] = 0
        self.dma_rr = 0
        self.seen = {e: {} for e in self.eng}
        self.nbuf = 0
    def sb(self, shape, dtype=F32, name=None):
        self.nbuf += 1
        name = ('%s_%d' % (name, self.nbuf)) if name else ('b%d' % self.nbuf)
        t = self.scopes[-1].enter_context(self.nc.sbuf_tensor(name, list(shape), dtype))
        return Buf(t, name)
    def ps(self, shape, dtype=F32, name=None):
        self.nbuf += 1
        name = ('%s_%d' % (name, self.nbuf)) if name else ('p%d' % self.nbuf)
        t = self.scopes[-1].enter_context(self.nc.psum_tensor(name, list(shape), dtype))
        return Buf(t, name)
    def _need(self, e, key, val):
        if e == 'pe' and key == 'pe':
            return
        if val <= self.seen[e].get(key, 0):
            return
        self.eng[e].wait_ge(self.sem[key], val)
        self.seen[e][key] = val
    def _deps(self, e, r, w):
        for b in r:
            if b.w is not None:
                self._need(e, *b.w)
        for b in w:
            if b.w is not None:
                self._need(e, *b.w)
            for key, val in b.r.items():
                self._need(e, key, val)
    def op(self, e, ins_fn, r=(), w=()):
        self._deps(e, r, w)
        ins = ins_fn(self.eng[e])
        self.cnt[e] += 1
        ins.then_inc(self.sem[e], 1)
        v = self.cnt[e]
        for b in r:
            b.r[e] = v
        for b in w:
            b.w = (e, v); b.r = {}
        return ins
    def dma(self, out_ap, in_ap, r=(), w=(), q='sp', **kw):
        self._deps(q, r, w)
        key = 'd%d' % self.dma_rr
        self.dma_rr = (self.dma_rr + 1) % self.nd
        if self.cnt[key] > 0:
            self._need(q, key, self.cnt[key])
        ins = self.eng[q].dma_start(out=out_ap, in_=in_ap, **kw)
        self.cnt[key] += 16
        ins.then_inc(self.sem[key], 16)
        v = self.cnt[key]
        for b in r:
            b.r[key] = v
        for b in w:
            b.w = (key, v); b.r = {}
        return ins
    def push(self):
        self.scopes.append(ExitStack())
    def pop(self):
        self.barrier()
        self.scopes.pop().close()
    def allgather(self, src_ap, dst_ap, dstbuf, groups):
        for key in list(self.cnt):
            if key.startswith('d') and self.cnt[key] > 0:
                self._need('pool', key, self.cnt[key])
        for key, val in dstbuf.r.items():
            self._need('pool', key, val)
        key = 'cc%d' % len([x for x in self.sem if x.startswith('cc')])
        sem = self.es.enter_context(self.nc.semaphore('s_' + key))
        ins = self.nc.gpsimd.collective_compute("AllGather", ALU.bypass, replica_groups=groups, ins=[src_ap.opt()], outs=[dst_ap.opt()])
        ins.then_inc(sem)
        self.sem[key] = sem; self.cnt[key] = 1
        dstbuf.w = (key, 1); dstbuf.r = {}
    def ind(self, out, out_off, in_, in_off, r=(), w=(), bounds=None):
        self._deps('pool', r, w)
        key = 'd%d' % self.dma_rr
        self.dma_rr = (self.dma_rr + 1) % self.nd
        if self.cnt[key] > 0:
            self._need('pool', key, self.cnt[key])
        kw = {}
        if bounds is not None:
            if not hasattr(self, '_breg'):
                self._breg = {}
            if bounds not in self._breg:
                self._breg[bounds] = self.nc.gpsimd.alloc_register('bnd%d' % len(self._breg))
            self.nc.gpsimd.reg_mov(self._breg[bounds], int(bounds))
            kw['bounds_check'] = self._breg[bounds]; kw['oob_is_err'] = False
        ins = self.nc.gpsimd.indirect_dma_start(out=out, out_offset=out_off, in_=in_, in_offset=in_off, **kw)
        self.cnt[key] += 16
        ins.then_inc(self.sem[key], 16)
        v = self.cnt[key]
        for b in r:
            b.r[key] = v
        for b in w:
            b.w = (key, v); b.r = {}
        return ins
    def finish(self):
        for i in range(self.nd):
            key = 'd%d' % i
            if self.cnt[key] > 0:
                self._need('sp', key, self.cnt[key])
    def close(self):
        self.es.close()

def _mm(self, out, lhsT, rhs, start=True, stop=True, r=(), w=()):
    return self.op('pe', lambda e: e.matmul(out, lhsT=lhsT, rhs=rhs, start=start, stop=stop), r=r, w=w)
def _tr(self, out, in_, ident, r=(), w=()):
    return self.op('pe', lambda e: e.transpose(out=out, in_=in_, identity=ident), r=r, w=w)
def _act(self, out, in_, func, bias=None, scale=1.0, accum_out=None, r=(), w=()):
    kw = {}
    if bias is not None: kw['bias'] = bias
    if accum_out is not None: kw['accum_out'] = accum_out
    return self.op('act', lambda e: e.activation(out=out, in_=in_, func=func, scale=scale, **kw), r=r, w=w)
def _tt(self, out, in0, in1, op, r=(), w=(), e='dve'):
    return self.op(e, lambda g: g.tensor_tensor(out=out, in0=in0, in1=in1, op=op), r=r, w=w)
def _ts(self, out, in0, s1, s2=None, op0=ALU.mult, op1=None, r=(), w=(), e='dve', accum_out=None):
    kw = {}
    if op1 is not None: kw['op1'] = op1
    if accum_out is not None: kw['accum_out'] = accum_out
    return self.op(e, lambda g: g.tensor_scalar(out=out, in0=in0, scalar1=s1, scalar2=s2, op0=op0, **kw), r=r, w=w)
def _stt(self, out, in0, scalar, in1, op0, op1, r=(), w=()):
    return self.op('dve', lambda g: g.scalar_tensor_tensor(out=out, in0=in0, scalar=scalar, in1=in1, op0=op0, op1=op1), r=r, w=w)
def _cp(self, out, in_, r=(), w=(), e='dve'):
    if e == 'act':
        return self.op('act', lambda g: g.activation(out=out, in_=in_, func=AF.Copy), r=r, w=w)
    return self.op(e, lambda g: g.tensor_copy(out=out, in_=in_), r=r, w=w)
def _barrier(self):
    for e in self.eng:
        for key in self.cnt:
            if key != e and self.cnt[key] > 0:
                self._need(e, key, self.cnt[key])
KB.mm = _mm; KB.tr = _tr; KB.act = _act; KB.tt = _tt; KB.ts = _ts; KB.stt = _stt; KB.cp = _cp; KB.barrier = _barrier


ALPHA = 4.0 ** 0.25
DEBUG = False
GBLK = 512
EPS = 1e-5

def layer_norm(k, src, dst, gbc, bbc, tmp):
    st, mv, rs = tmp
    k.op('dve', lambda e: e.bn_stats(out=st[:, 0:6], in_=src[:, 0:512]), r=[src], w=[st])
    k.op('dve', lambda e: e.bn_stats(out=st[:, 6:12], in_=src[:, 512:1024]), r=[src], w=[st])
    k.op('dve', lambda e: e.bn_aggr(out=mv[:, 0:2], in_=st[:, 0:12]), r=[st], w=[mv])
    k.act(rs[:, 0:1], mv[:, 1:2], AF.Sqrt, bias=k.eps_ap, r=[mv], w=[rs])
    k.op('dve', lambda e: e.reciprocal(out=rs[:, 1:2], in_=rs[:, 0:1]), r=[rs], w=[rs])
    k.stt(dst[:, :], src[:, :], mv[:, 0:1], gbc[:, :], ALU.subtract, ALU.mult, r=[src, mv, gbc], w=[dst])
    k.stt(dst[:, :], dst[:, :], rs[:, 1:2], bbc[:, :], ALU.mult, ALU.add, r=[dst, rs, bbc], w=[dst])

def post_consts(NT):
    G = min(GBLK, NT); J = max(1, NT // G); NOV = (2 * NT) // G - 1 if J > 1 else 0; NBLK = 32 + NOV
    c = {}
    c["ident"] = np.eye(128, dtype=np.float32)
    c["ut"] = np.triu(np.ones((128, 128), np.float32), 1)
    c["ones"] = np.ones((128, 128), np.float32)
    c["thr"] = np.tile(((np.arange(max(J - 1, 1), dtype=np.float32) + 1) * G)[None, None, :], (128, 32, 1)).reshape(128, 32 * max(J - 1, 1))
    c["biota"] = np.tile(np.arange(max(NOV, 1), dtype=np.float32)[None, :, None], (128, 1, 32)).reshape(128, max(NOV, 1) * 32)
    c["eiota"] = np.tile((np.arange(32, dtype=np.float32) * G)[None, :], (128, 1))
    c["iotap"] = np.arange(128, dtype=np.float32)[:, None].copy()
    return c

def build_post(NT, layer1, NE=32, ctx=None):
    own = ctx is None
    nc = bass.Bass("TRN2", target_bir_lowering=False) if own else ctx['nc']
    pfx = '' if own else ctx['pfx']
    over = {} if own else ctx['over']
    D = 1024
    ntile = NT // 128
    TG = min(GBLK, NT); ntg = NT // TG
    def din(name, shape):
        if name in over:
            return over[name]
        return nc.dram_tensor(pfx + name, shape, F32, kind="ExternalInput").ap()
    h_tok = din("h_tok", [NT, D]); yT = din("yT", [D, NT]) if 'yT_src' not in over else None; w_out = din("w_out", [D, D])
    ln = din("ln", [4, D])
    wr = din("wr", [D, 36]); br = din("br", [1, 36])
    w1r = din("w1r", [4096, 4096]); w3r = din("w3r", [4096, 4096]); w2r = din("w2r", [4096, 4096])
    G = min(GBLK, NT); J = max(1, NT // G); NOV = (2 * NT) // G - 1 if J > 1 else 0; NBLK = 32 + NOV; NSUB = G // 128; NROWS = NBLK * G
    JT = max(J - 1, 1); NOVA = max(NOV, 1)
    cd = {n: din(n, list(v.shape)) for n, v in post_consts(NT).items()}
    ident_d = cd["ident"]
    pfx_ = 'L1' if layer1 else 'L0'
    kind_ = "ExternalOutput" if DEBUG else "Internal"
    h1f_d = nc.dram_tensor(pfx_ + "h1f_d", [NT, D], F32, kind=kind_).ap(); h1b_d = nc.dram_tensor(pfx_ + "h1b_d", [NT, D], BF16, kind=kind_).ap()
    xs_d = nc.dram_tensor(pfx_ + "xs_d", [NROWS + 1, D], BF16, kind=kind_).ap(); yrows_d = nc.dram_tensor(pfx_ + "yrows_d", [NROWS, D], BF16, kind=kind_).ap()
    if layer1:
        glu_w = din("glu_w", [512, 512]); glu_b = din("glu_b", [128, 4])
    out = over["out"] if "out" in over else nc.dram_tensor(pfx + "out", [NT, D], F32, kind="ExternalOutput").ap()
    k = KB(nc) if own else ctx['k']
    if not own:
        k.push()
    ident = k.sb([128, 128]); k.dma(ident[:], ident_d, w=[ident])
    epsb = k.sb([128, 1]); k.op('pool', lambda e: e.memset(epsb[:], EPS), w=[epsb]); k.eps_ap = epsb[:, 0:1]
    cgate = [k.sb([128, 32], name='cg%d' % i) for i in range(ntile)]
    CC = {}
    for n_ in ["ut", "ones", "thr", "biota", "iotap", "eiota"]:
        CC[n_] = k.sb(list(post_consts(NT)[n_].shape), name='pc_' + n_); k.dma(CC[n_][:], cd[n_], w=[CC[n_]])
    identb = k.sb([128, 128], BF16); k.cp(identb[:], ident[:], r=[ident], w=[identb])
    ln2g = k.sb([128, D]); ln2b = k.sb([128, D])
    k.dma(ln2g[:], ln[2:3, :].partition_broadcast(128), w=[ln2g]); k.dma(ln2b[:], ln[3:4, :].partition_broadcast(128), w=[ln2b])
    st = k.sb([128, 12]); mv = k.sb([128, 2]); rs = k.sb([128, 2]); lntmp = (st, mv, rs)
    k.push()
    woutb = k.sb([128, 8, D], BF16); k.dma(woutb[:], w_out.rearrange("(kc p) d -> p kc d", p=128), w=[woutb], q='pool')
    ln1g = k.sb([128, D]); ln1b = k.sb([128, D])
    k.dma(ln1g[:], ln[0:1, :].partition_broadcast(128), w=[ln1g]); k.dma(ln1b[:], ln[1:2, :].partition_broadcast(128), w=[ln1b])
    wrs = k.sb([128, 8, 36]); k.dma(wrs[:], wr.rearrange("(kc p) n -> p kc n", p=128), w=[wrs])
    brb = k.sb([128, 36]); k.dma(brb[:], br.partition_broadcast(128), w=[brb])
    if layer1:
        glub = k.sb([128, 4, 512], BF16); k.dma(glub[:], glu_w.rearrange("(kc p) n -> p kc n", p=128), w=[glub], q='pool')
        glubias = k.sb([128, 4]); k.dma(glubias[:], glu_b, w=[glubias])
    htile = [k.sb([128, D]) for _ in range(2)]
    ytb = [k.sb([128, 8, 128], BF16) for _ in range(2)]
    ytfA = [k.sb([128, 4, 128]) for _ in range(2)]; ytfB = [k.sb([128, 4, 128]) for _ in range(2)]
    rbuf = [k.sb([128, D]) for _ in range(2)]
    h1 = [k.sb([128, D]) for _ in range(2)]
    h1bt = [k.sb([128, D], BF16) for _ in range(2)]
    h1Tf = [k.sb([128, 8, 128]) for _ in range(2)]
    pmix = k.ps([128, D]); pT = k.ps([128, D]); plg = k.ps([128, 36])
    pglu = k.ps([128, 512]) if layer1 else None
    sm = {n: k.sb([128, w_], name='sm_' + n) for n, w_ in [('lgs', 36), ('negm', 1), ('oh', 4), ('e4', 4), ('s4', 1), ('pg', 1), ('fs', 8), ('t8', 8), ('sel', 8), ('negm1', 1), ('ex', 8), ('e2', 1), ('coef', 1), ('cf', 8)]}
    if layer1:
        ysbs = [k.sb([128, 4, 128], BF16) for _ in range(2)]; zss = [k.sb([128, 4, 128]) for _ in range(2)]
    yT3 = yT.rearrange("(kc p) t -> p kc t", p=128) if yT is not None else None
    def load_yT(i, b):
        tsl_ = slice(i * 128, (i + 1) * 128)
        if yT3 is not None:
            k.dma(ytfA[b][:], yT3[:, 0:4, tsl_], w=[ytfA[b]])
            k.dma(ytfB[b][:], yT3[:, 4:8, tsl_], w=[ytfB[b]])
        else:
            if i == 0:
                over['yT_src'].pre()
            srcA, srcB, bufs = over['yT_src'](i)
            k.dma(ytfA[b][:], srcA, r=bufs, w=[ytfA[b]])
            k.dma(ytfB[b][:], srcB, r=bufs, w=[ytfB[b]])
    for i in range(ntile):
        b = i % 2
        tsl = slice(i * 128, (i + 1) * 128)
        k.dma(htile[b][:], h_tok[tsl, :], w=[htile[b]])
        if layer1:
            load_yT(i, b)
            k.cp(ytb[b][:, 4:8, :], ytfB[b][:], r=[ytfB[b]], w=[ytb[b]], e='act')
            ysb = ysbs[b]; zs = zss[b]
            k.cp(ysb[:], ytfA[b][:], r=[ytfA[b]], w=[ysb], e='act')
            for oc in range(4):
                for kc in range(4):
                    k.mm(pglu[:, oc * 128:(oc + 1) * 128], glub[:, kc, oc * 128:(oc + 1) * 128], ysb[:, kc, :], start=(kc == 0), stop=(kc == 3), r=[glub, ysb], w=[pglu])
            for oc in range(4):
                k.act(zs[:, oc, :], pglu[:, oc * 128:(oc + 1) * 128], AF.Sigmoid, bias=glubias[:, oc:oc + 1], r=[pglu, glubias], w=[zs])
            k.tt(ytb[b][:, 0:4, :], zs[:], ytfA[b][:], ALU.mult, r=[zs, ytfA[b]], w=[ytb[b]])
        else:
            load_yT(i, b)
            k.cp(ytb[b][:, 0:4, :], ytfA[b][:], r=[ytfA[b]], w=[ytb[b]], e='dve')
            k.cp(ytb[b][:, 4:8, :], ytfB[b][:], r=[ytfB[b]], w=[ytb[b]], e='act')
        for half in range(2):
            for kc in range(8):
                k.mm(pmix[:, half * 512:(half + 1) * 512], ytb[b][:, kc, :], woutb[:, kc, half * 512:(half + 1) * 512], start=(kc == 0), stop=(kc == 7), r=[ytb[b], woutb], w=[pmix])
        k.stt(rbuf[b][:, :], htile[b][:, :], ALPHA, pmix[:, :], ALU.mult, ALU.add, r=[htile[b], pmix], w=[rbuf[b]])
        layer_norm(k, rbuf[b], h1[b], ln1g, ln1b, lntmp)
        k.dma(h1f_d[tsl, :], h1[b][:, :], r=[h1[b]])
        k.cp(h1bt[b][:, :], h1[b][:, :], r=[h1[b]], w=[h1bt[b]], e='act')
        k.dma(h1b_d[tsl, :], h1bt[b][:, :], r=[h1bt[b]])
        for kc in range(8):
            k.tr(pT[:, kc * 128:(kc + 1) * 128], h1[b][:, kc * 128:(kc + 1) * 128], ident[:], r=[h1[b], ident], w=[pT])
        k.cp(h1Tf[b][:], pT[:, :].rearrange("p (kc t) -> p kc t", t=128), r=[pT], w=[h1Tf[b]], e='act')
        for kc in range(8):
            k.mm(plg[:, :], h1Tf[b][:, kc, :], wrs[:, kc, :], start=(kc == 0), stop=(kc == 7), r=[h1Tf[b], wrs], w=[plg])
        S = sm
        k.tt(S['lgs'][:], plg[:], brb[:], ALU.add, r=[plg, brb], w=[S['lgs']])
        k.op('dve', lambda e: e.reduce_max(out=S['negm'][:], in_=S['lgs'][:, 0:4], axis=AX.X), r=[S['lgs']], w=[S['negm']])
        k.ts(S['negm'][:], S['negm'][:], -1.0, None, op0=ALU.mult, r=[S['negm']], w=[S['negm']])
        k.ts(S['oh'][:], S['lgs'][:, 0:4], S['negm'][:, 0:1], 0.0, op0=ALU.add, op1=ALU.is_ge, r=[S['lgs'], S['negm']], w=[S['oh']])
        k.act(S['e4'][:], S['lgs'][:, 0:4], AF.Exp, bias=S['negm'][:, 0:1], accum_out=S['s4'][:, 0:1], r=[S['lgs'], S['negm']], w=[S['e4'], S['s4']])
        k.op('dve', lambda e: e.reciprocal(out=S['pg'][:], in_=S['s4'][:]), r=[S['s4']], w=[S['pg']])
        k.ts(S['fs'][:], S['lgs'][:, 4:12], S['oh'][:, 0:1], None, op0=ALU.mult, r=[S['lgs'], S['oh']], w=[S['fs']])
        for g in range(1, 4):
            k.stt(S['fs'][:], S['lgs'][:, 4 + 8 * g:12 + 8 * g], S['oh'][:, g:g + 1], S['fs'][:], ALU.mult, ALU.add, r=[S['lgs'], S['oh'], S['fs']], w=[S['fs']])
        k.op('dve', lambda e: e.max(out=S['t8'][:], in_=S['fs'][:]), r=[S['fs']], w=[S['t8']])
        k.ts(S['sel'][:], S['fs'][:], S['t8'][:, 1:2], None, op0=ALU.is_ge, r=[S['fs'], S['t8']], w=[S['sel']])
        k.ts(S['negm1'][:], S['t8'][:, 0:1], -1.0, None, op0=ALU.mult, r=[S['t8']], w=[S['negm1']])
        k.act(S['ex'][:], S['fs'][:], AF.Exp, bias=S['negm1'][:, 0:1], r=[S['fs'], S['negm1']], w=[S['ex']])
        k.act(S['e2'][:], S['t8'][:, 1:2], AF.Exp, bias=S['negm1'][:, 0:1], r=[S['t8'], S['negm1']], w=[S['e2']])
        k.ts(S['e2'][:], S['e2'][:], 1.0, None, op0=ALU.add, r=[S['e2']], w=[S['e2']])
        k.op('dve', lambda e: e.reciprocal(out=S['coef'][:], in_=S['e2'][:]), r=[S['e2']], w=[S['coef']])
        k.tt(S['coef'][:], S['coef'][:], S['pg'][:], ALU.mult, r=[S['coef'], S['pg']], w=[S['coef']])
        k.tt(S['cf'][:], S['ex'][:], S['sel'][:], ALU.mult, r=[S['ex'], S['sel']], w=[S['cf']])
        k.ts(S['cf'][:], S['cf'][:], S['coef'][:, 0:1], None, op0=ALU.mult, r=[S['cf'], S['coef']], w=[S['cf']])
        for g in range(4):
            k.ts(cgate[i][:, 8 * g:8 * g + 8], S['cf'][:], S['oh'][:, g:g + 1], None, op0=ALU.mult, r=[S['cf'], S['oh']], w=[cgate[i]])
    k.pop()
    didx = [k.sb([128, 4], I32, name='didx%d' % i) for i in range(ntile)]
    gts = [k.sb([128, 2], name='gts%d' % i) for i in range(ntile)]
    k.push()
    runb = k.sb([128, 32]); k.op('pool', lambda e: e.memset(runb[:], 0.0), w=[runb])
    mm_ = k.sb([128, 32]); ranks = [k.sb([128, 32], name='rank%d' % i) for i in range(ntile)]
    ppre = k.ps([128, 512], name='ppre_pAT')
    for i in range(ntile):
        k.ts(mm_[:], cgate[i][:], 0.0, None, op0=ALU.is_gt, r=[cgate[i]], w=[mm_])
        k.mm(ppre[:, 0:32], CC['ut'][:], mm_[:], r=[CC['ut'], mm_], w=[ppre])
        k.mm(ppre[:, 32:64], CC['ones'][:], mm_[:], r=[CC['ones'], mm_], w=[ppre])
        k.tt(ranks[i][:], ppre[:, 0:32], runb[:], ALU.add, r=[ppre, runb], w=[ranks[i]])
        k.tt(runb[:], ppre[:, 32:64], runb[:], ALU.add, r=[ppre, runb], w=[runb])
    cmp1 = k.sb([128, 32, JT]); nb = k.sb([128, 32]); pend = k.sb([128, 32]); delta = k.sb([128, 32]); ones32 = k.sb([128, 32])
    k.op('pool', lambda e: e.memset(ones32[:], 1.0), w=[ones32])
    if J > 1:
        k.tt(cmp1[:], runb[:].unsqueeze(2).to_broadcast([128, 32, JT]), CC['thr'][:].rearrange("p (e j) -> p e j", j=JT), ALU.is_gt, r=[runb, CC['thr']], w=[cmp1])
        k.op('dve', lambda e: e.reduce_sum(out=nb[:], in_=cmp1[:], axis=AX.X), r=[cmp1], w=[nb])
    else:
        k.op('pool', lambda e: e.memset(nb[:], 0.0), w=[nb])
    k.op('dve', lambda e: e.tensor_tensor_scan(out=pend[:], data0=ones32[:], data1=nb[:], initial=0.0, op0=ALU.mult, op1=ALU.add), r=[ones32, nb], w=[pend])
    k.tt(delta[:], pend[:], nb[:], ALU.subtract, r=[pend, nb], w=[delta])
    k.ts(delta[:], delta[:], float(G), float(31 * G), op0=ALU.mult, op1=ALU.add, r=[delta], w=[delta])
    k.tt(delta[:], delta[:], CC['eiota'][:], ALU.subtract, r=[delta, CC['eiota']], w=[delta])
    cmp2 = k.sb([128, NOVA, 32]); bef = k.sb([128, NOVA]); widx = k.sb([128, NOVA], I32)
    k.tt(cmp2[:], pend[:].unsqueeze(1).to_broadcast([128, NOVA, 32]), CC['biota'][:].rearrange("p (b e) -> p b e", e=32), ALU.is_le, r=[pend, CC['biota']], w=[cmp2])
    k.op('dve', lambda e: e.reduce_sum(out=bef[:], in_=cmp2[:], axis=AX.X), r=[cmp2], w=[bef])
    k.ts(bef[:], bef[:], 31.0, 128.0, op0=ALU.min, op1=ALU.mult, r=[bef], w=[bef])
    k.ts(bef[:], bef[:], CC['iotap'][:, 0:1], None, op0=ALU.add, r=[bef, CC['iotap']], w=[bef])
    k.cp(widx[:], bef[:], r=[bef], w=[widx])
    selb = k.sb([128, 32])
    Dm = k.sb([128, 32]); eq = k.sb([128, 32]); dd = k.sb([128, 4]); df = k.sb([128, 4]); dneg = k.sb([128, 2])
    for i in range(ntile):
        k.ts(mm_[:], cgate[i][:], 0.0, None, op0=ALU.is_gt, r=[cgate[i]], w=[mm_])
        k.ts(selb[:], ranks[i][:], float(G), None, op0=ALU.is_ge, r=[ranks[i]], w=[selb])
        k.tt(selb[:], selb[:], delta[:], ALU.mult, r=[selb, delta], w=[selb])
        k.tt(Dm[:], ranks[i][:], CC['eiota'][:], ALU.add, r=[ranks[i], CC['eiota']], w=[Dm])
        k.tt(Dm[:], Dm[:], selb[:], ALU.add, r=[Dm, selb], w=[Dm])
        k.stt(Dm[:], Dm[:], 1.0, mm_[:], ALU.add, ALU.mult, r=[Dm, mm_], w=[Dm])
        k.op('dve', lambda e: e.reduce_max(out=dd[:, 0:1], in_=Dm[:], axis=AX.X), r=[Dm], w=[dd])
        k.op('dve', lambda e: e.reduce_sum(out=dd[:, 1:2], in_=Dm[:], axis=AX.X), r=[Dm], w=[dd])
        k.ts(df[:, 0:1], dd[:, 0:1], -1.0, None, op0=ALU.add, r=[dd], w=[df])
        k.stt(df[:, 1:2], dd[:, 1:2], -1.0, dd[:, 0:1], ALU.add, ALU.subtract, r=[dd], w=[df])
        k.ts(dneg[:], df[:, 0:2], 0.0, None, op0=ALU.is_lt, r=[df], w=[dneg])
        k.ts(df[:, 2:4], df[:, 0:2], 0.0, None, op0=ALU.max, r=[df], w=[df])
        k.stt(df[:, 0:2], dneg[:], float(NROWS + 1), df[:, 0:2], ALU.mult, ALU.add, r=[dneg, df], w=[df])
        k.cp(didx[i][:], df[:], r=[df], w=[didx[i]])
        k.ts(eq[:], Dm[:], dd[:, 0:1], None, op0=ALU.is_equal, r=[Dm, dd], w=[eq])
        k.tt(eq[:], eq[:], cgate[i][:], ALU.mult, r=[eq, cgate[i]], w=[eq])
        k.op('dve', lambda e: e.reduce_sum(out=gts[i][:, 0:1], in_=eq[:], axis=AX.X), r=[eq], w=[gts[i]])
        k.op('dve', lambda e: e.reduce_sum(out=dd[:, 2:3], in_=cgate[i][:], axis=AX.X), r=[cgate[i]], w=[dd])
        k.tt(gts[i][:, 1:2], dd[:, 2:3], gts[i][:, 0:1], ALU.subtract, r=[dd, gts[i]], w=[gts[i]])
    hbt = [k.sb([128, D], BF16) for _ in range(2)]
    XS = Buf(None, 'xs')
    for i in range(ntile):
        b = i % 2
        k.dma(hbt[b][:], h1b_d[i * 128:(i + 1) * 128, :], w=[hbt[b]])
        k.ind(xs_d, bass.IndirectOffsetOnAxis(ap=didx[i][:, 0:1], axis=0), hbt[b][:], None, r=[didx[i], hbt[b]], bounds=NROWS)
        k.ind(xs_d, bass.IndirectOffsetOnAxis(ap=didx[i][:, 1:2], axis=0), hbt[b][:], None, r=[didx[i], hbt[b]], bounds=NROWS)
    if DEBUG:
        dbg_cg = nc.dram_tensor("dbg_cg", [NT, 32], F32, kind="ExternalOutput").ap()
        dbg_di = nc.dram_tensor("dbg_di", [NT, 2], I32, kind="ExternalOutput").ap()
        dbg_gt = nc.dram_tensor("dbg_gt", [NT, 2], F32, kind="ExternalOutput").ap()
        dbg_wi = nc.dram_tensor("dbg_wi", [128, NOVA], I32, kind="ExternalOutput").ap()
        dbg_rk = nc.dram_tensor("dbg_rk", [NT, 32], F32, kind="ExternalOutput").ap()
        for i in range(ntile):
            k.dma(dbg_cg[i * 128:(i + 1) * 128, :], cgate[i][:], r=[cgate[i]])
            k.dma(dbg_di[i * 128:(i + 1) * 128, :], didx[i][:, 0:2], r=[didx[i]])
            k.dma(dbg_gt[i * 128:(i + 1) * 128, :], gts[i][:], r=[gts[i]])
            k.dma(dbg_rk[i * 128:(i + 1) * 128, :], ranks[i][:], r=[ranks[i]])
        k.dma(dbg_wi, widx[:], r=[widx])
    k.barrier()
    w1s = [k.sb([128, 4096])] * 2; w3s = [k.sb([128, 4096])] * 2; w2s = [k.sb([128, 4096])] * 2
    w1b = [k.sb([128, 4096], BF16) for _ in range(2)]; w3b = [k.sb([128, 4096], BF16) for _ in range(2)]; w2b = [k.sb([128, 4096], BF16) for _ in range(2)]
    xt = [k.sb([128, NSUB, D], BF16) for _ in range(2)]
    XT = [k.sb([128, 8, 128], BF16) for _ in range(2)]
    sl = [k.sb([128, 512]) for _ in range(2)]; actb = [k.sb([128, 512], BF16) for _ in range(2)]
    actT = [k.sb([128, 4, 128], BF16) for _ in range(2)]
    yrow = [k.sb([128, D], BF16) for _ in range(2)]
    pXT = k.ps([128, D], BF16); ph1 = k.ps([128, 512]); ph3 = k.ps([128, 512]); pATv = ppre[:, :].bitcast(BF16)
    py = [k.ps([128, 512]) for _ in range(2)]
    def load_blk(bk):
        sb_ = bk % 2
        if bk < 32:
            k.dma(w1s[sb_][:], w1r[bk * 128:(bk + 1) * 128, :], w=[w1s[sb_]])
            k.dma(w3s[sb_][:], w3r[bk * 128:(bk + 1) * 128, :], w=[w3s[sb_]], q='act')
            k.dma(w2s[sb_][:], w2r[bk * 128:(bk + 1) * 128, :], w=[w2s[sb_]])
        else:
            off = bass.IndirectOffsetOnAxis(ap=widx[:, bk - 32:bk - 31], axis=0)
            k.ind(w1s[sb_][:], None, w1r, off, r=[widx], w=[w1s[sb_]], bounds=4095)
            k.ind(w3s[sb_][:], None, w3r, off, r=[widx], w=[w3s[sb_]], bounds=4095)
            k.ind(w2s[sb_][:], None, w2r, off, r=[widx], w=[w2s[sb_]], bounds=4095)
        k.dma(xt[sb_][:], xs_d[bk * G:(bk + 1) * G, :].rearrange("(s p) d -> p s d", p=128), w=[xt[sb_]])
    ph1b = [ph1, k.ps([128, 512])]; ph3b = [ph3, k.ps([128, 512])]
    U = NBLK * NSUB
    def casts(bk):
        sb_ = bk % 2
        k.cp(w1b[sb_][:], w1s[sb_][:], r=[w1s[sb_]], w=[w1b[sb_]], e='act')
        k.cp(w3b[sb_][:], w3s[sb_][:], r=[w3s[sb_]], w=[w3b[sb_]], e='dve')
        k.cp(w2b[sb_][:, 0:2048], w2s[sb_][:, 0:2048], r=[w2s[sb_]], w=[w2b[sb_]], e='act')
        k.cp(w2b[sb_][:, 2048:4096], w2s[sb_][:, 2048:4096], r=[w2s[sb_]], w=[w2b[sb_]], e='dve')
        if bk + 1 < NBLK:
            load_blk(bk + 1)
    def S1(u):
        bk, s_ = divmod(u, NSUB); q = u % 2
        for kc in range(8):
            k.tr(pXT[:, kc * 128:(kc + 1) * 128], xt[bk % 2][:, s_, kc * 128:(kc + 1) * 128], identb[:], r=[xt[bk % 2], identb], w=[pXT])
        k.cp(XT[q][:], pXT[:, :].rearrange("p (kc t) -> p kc t", t=128), r=[pXT], w=[XT[q]], e='act')
    def S2(u):
        bk, s_ = divmod(u, NSUB); q = u % 2; sb_ = bk % 2
        if s_ == 0:
            casts(bk)
        for kc in range(8):
            k.mm(ph1b[q][:, :], XT[q][:, kc, :], w1b[sb_][:, kc * 512:(kc + 1) * 512], start=(kc == 0), stop=(kc == 7), r=[XT[q], w1b[sb_]], w=[ph1b[q]])
        for kc in range(8):
            k.mm(ph3b[q][:, :], XT[q][:, kc, :], w3b[sb_][:, kc * 512:(kc + 1) * 512], start=(kc == 0), stop=(kc == 7), r=[XT[q], w3b[sb_]], w=[ph3b[q]])
        k.act(sl[q][:, :], ph1b[q][:, :], AF.Silu, r=[ph1b[q]], w=[sl[q]])
        k.tt(actb[q][:, :], sl[q][:, :], ph3b[q][:, :], ALU.mult, r=[sl[q], ph3b[q]], w=[actb[q]])
    def S3a(u):
        q = u % 2
        for hc in range(4):
            k.tr(pATv[:, hc * 128:(hc + 1) * 128], actb[q][:, hc * 128:(hc + 1) * 128], identb[:], r=[actb[q], identb], w=[ppre])
        k.cp(actT[q][:], pATv[:, 0:512].rearrange("p (hc t) -> p hc t", t=128), r=[ppre], w=[actT[q]], e='dve')
    def S3b(u):
        bk, s_ = divmod(u, NSUB); q = u % 2; sb_ = bk % 2
        for half in range(2):
            for hc in range(4):
                k.mm(py[half][:, :], actT[q][:, hc, :], w2b[sb_][:, hc * 1024 + half * 512:hc * 1024 + (half + 1) * 512], start=(hc == 0), stop=(hc == 3), r=[actT[q], w2b[sb_]], w=[py[half]])
        k.cp(yrow[q][:, 0:512], py[0][:, :], r=[py[0]], w=[yrow[q]], e='act')
        k.cp(yrow[q][:, 512:1024], py[1][:, :], r=[py[1]], w=[yrow[q]], e='dve')
        r0 = bk * G + s_ * 128
        k.dma(yrows_d[r0:r0 + 128, :], yrow[q][:, :], r=[yrow[q]])
    load_blk(0)
    S1(0)
    for step in range(U + 1):
        if step - 1 >= 0:
            S3a(step - 1)
        if step + 1 < U:
            S1(step + 1)
        if step < U:
            S2(step)
        if step - 1 >= 0:
            S3b(step - 1)
    k.pop()
    ob = [k.sb([128, D]) for _ in range(2)]
    hT_out = over.get('hT_dst')
    if hT_out is not None:
        pT3 = k.ps([128, D]); obT = [k.sb([128, 8, 128], BF16) for _ in range(2)]
    NB3 = 2
    hf = [k.sb([128, D]) for _ in range(NB3)]; rhi = [k.sb([128, D], BF16) for _ in range(NB3)]; rlo = [k.sb([128, D], BF16) for _ in range(NB3)]
    YR = Buf(None, 'yrows')
    for i in range(ntile):
        b = i % NB3
        k.dma(hf[b][:], h1f_d[i * 128:(i + 1) * 128, :], w=[hf[b]])
        k.ind(rhi[b][:], None, yrows_d, bass.IndirectOffsetOnAxis(ap=didx[i][:, 2:3], axis=0), r=[didx[i]], w=[rhi[b]], bounds=NROWS - 1)
        k.ind(rlo[b][:], None, yrows_d, bass.IndirectOffsetOnAxis(ap=didx[i][:, 3:4], axis=0), r=[didx[i]], w=[rlo[b]], bounds=NROWS - 1)
        if DEBUG:
            if i == 0:
                dbg_rhi = nc.dram_tensor("dbg_rhi", [NT, D], BF16, kind="ExternalOutput").ap(); dbg_pre = nc.dram_tensor("dbg_pre", [NT, D], F32, kind="ExternalOutput").ap()
            k.dma(dbg_rhi[i * 128:(i + 1) * 128, :], rhi[b][:, :], r=[rhi[b]])
        k.act(hf[b][:, :], hf[b][:, :], AF.Copy, scale=ALPHA, r=[hf[b]], w=[hf[b]])
        k.stt(hf[b][:, :], rhi[b][:, :], gts[i][:, 0:1], hf[b][:, :], ALU.mult, ALU.add, r=[rhi[b], gts[i], hf[b]], w=[hf[b]])
        k.stt(hf[b][:, :], rlo[b][:, :], gts[i][:, 1:2], hf[b][:, :], ALU.mult, ALU.add, r=[rlo[b], gts[i], hf[b]], w=[hf[b]])
        if DEBUG:
            k.dma(dbg_pre[i * 128:(i + 1) * 128, :], hf[b][:, :], r=[hf[b]])
        layer_norm(k, hf[b], ob[i % 2], ln2g, ln2b, lntmp)
        k.dma(out[i * 128:(i + 1) * 128, :], ob[i % 2][:, :], r=[ob[i % 2]])
        if hT_out is not None:
            for kc in range(8):
                k.tr(pT3[:, kc * 128:(kc + 1) * 128], ob[i % 2][:, kc * 128:(kc + 1) * 128], ident[:], r=[ob[i % 2], ident], w=[pT3])
            k.cp(obT[i % 2][:], pT3[:, :].rearrange("p (kc t) -> p kc t", t=128), r=[pT3], w=[obT[i % 2]], e='act')
            k.dma(hT_out(i), obT[i % 2][:], r=[obT[i % 2]])
            over['after_tile'](i)
    if own:
        k.finish()
        k.close()
        return nc
    k.pop()


TB = 512
NCH = 8
GN_EPS = 64e-5
NORM_EPS = 1e-5

def consts0():
    c = {}
    c["ident"] = np.eye(128, dtype=np.float32)
    bo = np.zeros((128, 128), np.float32); bo[:64, :64] = 1; bo[64:, 64:] = 1
    c["blockones"] = bo
    c["ones"] = np.ones((128, 128), np.float32)
    p = np.arange(128)[:, None] % 64; q = np.arange(512)[None, :] % 64
    c["m_us"] = (q > p).astype(np.float32)
    c["m_ui"] = (q >= p).astype(np.float32)
    c["m_ls"] = (p > q).astype(np.float32)
    c["ident4"] = np.tile(np.eye(128, dtype=np.float32), (1, 4))
    hm = np.zeros((128, 4), np.float32); hm[:64, 0] = 1; hm[64:, 1] = 1; hm[:64, 2] = -1; hm[64:, 3] = -1
    c["hm"] = hm
    rm = np.ones((128, 512), np.float32); rm[:, ::64] = 0
    c["resetm"] = rm
    return c

def build_mix0(L, ctx=None):
    own = ctx is None
    nc = bass.Bass("TRN2", target_bir_lowering=False) if own else ctx['nc']
    pfx = '' if own else ctx['pfx']
    over = {} if own else ctx['over']
    nblk = L // TB
    def din(name, shape):
        if name in over:
            return over[name]
        return nc.dram_tensor(pfx + name, shape, F32, kind="ExternalInput").ap()
    hT = din("hT", [1024, L])
    w_rw = din("w_rw", [1024, 640]); w_gla = din("w_gla", [1024, 400])
    rwvec = din("rwvec", [128, 16])
    lora_wa = din("lora_wa", [128, 128]); g2c = din("g2c", [128, 128])
    gk_w2 = din("gk_w2", [16, 64]); glavec = din("glavec", [128, 2])
    cd = {n: din(n, list(v.shape)) for n, v in consts0().items()}
    y_rw = None if "y_dst" in over else nc.dram_tensor("y_rw", [128, L], F32, kind="ExternalOutput").ap()
    y_gla = None if "y_dst" in over else nc.dram_tensor("y_gla", [128, L], F32, kind="ExternalOutput").ap()
    k = KB(nc) if own else ctx['k']
    if not own:
        k.push()
    C = {}
    for n, v in consts0().items():
        C[n] = k.sb(list(v.shape), name='c_' + n); k.dma(C[n][:], cd[n], w=[C[n]])
    vec = k.sb([128, 16]); k.dma(vec[:], rwvec, w=[vec])
    MU = lambda i: vec[:, i:i + 1]
    W0, A0, KK, KA, RK, GNG, GNB = [vec[:, 5 + i:6 + i] for i in range(7)]
    vx = k.sb([128, 4])
    k.ts(vx[:, 0:1], KA, -1.0, 1.0, op0=ALU.mult, op1=ALU.add, r=[vec], w=[vx])
    k.op('pool', lambda e: e.memset(vx[:, 1:2], GN_EPS), w=[vx])
    k.op('pool', lambda e: e.memset(vx[:, 2:3], NORM_EPS), w=[vx])
    gv = k.sb([128, 2]); k.dma(gv[:], glavec, w=[gv])
    k.ts(vx[:, 3:4], gv[:, 0:1], -1.0, None, op0=ALU.mult, r=[gv, vx], w=[vx])
    lwa = k.sb([128, 128]); k.dma(lwa[:], lora_wa, w=[lwa])
    g2s = k.sb([128, 128]); k.dma(g2s[:], g2c, w=[g2s])
    gkw = k.sb([16, 64]); k.dma(gkw[:], gk_w2, w=[gkw])
    wrwb = k.sb([128, 8, 640], BF16); k.dma(wrwb[:], w_rw.rearrange("(kc p) n -> p kc n", p=128), w=[wrwb], q='pool')
    wglb = k.sb([128, 8, 400], BF16); k.dma(wglb[:], w_gla.rearrange("(kc p) n -> p kc n", p=128), w=[wglb], q='pool')
    hTb = [k.sb([128, 8, TB], BF16) for _ in range(2)]
    hT3 = hT.rearrange("(kc p) t -> p kc t", p=128)
    pp = k.ps([128, 512]); pA = k.ps([128, 512])
    pM = k.ps([128, 512]); pN = k.ps([128, 512]); pP = k.ps([128, 512])
    pWUS = k.ps([128, 512]); pY = k.ps([128, 512]); pO = k.ps([128, 512])
    T = lambda name: k.sb([128, TB], name=name)
    psh = [k.sb([128, TB + 1], name='psh%d' % i) for i in range(5)]
    for b_ in psh:
        k.op('pool', lambda e: e.memset(b_[:, 0:1], 0.0), w=[b_])
    xs = [T('xs%d' % i) for i in range(5)]
    tmp = T('tmp'); tmp2 = T('tmp2')
    ld = T('ld'); a_sb = T('a_sb'); g_sb = T('g_sb'); kkn = T('kkn'); k2 = T('k2'); bvec = T('bvec'); bonus = T('bonus')
    bcum = T('bcum'); eb = T('eb'); enb = T('enb'); ebx = T('ebx')
    BD = lambda name: k.sb([128, NCH, 2, 64], name=name)
    r_bd, k_bd, b_bd, a_bd, kd_bd, bd_bd, v_bd = [BD(n) for n in ['r_bd', 'k_bd', 'b_bd', 'a_bd', 'kd_bd', 'bd_bd', 'v_bd']]
    AM = lambda name: k.sb([128, NCH, 128], name=name)
    AakT, ArbT, ArkT, Vtok, Kdt, Bdt = [AM(n) for n in ['AakT', 'ArbT', 'ArkT', 'Vtok', 'Kdt', 'Bdt']]
    NtA, MA, Pfin = [k.sb([128, NCH, 128], BF16, name=n) for n in ['NtA', 'MA', 'Pfin']]
    Ntb = [k.sb([128, 4, 128], BF16, name='Ntb%d' % i) for i in range(4)]
    Mb = [k.sb([128, 4, 128], BF16, name='Mb%d' % i) for i in range(4)]
    Pb = [k.sb([128, 4, 128], BF16, name='Pb%d' % i) for i in range(4)]
    ident4b = k.sb([128, 512], BF16, name='ident4b'); k.cp(ident4b[:], C['ident4'][:], r=[C['ident4']], w=[ident4b])
    Wsb = k.sb([128, 128], BF16, name='Wsb'); Usb = k.sb([128, 128], name='Usb')
    Sbd = [k.sb([128, 128], name='Sbd%d' % i) for i in range(2)]
    k.op('pool', lambda e: e.memset(Sbd[0][:], 0.0), w=[Sbd[0]])
    yT = T('yT'); yo = T('yo')
    gq = k.sb([64, TB], name='gq'); gk = k.sb([64, TB], name='gk'); ggate = T('ggate'); gkl = k.sb([16, TB], name='gkl')
    gvt = k.sb([64, NCH, 128], name='gvt')
    gsp = k.sb([64, TB], name='gsp'); gb = k.sb([64, TB], name='gb'); geb = k.sb([64, TB], name='geb'); genb = k.sb([64, TB], name='genb')
    gqt = k.sb([64, TB], name='gqt'); gkt = k.sb([64, TB], name='gkt'); gkd = k.sb([64, TB], name='gkd')
    gAT = k.sb([64, NCH, 64], name='gAT'); gkdt = k.sb([64, NCH, 64], name='gkdt')
    gS = [k.sb([64, 128], name='gS%d' % i) for i in range(2)]
    k.op('pool', lambda e: e.memset(gS[0][:], 0.0), w=[gS[0]])
    goT = yT; gsq = tmp2; gout = tmp
    sidx = 0; gsidx = 0
    c3 = lambda buf: buf[:, :].rearrange("p (c t) -> p c t", t=64)
    for blk in range(nblk):
        hb = hTb[blk % 2]
        cs = slice(blk * TB, (blk + 1) * TB)
        k.dma(hb[:], hT3[:, :, cs], w=[hb], q='pool')
        for ct in range(5):
            for kc in range(8):
                k.mm(pp[:, :], wrwb[:, kc, ct * 128:(ct + 1) * 128], hb[:, kc, :], start=(kc == 0), stop=(kc == 7), r=[wrwb, hb], w=[pp])
            k.cp(psh[ct][:, 1:TB + 1], pp[:, :], r=[pp], w=[psh[ct]], e='act')
            k.tt(tmp[:, :], psh[ct][:, 0:TB], psh[ct][:, 1:TB + 1], ALU.subtract, r=[psh[ct]], w=[tmp])
            k.stt(xs[ct][:, :], tmp[:, :], MU(ct), psh[ct][:, 1:TB + 1], ALU.mult, ALU.add, r=[tmp, psh[ct], vec], w=[xs[ct]])
            k.cp(psh[ct][:, 0:1], psh[ct][:, TB:TB + 1], r=[psh[ct]], w=[psh[ct]], e='pool')
        xr, xk, xv, xwa, xgl = xs
        k.act(xwa[0:64, :], xwa[0:64, :], AF.Tanh, r=[xwa], w=[xwa])
        k.mm(pp[:, :], lwa[0:64, :], xwa[0:64, :], r=[lwa, xwa], w=[pp])
        k.act(tmp[:, :], pp[:, :], AF.Sigmoid, bias=W0, r=[pp, vec], w=[tmp])
        k.act(ld[:, :], tmp[:, :], AF.Copy, scale=-0.6065306597126334, r=[tmp], w=[ld])
        k.mm(pA[:, :], lwa[64:128, :], xwa[64:128, :], r=[lwa, xwa], w=[pA])
        k.act(a_sb[:, :], pA[:, :], AF.Sigmoid, bias=A0, r=[pA, vec], w=[a_sb])
        k.act(tmp[:, :], xgl[:, :], AF.Sigmoid, r=[xgl], w=[tmp])
        k.mm(pp[:, :], g2s[:, :], tmp[:, :], r=[g2s, tmp], w=[pp])
        k.cp(g_sb[:, :], pp[:, :], r=[pp], w=[g_sb], e='act')
        k.act(kkn[:, :], xk[:, :], AF.Copy, scale=KK, r=[xk, vec], w=[kkn])
        k.tt(tmp[:, :], kkn[:, :], kkn[:, :], ALU.mult, r=[kkn], w=[tmp])
        k.mm(pA[:, :], C['blockones'][:, :], tmp[:, :], r=[C['blockones'], tmp], w=[pA])
        k.act(tmp2[:, :], pA[:, :], AF.Sqrt, r=[pA], w=[tmp2])
        k.ts(tmp2[:, :], tmp2[:, :], 1e-12, None, op0=ALU.max, r=[tmp2], w=[tmp2])
        k.op('dve', lambda e: e.reciprocal(out=tmp2[:, :], in_=tmp2[:, :]), r=[tmp2], w=[tmp2])
        k.tt(kkn[:, :], kkn[:, :], tmp2[:, :], ALU.mult, r=[kkn, tmp2], w=[kkn])
        k.ts(tmp[:, :], a_sb[:, :], KA, vx[:, 0:1], op0=ALU.mult, op1=ALU.add, r=[a_sb, vec, vx], w=[tmp])
        k.tt(k2[:, :], xk[:, :], tmp[:, :], ALU.mult, r=[xk, tmp], w=[k2])
        k.tt(bvec[:, :], kkn[:, :], a_sb[:, :], ALU.mult, r=[kkn, a_sb], w=[bvec])
        k.stt(tmp[:, :], xr[:, :], RK, k2[:, :], ALU.mult, ALU.mult, r=[xr, vec, k2], w=[tmp])
        k.mm(pp[:, :], C['blockones'][:, :], tmp[:, :], r=[C['blockones'], tmp], w=[pp])
        k.tt(bonus[:, :], pp[:, :], xv[:, :], ALU.mult, r=[pp, xv], w=[bonus])
        k.op('dve', lambda e: e.tensor_tensor_scan(out=bcum[:, :], data0=C['resetm'][:, :], data1=ld[:, :], initial=0.0, op0=ALU.mult, op1=ALU.add), r=[C['resetm'], ld], w=[bcum])
        k.act(eb[:, :], bcum[:, :], AF.Exp, r=[bcum], w=[eb])
        k.act(enb[:, :], bcum[:, :], AF.Exp, scale=-1.0, r=[bcum], w=[enb])
        k.tt(tmp[:, :], bcum[:, :], ld[:, :], ALU.subtract, r=[bcum, ld], w=[tmp])
        k.act(ebx[:, :], tmp[:, :], AF.Exp, r=[tmp], w=[ebx])
        gC = c3(eb)[:, :, 63:64].to_broadcast([128, NCH, 64])
        for h in range(2):
            hm = C['hm'][:, h:h + 1]; hmn = C['hm'][:, 2 + h:3 + h]
            k.stt(r_bd[:, :, h, :], c3(xr), hm, c3(eb), ALU.mult, ALU.mult, r=[xr, eb, C['hm']], w=[r_bd])
            k.stt(k_bd[:, :, h, :], c3(k2), hm, c3(enb), ALU.mult, ALU.mult, r=[k2, enb, C['hm']], w=[k_bd])
            k.stt(b_bd[:, :, h, :], c3(bvec), hm, c3(enb), ALU.mult, ALU.mult, r=[bvec, enb, C['hm']], w=[b_bd])
            k.stt(a_bd[:, :, h, :], c3(kkn), hmn, c3(ebx), ALU.mult, ALU.mult, r=[kkn, ebx, C['hm']], w=[a_bd])
            k.tt(kd_bd[:, :, h, :], k_bd[:, :, h, :], gC, ALU.mult, r=[k_bd, eb], w=[kd_bd])
            k.tt(bd_bd[:, :, h, :], b_bd[:, :, h, :], gC, ALU.mult, r=[b_bd, eb], w=[bd_bd])
            k.act(v_bd[:, :, h, :], c3(xv), AF.Copy, scale=hm, r=[xv, C['hm']], w=[v_bd])
        f2 = lambda bd, c: bd[:, c, :, :].rearrange("p h t -> p (h t)")
        def amat(dst, lbd, rbd, mask):
            for g4 in range(2):
                for cc in range(4):
                    c = g4 * 4 + cc
                    k.mm(pA[:, cc * 128:(cc + 1) * 128], f2(lbd, c), f2(rbd, c), r=[lbd, rbd], w=[pA])
                if mask is None:
                    k.cp(dst[:, g4 * 4:(g4 + 1) * 4, :], pA[:, :].rearrange("p (c t) -> p c t", t=128), r=[pA], w=[dst], e='act')
                else:
                    k.tt(dst[:, g4 * 4:(g4 + 1) * 4, :], pA[:, :].rearrange("p (c t) -> p c t", t=128), mask[:, :].rearrange("p (c t) -> p c t", t=128), ALU.mult, r=[pA, mask], w=[dst])
        amat(NtA, b_bd, a_bd, C['m_us'])
        amat(MA, a_bd, b_bd, C['m_ls'])
        amat(AakT, k_bd, a_bd, C['m_us'])
        amat(ArbT, b_bd, r_bd, C['m_ui'])
        amat(ArkT, k_bd, r_bd, C['m_ui'])
        def tmat(dst, src):
            for g4 in range(2):
                for cc in range(4):
                    c = g4 * 4 + cc
                    k.tr(pA[:, cc * 128:(cc + 1) * 128], f2(src, c), C['ident'][:, :], r=[src, C['ident']], w=[pA])
                k.cp(dst[:, g4 * 4:(g4 + 1) * 4, :], pA[:, :].rearrange("p (c t) -> p c t", t=128), r=[pA], w=[dst], e='act')
        tmat(Vtok, v_bd); tmat(Kdt, kd_bd); tmat(Bdt, bd_bd)
        def A3(buf):
            return buf[:, :, :] if len(buf.t.shape) == 3 else buf[:, :].rearrange("p (c t) -> p c t", t=128)
        sets = [dict(pM=pM, pN=pN, pP=pP, N=Ntb[0:2], M=Mb[0:2], P=Pb[0:2]),
                dict(pM=pp, pN=pA, pP=pO, N=Ntb[2:4], M=Mb[2:4], P=Pb[2:4])]
        stt_ = []
        i4 = ident4b[:, :].rearrange("p (c t) -> p c t", t=128)
        for g4 in range(2):
            S_ = sets[g4]; gs = slice(g4 * 4, (g4 + 1) * 4)
            k.tt(A3(S_['P'][0]), NtA[:, gs, :], i4, ALU.add, r=[NtA, ident4b], w=[S_['P'][0]])
            stt_.append(dict(N=None, M=None, pi=0))
        def Nsl(g4, c):
            s_ = stt_[g4]
            return (NtA[:, g4 * 4 + c, :], NtA) if s_['N'] is None else (A3(s_['N'])[:, c, :], s_['N'])
        def Msl(g4, c):
            s_ = stt_[g4]
            return (MA[:, g4 * 4 + c, :], MA) if s_['M'] is None else (A3(s_['M'])[:, c, :], s_['M'])
        for lvl in range(1, 6):
            for g4 in range(2):
                S_ = sets[g4]
                for c in range(4):
                    (na, nb_), (ma, mb_) = Nsl(g4, c), Msl(g4, c)
                    k.mm(S_['pM'][:, c * 128:(c + 1) * 128], na, ma, r=[nb_, mb_], w=[S_['pM']])
                if lvl < 5:
                    for c in range(4):
                        (na, nb_), (ma, mb_) = Nsl(g4, c), Msl(g4, c)
                        k.mm(S_['pN'][:, c * 128:(c + 1) * 128], ma, na, r=[nb_, mb_], w=[S_['pN']])
            for g4 in range(2):
                S_ = sets[g4]
                Mn = S_['M'][lvl % 2]; Nn = S_['N'][lvl % 2]
                k.cp(A3(Mn), S_['pM'][:, :].rearrange("p (c t) -> p c t", t=128), r=[S_['pM']], w=[Mn], e='act')
                if lvl < 5:
                    k.cp(A3(Nn), S_['pN'][:, :].rearrange("p (c t) -> p c t", t=128), r=[S_['pN']], w=[Nn], e='act')
            for g4 in range(2):
                S_ = sets[g4]; Mn = S_['M'][lvl % 2]; Pc = S_['P'][stt_[g4]['pi']]
                for c in range(4):
                    k.mm(S_['pP'][:, c * 128:(c + 1) * 128], A3(Mn)[:, c, :], A3(Pc)[:, c, :], r=[Mn, Pc], w=[S_['pP']])
            for g4 in range(2):
                S_ = sets[g4]; s_ = stt_[g4]; gs = slice(g4 * 4, (g4 + 1) * 4)
                Pc = S_['P'][s_['pi']]
                if lvl < 5:
                    Pn = S_['P'][1 - s_['pi']]
                    k.tt(A3(Pn), S_['pP'][:, :].rearrange("p (c t) -> p c t", t=128), A3(Pc), ALU.add, r=[S_['pP'], Pc], w=[Pn])
                    s_['pi'] = 1 - s_['pi']
                else:
                    k.tt(Pfin[:, gs, :], S_['pP'][:, :].rearrange("p (c t) -> p c t", t=128), A3(Pc), ALU.add, r=[S_['pP'], Pc], w=[Pfin])
                s_['M'] = S_['M'][lvl % 2]
                if lvl < 5:
                    s_['N'] = S_['N'][lvl % 2]
        for c in range(NCH):
            S = Sbd[sidx % 2]; Sn = Sbd[(sidx + 1) % 2]; sidx += 1
            k.mm(pWUS[:, 0:128], AakT[:, c, :], Vtok[:, c, :], start=True, stop=False, r=[AakT, Vtok], w=[pWUS])
            k.mm(pWUS[:, 0:128], f2(a_bd, c), S[:, :], start=False, stop=True, r=[a_bd, S], w=[pWUS])
            k.cp(Wsb[:, :], pWUS[:, 0:128], r=[pWUS], w=[Wsb], e='act')
            k.mm(pWUS[:, 128:256], Pfin[:, c, :], Wsb[:, :], r=[Pfin, Wsb], w=[pWUS])
            k.cp(Usb[:, :], pWUS[:, 128:256], r=[pWUS], w=[Usb], e='dve')
            cc = c % 4
            k.mm(pY[:, cc * 128:(cc + 1) * 128], Vtok[:, c, :], ArkT[:, c, :], start=True, stop=False, r=[Vtok, ArkT], w=[pY])
            k.mm(pY[:, cc * 128:(cc + 1) * 128], S[:, :], f2(r_bd, c), start=False, stop=False, r=[S, r_bd], w=[pY])
            k.mm(pY[:, cc * 128:(cc + 1) * 128], Usb[:, :], ArbT[:, c, :], start=False, stop=True, r=[Usb, ArbT], w=[pY])
            k.mm(pWUS[:, 256:384], Kdt[:, c, :], Vtok[:, c, :], start=True, stop=False, r=[Kdt, Vtok], w=[pWUS])
            k.mm(pWUS[:, 256:384], Bdt[:, c, :], Usb[:, :], start=False, stop=True, r=[Bdt, Usb], w=[pWUS])
            k.stt(Sn[:, :], S[:, :], eb[:, c * 64 + 63:c * 64 + 64], pWUS[:, 256:384], ALU.mult, ALU.add, r=[S, eb, pWUS], w=[Sn])
            if cc == 3:
                g4 = c // 4
                pv = pY[:, :].rearrange("p (c h t) -> p c h t", h=2, t=64)
                k.cp(yT[0:64, g4 * 256:(g4 + 1) * 256].rearrange("p (c t) -> p c t", t=64), pv[0:64, :, 0, :], r=[pY], w=[yT], e='act')
                k.cp(yT[64:128, g4 * 256:(g4 + 1) * 256].rearrange("p (c t) -> p c t", t=64), pv[64:128, :, 1, :], r=[pY], w=[yT], e='act')
        k.mm(pp[:, :], C['blockones'][:, :], yT[:, :], r=[C['blockones'], yT], w=[pp])
        k.stt(tmp[:, :], pp[:, :], -1.0 / 64, yT[:, :], ALU.mult, ALU.add, r=[pp, yT], w=[tmp])
        k.tt(tmp2[:, :], tmp[:, :], tmp[:, :], ALU.mult, r=[tmp], w=[tmp2])
        k.mm(pp[:, :], C['blockones'][:, :], tmp2[:, :], r=[C['blockones'], tmp2], w=[pp])
        k.act(tmp2[:, :], pp[:, :], AF.Sqrt, bias=vx[:, 1:2], scale=1.0 / 64, r=[pp, vx], w=[tmp2])
        k.op('dve', lambda e: e.reciprocal(out=tmp2[:, :], in_=tmp2[:, :]), r=[tmp2], w=[tmp2])
        k.tt(tmp[:, :], tmp[:, :], tmp2[:, :], ALU.mult, r=[tmp, tmp2], w=[tmp])
        k.ts(tmp[:, :], tmp[:, :], GNG, GNB, op0=ALU.mult, op1=ALU.add, r=[tmp, vec], w=[tmp])
        k.tt(tmp[:, :], tmp[:, :], bonus[:, :], ALU.add, r=[tmp, bonus], w=[tmp])
        k.tt(yo[:, :], tmp[:, :], g_sb[:, :], ALU.mult, r=[tmp, g_sb], w=[yo])
        k.dma(over['y_dst'](0, blk) if 'y_dst' in over else y_rw[:, cs], yo[:, :], r=[yo])
        for (dst, c0, c1) in [(gq, 0, 64), (gk, 64, 128), (ggate, 256, 384), (gkl, 384, 400)]:
            m = c1 - c0
            for kc in range(8):
                k.mm(pp[0:m, :], wglb[:, kc, c0:c1], hb[:, kc, :], start=(kc == 0), stop=(kc == 7), r=[wglb, hb], w=[pp])
            k.cp(dst[0:m, :], pp[0:m, :], r=[pp], w=[dst], e='act')
        for g4 in range(2):
            for cc in range(4):
                c = g4 * 4 + cc
                for kc in range(8):
                    k.mm(pA[0:64, cc * 128:(cc + 1) * 128], hb[:, kc, c * 64:(c + 1) * 64], wglb[:, kc, 128:256], start=(kc == 0), stop=(kc == 7), r=[wglb, hb], w=[pA])
            k.cp(gvt[:, g4 * 4:(g4 + 1) * 4, :], pA[0:64, :].rearrange("p (c t) -> p c t", t=128), r=[pA], w=[gvt], e='act')
        k.mm(pp[0:64, :], gkw[:, :], gkl[:, :], r=[gkw, gkl], w=[pp])
        k.act(gsp[:, :], pp[0:64, :], AF.Exp, bias=vx[0:64, 3:4], scale=-1.0, r=[pp, vx], w=[gsp])
        k.act(gsp[:, :], gsp[:, :], AF.Ln, bias=1.0, r=[gsp], w=[gsp])
        k.op('dve', lambda e: e.tensor_tensor_scan(out=gb[:, :], data0=C['resetm'][0:64, :], data1=gsp[:, :], initial=0.0, op0=ALU.mult, op1=ALU.add), r=[C['resetm'], gsp], w=[gb])
        k.act(geb[:, :], gb[:, :], AF.Exp, scale=-1.0 / 16, r=[gb], w=[geb])
        k.act(genb[:, :], gb[:, :], AF.Exp, scale=1.0 / 16, r=[gb], w=[genb])
        k.stt(gqt[:, :], gq[:, :], 0.125, geb[:, :], ALU.mult, ALU.mult, r=[gq, geb], w=[gqt])
        k.tt(gkt[:, :], gk[:, :], genb[:, :], ALU.mult, r=[gk, genb], w=[gkt])
        g3 = lambda buf: buf[:, :].rearrange("p (c t) -> p c t", t=64)
        k.tt(g3(gkd), g3(gkt), g3(geb)[:, :, 63:64].to_broadcast([64, NCH, 64]), ALU.mult, r=[gkt, geb], w=[gkd])
        for c in range(NCH):
            k.mm(pA[0:64, c * 64:(c + 1) * 64], gkt[:, c * 64:(c + 1) * 64], gqt[:, c * 64:(c + 1) * 64], r=[gkt, gqt], w=[pA])
        k.tt(gAT[:, :, :], pA[0:64, :].rearrange("p (c t) -> p c t", t=64), C['m_ui'][0:64, :].rearrange("p (c t) -> p c t", t=64), ALU.mult, r=[pA, C['m_ui']], w=[gAT])
        for c in range(NCH):
            k.tr(pA[0:64, c * 64:(c + 1) * 64], gkd[:, c * 64:(c + 1) * 64], C['ident'][0:64, 0:64], r=[gkd, C['ident']], w=[pA])
        k.cp(gkdt[:, :, :], pA[0:64, :].rearrange("p (c t) -> p c t", t=64), r=[pA], w=[gkdt], e='act')
        for c in range(NCH):
            S = gS[gsidx % 2]; Sn = gS[(gsidx + 1) % 2]; gsidx += 1
            k.mm(pO[:, c * 64:(c + 1) * 64], gvt[:, c, :], gAT[:, c, :], start=True, stop=False, r=[gvt, gAT], w=[pO])
            k.mm(pO[:, c * 64:(c + 1) * 64], S[:, :], gqt[:, c * 64:(c + 1) * 64], start=False, stop=True, r=[S, gqt], w=[pO])
            k.mm(pWUS[0:64, 384:512], gkdt[:, c, :], gvt[:, c, :], r=[gkdt, gvt], w=[pWUS])
            k.stt(Sn[:, :], S[:, :], geb[:, c * 64 + 63:c * 64 + 64], pWUS[0:64, 384:512], ALU.mult, ALU.add, r=[S, geb, pWUS], w=[Sn])
        k.cp(goT[:, :], pO[:, :], r=[pO], w=[goT], e='act')
        k.tt(gsq[:, :], goT[:, :], goT[:, :], ALU.mult, r=[goT], w=[gsq])
        k.mm(pp[:, :], C['ones'][:, :], gsq[:, :], r=[C['ones'], gsq], w=[pp])
        k.act(gsq[:, :], pp[:, :], AF.Sqrt, bias=vx[:, 2:3], scale=1.0 / 128, r=[pp, vx], w=[gsq])
        k.op('dve', lambda e: e.reciprocal(out=gsq[:, :], in_=gsq[:, :]), r=[gsq], w=[gsq])
        k.stt(goT[:, :], goT[:, :], gv[:, 1:2], gsq[:, :], ALU.mult, ALU.mult, r=[goT, gv, gsq], w=[goT])
        k.act(gsq[:, :], ggate[:, :], AF.Silu, r=[ggate], w=[gsq])
        k.tt(gout[:, :], goT[:, :], gsq[:, :], ALU.mult, r=[goT, gsq], w=[gout])
        k.dma(over['y_dst'](1, blk) if 'y_dst' in over else y_gla[:, cs], gout[:, :], r=[gout])
        if 'after_blk' in over:
            over['after_blk'](blk)
    if own:
        k.finish()
        k.close()
        return nc
    k.pop()

def mix0_inputs(d, hT_b, j):
    W = d['ab_w_in'][0]
    ch = slice(128 * j, 128 * j + 128)
    RW = 512
    o_r, o_wl, o_k, o_v, o_al, o_gl = 0, 512, 576, 1088, 1600, 1664
    cols = np.concatenate([np.arange(o_r + 128 * j, o_r + 128 * j + 128), np.arange(o_k + 128 * j, o_k + 128 * j + 128),
                           np.arange(o_v + 128 * j, o_v + 128 * j + 128), np.arange(o_wl, o_wl + 64), np.arange(o_al, o_al + 64),
                           np.arange(o_gl, o_gl + 128)])
    w_rw = np.ascontiguousarray(W[:, cols])
    mu = d['rw_mu'][0][cols].reshape(5, 128).T
    G0 = 1792
    gq, gk, gv, gkl, gg = G0, G0 + 256, G0 + 512, G0 + 1024, G0 + 1040
    gcols = np.concatenate([np.arange(gq + 64 * j, gq + 64 * j + 64), np.arange(gk + 64 * j, gk + 64 * j + 64),
                            np.arange(gv + 128 * j, gv + 128 * j + 128), np.arange(gg + 128 * j, gg + 128 * j + 128), np.arange(gkl, gkl + 16)])
    w_gla = np.ascontiguousarray(W[:, gcols])
    rwvec = np.zeros((128, 16), np.float32)
    rwvec[:, 0:5] = mu
    for i, n in enumerate(['rw_w0', 'rw_a0', 'rw_k_k', 'rw_k_a']):
        rwvec[:, 5 + i] = d[n][0][ch]
    rwvec[:, 9] = d['rw_r_k'][0].reshape(512)[ch]
    rwvec[:, 10] = d['rw_gn_g'][0][ch]; rwvec[:, 11] = d['rw_gn_b'][0][ch]
    lora_wa = np.concatenate([d['rw_w2'][0][:, ch], d['rw_a2'][0][:, ch]], 0)
    g2c = np.ascontiguousarray(d['rw_g2'][0][:, ch])
    gk_w2 = np.ascontiguousarray(d['gla_gk_w2'][0][:, 64 * j:64 * j + 64])
    glavec = np.zeros((128, 2), np.float32)
    glavec[:64, 0] = d['gla_gk_b'][0][64 * j:64 * j + 64]; glavec[:, 1] = d['gla_norm_g'][0]
    im = {"hT": np.ascontiguousarray(hT_b), "w_rw": w_rw, "w_gla": w_gla, "rwvec": rwvec, "lora_wa": np.ascontiguousarray(lora_wa), "g2c": g2c,
          "gk_w2": gk_w2, "glavec": glavec}
    im.update(consts0())
    return im


import math

TB = 512
NCH = 8
NORM_EPS = 1e-5
TWO_PI = 2.0 * math.pi

def consts1():
    c = {}
    c["ident"] = np.eye(128, dtype=np.float32)
    c["ones"] = np.ones((128, 128), np.float32)
    p = np.arange(128)[:, None] % 64; q = np.arange(512)[None, :] % 64
    c["m_ui"] = (q >= p).astype(np.float32)
    rm = np.ones((128, 512), np.float32); rm[:, ::64] = 0
    c["resetm"] = rm
    c["iota"] = np.tile(np.arange(512, dtype=np.float32)[None, :], (128, 1))
    return c

def sincos(k, u, sn, cs, t1, t2, ti, sl):
    def wrap(x):
        k.ts(sl(t2), sl(x), 0.5, None, op0=ALU.is_gt, r=[x], w=[t2])
        k.tt(sl(x), sl(x), sl(t2), ALU.subtract, r=[x, t2], w=[x])
        k.ts(sl(t2), sl(x), -0.5, None, op0=ALU.is_lt, r=[x], w=[t2])
        k.tt(sl(x), sl(x), sl(t2), ALU.add, r=[x, t2], w=[x])
    k.cp(sl(ti), sl(u), r=[u], w=[ti])
    k.cp(sl(t1), sl(ti), r=[ti], w=[t1])
    k.tt(sl(t1), sl(u), sl(t1), ALU.subtract, r=[u, t1], w=[t1])
    wrap(t1)
    k.act(sl(sn), sl(t1), AF.Sin, scale=TWO_PI, r=[t1], w=[sn])
    k.ts(sl(t1), sl(t1), 0.25, None, op0=ALU.add, r=[t1], w=[t1])
    wrap(t1)
    k.act(sl(cs), sl(t1), AF.Sin, scale=TWO_PI, r=[t1], w=[cs])

def build_mix1(L, ctx=None):
    own = ctx is None
    nc = bass.Bass("TRN2", target_bir_lowering=False) if own else ctx['nc']
    pfx = '' if own else ctx['pfx']
    over = {} if own else ctx['over']
    nblk = L // TB
    def din(name, shape):
        if name in over:
            return over[name]
        return nc.dram_tensor(pfx + name, shape, F32, kind="ExternalInput").ap()
    hT = din("hT", [1024, L]) if "hT_blk" not in over else None
    w_cd = din("w_cd", [1024, 640])
    Bre = din("Bre", [4, 128, 128]); Bim = din("Bim", [4, 128, 128]); Cre = din("Cre", [4, 128, 128]); Cim = din("Cim", [4, 128, 128])
    s5vec = din("s5vec", [128, 16]); hgvec = din("hgvec", [128, 4])
    cd = {n: din(n, list(v.shape)) for n, v in consts1().items()}
    y_s5 = None if "y_dst" in over else nc.dram_tensor("y_s5", [128, L], F32, kind="ExternalOutput").ap()
    y_hg = None if "y_dst" in over else nc.dram_tensor("y_hg", [128, L], F32, kind="ExternalOutput").ap()
    k = KB(nc) if own else ctx['k']
    if not own:
        k.push()
    C = {}
    for n, v in consts1().items():
        C[n] = k.sb(list(v.shape), name='c_' + n); k.dma(C[n][:], cd[n], w=[C[n]])
    sv = k.sb([128, 16]); k.dma(sv[:], s5vec, w=[sv])
    hv = k.sb([128, 4]); k.dma(hv[:], hgvec, w=[hv])
    BreS = k.sb([128, 4, 128]); BimS = k.sb([128, 4, 128]); CreS = k.sb([128, 4, 128]); CimS = k.sb([128, 4, 128])
    for (dst, src) in [(BreS, Bre), (BimS, Bim), (CreS, Cre), (CimS, Cim)]:
        k.dma(dst[:], src.rearrange("s p n -> p s n"), w=[dst])
    k.ts(CimS[:, :, :], CimS[:, :, :], -1.0, None, op0=ALU.mult, r=[CimS], w=[CimS])
    wcdb = k.sb([128, 8, 640], BF16); k.dma(wcdb[:], w_cd.rearrange("(kc p) n -> p kc n", p=128), w=[wcdb], q='pool')
    hTb = [k.sb([128, 8, TB], BF16) for _ in range(2)]
    hT3 = hT.rearrange("(kc p) t -> p kc t", p=128) if hT is not None else None
    pp = k.ps([128, 512]); pA = k.ps([128, 512]); pbr = k.ps([128, 512]); pbi = k.ps([128, 512])
    py = k.ps([128, 512]); pO = k.ps([128, 512]); pS = k.ps([128, 512])
    T = lambda name: k.sb([128, TB], name=name)
    S4 = lambda name: k.sb([128, 4], name=name)
    lre, lim, dt, rho, th, f4, sn4, cs4, t4a, t4b, are, aim, nre, den, zre, zim, u512, s512, c512 = [S4(n) for n in
        ['lre', 'lim', 'dt', 'rho', 'th', 'f4', 'sn4', 'cs4', 't4a', 't4b', 'are', 'aim', 'nre', 'den', 'zre', 'zim', 'u512', 's512', 'c512']]
    ti4 = k.sb([128, 4], I32, name='ti4')
    A_ = lambda b: b[:, :]
    k.ts(A_(lre), sv[:, 0:4], -1e-4, None, op0=ALU.min, r=[sv], w=[lre])
    k.cp(A_(lim), sv[:, 4:8], r=[sv], w=[lim])
    k.act(A_(dt), sv[:, 8:12], AF.Exp, r=[sv], w=[dt])
    k.tt(A_(t4a), A_(lre), A_(dt), ALU.mult, r=[lre, dt], w=[t4a])
    k.act(A_(rho), A_(t4a), AF.Exp, r=[t4a], w=[rho])
    k.tt(A_(th), A_(lim), A_(dt), ALU.mult, r=[lim, dt], w=[th])
    k.ts(A_(f4), A_(th), 1.0 / TWO_PI, None, op0=ALU.mult, r=[th], w=[f4])
    sincos(k, f4, sn4, cs4, t4a, t4b, ti4, A_)
    k.tt(A_(are), A_(rho), A_(cs4), ALU.mult, r=[rho, cs4], w=[are])
    k.tt(A_(aim), A_(rho), A_(sn4), ALU.mult, r=[rho, sn4], w=[aim])
    k.ts(A_(nre), A_(are), -1.0, None, op0=ALU.add, r=[are], w=[nre])
    k.tt(A_(den), A_(lre), A_(lre), ALU.mult, r=[lre], w=[den])
    k.tt(A_(t4a), A_(lim), A_(lim), ALU.mult, r=[lim], w=[t4a])
    k.tt(A_(den), A_(den), A_(t4a), ALU.add, r=[den, t4a], w=[den])
    k.op('dve', lambda e: e.reciprocal(out=A_(den), in_=A_(den)), r=[den], w=[den])
    k.tt(A_(zre), A_(nre), A_(lre), ALU.mult, r=[nre, lre], w=[zre])
    k.tt(A_(t4a), A_(aim), A_(lim), ALU.mult, r=[aim, lim], w=[t4a])
    k.tt(A_(zre), A_(zre), A_(t4a), ALU.add, r=[zre, t4a], w=[zre])
    k.tt(A_(zre), A_(zre), A_(den), ALU.mult, r=[zre, den], w=[zre])
    k.tt(A_(zim), A_(aim), A_(lre), ALU.mult, r=[aim, lre], w=[zim])
    k.tt(A_(t4a), A_(nre), A_(lim), ALU.mult, r=[nre, lim], w=[t4a])
    k.tt(A_(zim), A_(zim), A_(t4a), ALU.subtract, r=[zim, t4a], w=[zim])
    k.tt(A_(zim), A_(zim), A_(den), ALU.mult, r=[zim, den], w=[zim])
    k.ts(A_(u512), A_(f4), float(TB), None, op0=ALU.mult, r=[f4], w=[u512])
    sincos(k, u512, s512, c512, t4a, t4b, ti4, A_)
    cosl = [T('cosl%d' % s) for s in range(4)]; sinl = [T('sinl%d' % s) for s in range(4)]
    Ere = [T('Ere%d' % s) for s in range(4)]; Eim = [T('Eim%d' % s) for s in range(4)]
    rhoT = [T('rhoT%d' % s) for s in range(4)]
    tu = T('tu'); tt1 = T('tt1'); tt2 = T('tt2'); tti = k.sb([128, TB], I32, name='tti')
    for s in range(4):
        k.ts(tu[:, :], C['iota'][:, :], f4[:, s:s + 1], None, op0=ALU.mult, r=[C['iota'], f4], w=[tu])
        sincos(k, tu, sinl[s], cosl[s], tt1, tt2, tti, A_)
        k.ts(Ere[s][:, :], cosl[s][:, :], zre[:, s:s + 1], None, op0=ALU.mult, r=[cosl[s], zre], w=[Ere[s]])
        k.stt(Ere[s][:, :], sinl[s][:, :], zim[:, s:s + 1], Ere[s][:, :], ALU.mult, ALU.add, r=[sinl[s], zim, Ere[s]], w=[Ere[s]])
        k.ts(Eim[s][:, :], cosl[s][:, :], zim[:, s:s + 1], None, op0=ALU.mult, r=[cosl[s], zim], w=[Eim[s]])
        k.stt(tt1[:, :], sinl[s][:, :], zre[:, s:s + 1], Eim[s][:, :], ALU.mult, ALU.subtract, r=[sinl[s], zre, Eim[s]], w=[tt1])
        k.ts(Eim[s][:, :], tt1[:, :], -1.0, None, op0=ALU.mult, r=[tt1], w=[Eim[s]])
        k.op('pool', lambda e: e.memset(rhoT[s][:, :], 1.0), w=[rhoT[s]])
        k.ts(rhoT[s][:, :], rhoT[s][:, :], rho[:, s:s + 1], None, op0=ALU.mult, r=[rhoT[s], rho], w=[rhoT[s]])
    carry = [k.sb([128, 2], name='carry%d' % s) for s in range(4)]
    for s in range(4):
        k.op('pool', lambda e: e.memset(carry[s][:, :], 0.0), w=[carry[s]])
    ctmp = k.sb([128, 2], name='ctmp')
    hx = k.sb([128, 4], name='hx')
    k.tt(hx[:, 0:1], hv[:, 1:2], hv[:, 0:1], ALU.subtract, r=[hv], w=[hx])
    k.act(hx[:, 0:1], hx[:, 0:1], AF.Sigmoid, r=[hx], w=[hx])
    k.ts(hx[:, 1:2], hx[:, 0:1], -1.0, 1.0, op0=ALU.mult, op1=ALU.add, r=[hx], w=[hx])
    k.ts(hx[:, 2:3], hx[:, 1:2], -1.0, None, op0=ALU.mult, r=[hx], w=[hx])
    k.op('pool', lambda e: e.memset(hx[:, 3:4], NORM_EPS), w=[hx])
    uT = T('uT'); xre = T('xre'); xim = T('xim'); gre = T('gre'); gim = T('gim'); w1_ = T('w1_'); w2_ = T('w2_'); hre = T('hre'); him = T('him')
    ys = T('ys'); yo = T('yo')
    hq = T('hq'); hf = T('hf'); hgate = T('hgate'); hit = k.sb([64, NCH, 128], name='hit')
    hlg = T('hlg'); hb = T('hb'); heb = T('heb'); henb = T('henb'); hqt = T('hqt'); hkt = T('hkt'); hkd = T('hkd')
    hAT = k.sb([64, NCH, 64], name='hAT'); hkdt = k.sb([64, NCH, 128], name='hkdt')
    hS = [k.sb([128, 128], name='hS%d' % i) for i in range(2)]
    k.op('pool', lambda e: e.memset(hS[0][:], 0.0), w=[hS[0]])
    hoT = T('hoT'); hsq = T('hsq'); hout = T('hout')
    hsidx = 0
    for blk in range(nblk):
        hb_ = hTb[blk % 2]
        cs = slice(blk * TB, (blk + 1) * TB)
        if hT3 is not None:
            k.dma(hb_[:], hT3[:, :, cs], w=[hb_], q='pool')
        else:
            hsrc_, hbufs_ = over['hT_blk'](blk)
            k.dma(hb_[:], hsrc_, r=hbufs_, w=[hb_], q='pool')
        for (dst, c0) in [(uT, 0), (hq, 128), (hf, 256), (hgate, 512)]:
            for kc in range(8):
                k.mm(pp[:, :], wcdb[:, kc, c0:c0 + 128], hb_[:, kc, :], start=(kc == 0), stop=(kc == 7), r=[wcdb, hb_], w=[pp])
            k.cp(dst[:, :], pp[:, :], r=[pp], w=[dst], e='act')
        for g4 in range(2):
            for cc in range(4):
                c = g4 * 4 + cc
                for kc in range(8):
                    k.mm(pA[0:64, cc * 128:(cc + 1) * 128], hb_[:, kc, c * 64:(c + 1) * 64], wcdb[:, kc, 384:512], start=(kc == 0), stop=(kc == 7), r=[wcdb, hb_], w=[pA])
            k.cp(hit[:, g4 * 4:(g4 + 1) * 4, :], pA[0:64, :].rearrange("p (c t) -> p c t", t=128), r=[pA], w=[hit], e='act')
        for s in range(4):
            k.mm(pbr[:, :], BreS[:, s, :], uT[:, :], r=[BreS, uT], w=[pbr])
            k.mm(pbi[:, :], BimS[:, s, :], uT[:, :], r=[BimS, uT], w=[pbi])
            k.tt(xre[:, :], pbr[:, :], Ere[s][:, :], ALU.mult, r=[pbr, Ere[s]], w=[xre])
            k.tt(w1_[:, :], pbi[:, :], Eim[s][:, :], ALU.mult, r=[pbi, Eim[s]], w=[w1_])
            k.tt(xre[:, :], xre[:, :], w1_[:, :], ALU.subtract, r=[xre, w1_], w=[xre])
            k.tt(xim[:, :], pbr[:, :], Eim[s][:, :], ALU.mult, r=[pbr, Eim[s]], w=[xim])
            k.tt(w2_[:, :], pbi[:, :], Ere[s][:, :], ALU.mult, r=[pbi, Ere[s]], w=[w2_])
            k.tt(xim[:, :], xim[:, :], w2_[:, :], ALU.add, r=[xim, w2_], w=[xim])
            k.op('dve', lambda e: e.tensor_tensor_scan(out=gre[:, :], data0=rhoT[s][:, :], data1=xre[:, :], initial=carry[s][:, 0:1], op0=ALU.mult, op1=ALU.add), r=[rhoT[s], xre, carry[s]], w=[gre])
            k.op('dve', lambda e: e.tensor_tensor_scan(out=gim[:, :], data0=rhoT[s][:, :], data1=xim[:, :], initial=carry[s][:, 1:2], op0=ALU.mult, op1=ALU.add), r=[rhoT[s], xim, carry[s]], w=[gim])
            k.ts(ctmp[:, 0:1], gim[:, TB - 1:TB], s512[:, s:s + 1], None, op0=ALU.mult, r=[gim, s512], w=[ctmp])
            k.ts(ctmp[:, 1:2], gim[:, TB - 1:TB], c512[:, s:s + 1], None, op0=ALU.mult, r=[gim, c512], w=[ctmp])
            k.stt(carry[s][:, 0:1], gre[:, TB - 1:TB], c512[:, s:s + 1], ctmp[:, 0:1], ALU.mult, ALU.subtract, r=[gre, c512, ctmp], w=[carry[s]])
            k.stt(carry[s][:, 1:2], gre[:, TB - 1:TB], s512[:, s:s + 1], ctmp[:, 1:2], ALU.mult, ALU.add, r=[gre, s512, ctmp], w=[carry[s]])
            k.tt(hre[:, :], gre[:, :], cosl[s][:, :], ALU.mult, r=[gre, cosl[s]], w=[hre])
            k.tt(w1_[:, :], gim[:, :], sinl[s][:, :], ALU.mult, r=[gim, sinl[s]], w=[w1_])
            k.tt(hre[:, :], hre[:, :], w1_[:, :], ALU.subtract, r=[hre, w1_], w=[hre])
            k.tt(him[:, :], gre[:, :], sinl[s][:, :], ALU.mult, r=[gre, sinl[s]], w=[him])
            k.tt(w2_[:, :], gim[:, :], cosl[s][:, :], ALU.mult, r=[gim, cosl[s]], w=[w2_])
            k.tt(him[:, :], him[:, :], w2_[:, :], ALU.add, r=[him, w2_], w=[him])
            k.mm(py[:, :], CreS[:, s, :], hre[:, :], start=(s == 0), stop=False, r=[CreS, hre], w=[py])
            k.mm(py[:, :], CimS[:, s, :], him[:, :], start=False, stop=(s == 3), r=[CimS, him], w=[py])
        k.stt(ys[:, :], uT[:, :], sv[:, 12:13], py[:, :], ALU.mult, ALU.add, r=[uT, sv, py], w=[ys])
        k.tt(w1_[:, :], ys[:, :], ys[:, :], ALU.mult, r=[ys], w=[w1_])
        k.ts(w1_[:, :], w1_[:, :], 0.044715, 1.0, op0=ALU.mult, op1=ALU.add, r=[w1_], w=[w1_])
        k.tt(w1_[:, :], w1_[:, :], ys[:, :], ALU.mult, r=[w1_, ys], w=[w1_])
        k.act(w1_[:, :], w1_[:, :], AF.Sigmoid, scale=1.5957691216057308, r=[w1_], w=[w1_])
        k.tt(yo[:, :], ys[:, :], w1_[:, :], ALU.mult, r=[ys, w1_], w=[yo])
        k.dma(over['y_dst'](0, blk) if 'y_dst' in over else y_s5[:, cs], yo[:, :], r=[yo])
        k.act(hsq[:, :], hf[:, :], AF.Sigmoid, r=[hf], w=[hsq])
        k.ts(hlg[:, :], hsq[:, :], hx[:, 1:2], hx[:, 0:1], op0=ALU.mult, op1=ALU.add, r=[hsq, hx], w=[hlg])
        k.act(hlg[:, :], hlg[:, :], AF.Ln, r=[hlg], w=[hlg])
        k.ts(hkt[:, :], hsq[:, :], hx[:, 2:3], hx[:, 1:2], op0=ALU.mult, op1=ALU.add, r=[hsq, hx], w=[hkt])
        k.op('dve', lambda e: e.tensor_tensor_scan(out=hb[:, :], data0=C['resetm'][:, :], data1=hlg[:, :], initial=0.0, op0=ALU.mult, op1=ALU.add), r=[C['resetm'], hlg], w=[hb])
        k.act(heb[:, :], hb[:, :], AF.Exp, r=[hb], w=[heb])
        k.act(henb[:, :], hb[:, :], AF.Exp, scale=-1.0, r=[hb], w=[henb])
        k.act(hqt[:, :], hq[:, :], AF.Silu, r=[hq], w=[hqt])
        k.tt(hqt[:, :], hqt[:, :], heb[:, :], ALU.mult, r=[hqt, heb], w=[hqt])
        k.tt(hkt[:, :], hkt[:, :], henb[:, :], ALU.mult, r=[hkt, henb], w=[hkt])
        g3 = lambda buf: buf[:, :].rearrange("p (c t) -> p c t", t=64)
        k.tt(g3(hkd), g3(hkt), g3(heb)[:, :, 63:64].to_broadcast([128, NCH, 64]), ALU.mult, r=[hkt, heb], w=[hkd])
        for c in range(NCH):
            k.mm(pA[0:64, c * 64:(c + 1) * 64], hkt[:, c * 64:(c + 1) * 64], hqt[:, c * 64:(c + 1) * 64], r=[hkt, hqt], w=[pA])
        k.tt(hAT[:, :, :], pA[0:64, :].rearrange("p (c t) -> p c t", t=64), C['m_ui'][0:64, :].rearrange("p (c t) -> p c t", t=64), ALU.mult, r=[pA, C['m_ui']], w=[hAT])
        for g4 in range(2):
            for cc in range(4):
                c = g4 * 4 + cc
                k.tr(pA[0:64, cc * 128:(cc + 1) * 128], hkd[:, c * 64:(c + 1) * 64], C['ident'][:, :], r=[hkd, C['ident']], w=[pA])
            k.cp(hkdt[:, g4 * 4:(g4 + 1) * 4, :], pA[0:64, :].rearrange("p (c t) -> p c t", t=128), r=[pA], w=[hkdt], e='act')
        for c in range(NCH):
            S = hS[hsidx % 2]; Sn = hS[(hsidx + 1) % 2]; hsidx += 1
            k.mm(pO[:, c * 64:(c + 1) * 64], hit[:, c, :], hAT[:, c, :], start=True, stop=False, r=[hit, hAT], w=[pO])
            k.mm(pO[:, c * 64:(c + 1) * 64], S[:, :], hqt[:, c * 64:(c + 1) * 64], start=False, stop=True, r=[S, hqt], w=[pO])
            k.mm(pS[:, 0:128], hkdt[:, c, :], hit[:, c, :], r=[hkdt, hit], w=[pS])
            k.stt(Sn[:, :], S[:, :], heb[:, c * 64 + 63:c * 64 + 64], pS[:, 0:128], ALU.mult, ALU.add, r=[S, heb, pS], w=[Sn])
        k.cp(hoT[:, :], pO[:, :], r=[pO], w=[hoT], e='act')
        k.tt(hsq[:, :], hoT[:, :], hoT[:, :], ALU.mult, r=[hoT], w=[hsq])
        k.mm(pp[:, :], C['ones'][:, :], hsq[:, :], r=[C['ones'], hsq], w=[pp])
        k.act(hsq[:, :], pp[:, :], AF.Sqrt, bias=hx[:, 3:4], scale=1.0 / 128, r=[pp, hx], w=[hsq])
        k.op('dve', lambda e: e.reciprocal(out=hsq[:, :], in_=hsq[:, :]), r=[hsq], w=[hsq])
        k.stt(hoT[:, :], hoT[:, :], hv[:, 2:3], hsq[:, :], ALU.mult, ALU.mult, r=[hoT, hv, hsq], w=[hoT])
        k.act(hsq[:, :], hgate[:, :], AF.Silu, r=[hgate], w=[hsq])
        k.tt(hout[:, :], hoT[:, :], hsq[:, :], ALU.mult, r=[hoT, hsq], w=[hout])
        k.dma(over['y_dst'](1, blk) if 'y_dst' in over else y_hg[:, cs], hout[:, :], r=[hout])
        if 'after_blk' in over:
            over['after_blk'](blk)
    if own:
        k.finish()
        k.close()
        return nc
    k.pop()

def mix1_inputs(d, hT_b, j):
    W = d['cd_w_in'][0]
    cols = np.concatenate([np.arange(128 * j, 128 * j + 128)] + [np.arange(512 * m + 128 * j, 512 * m + 128 * j + 128) for m in (1, 2, 3, 4)])
    w_cd = np.ascontiguousarray(W[:, cols])
    Bre = np.zeros((4, 128, 128), np.float32); Bim = np.zeros_like(Bre); Cre = np.zeros_like(Bre); Cim = np.zeros_like(Bre)
    s5vec = np.zeros((128, 16), np.float32)
    for st in range(4):
        for gl in range(2):
            g = 8 * j + 2 * st + gl
            chs = slice((2 * st + gl) * 16, (2 * st + gl) * 16 + 16); ps = slice(gl * 64, gl * 64 + 64)
            Bre[st, chs, ps] = d['s5_b_re'][0][g].T; Bim[st, chs, ps] = d['s5_b_im'][0][g].T
            Cre[st, ps, chs] = d['s5_c_re'][0][g].T; Cim[st, ps, chs] = d['s5_c_im'][0][g].T
            s5vec[ps, st] = d['s5_a_re'][0][g]; s5vec[ps, 4 + st] = d['s5_a_im'][0][g]; s5vec[ps, 8 + st] = d['s5_log_dt'][0][g]
    s5vec[:, 12] = d['s5_d'][0][128 * j:128 * j + 128]
    hgvec = np.zeros((128, 4), np.float32)
    hgvec[:, 0] = d['hg_lb'][0][128 * j:128 * j + 128]; hgvec[:, 1] = d['hg_lb'][1][128 * j:128 * j + 128]; hgvec[:, 2] = d['hg_norm_g'][0]
    im = {"hT": np.ascontiguousarray(hT_b), "w_cd": w_cd, "Bre": Bre, "Bim": Bim, "Cre": Cre, "Cim": Cim, "s5vec": s5vec, "hgvec": hgvec}
    im.update(consts1())
    return im


GROUPS = [[0, 1, 2, 3], [4, 5, 6, 7]]

def build_fused(L, NT):
    nc = bass.Bass("TRN2", target_bir_lowering=False)
    k = KB(nc)
    D = 1024
    CW = min(1024, NT)
    NQY = L // CW; BPC = CW // TB
    HW_ = 512
    NQH = NT // HW_
    def ydram(tag):
        src = nc.dram_tensor(tag + "src", [NQY, 256, CW], F32).ap(); dst = nc.dram_tensor(tag + "all", [NQY, 1024, CW], F32).ap()
        return src, dst, [Buf(None, tag + 'all%d' % q) for q in range(NQY)]
    y0src, y0all, Y0 = ydram("y0"); y1src, y1all, Y1 = ydram("y1")
    h0tok = nc.dram_tensor("h0tok", [NT, D], F32).ap()
    h0Tsrc = nc.dram_tensor("h0Tsrc", [NQH, D, HW_], BF16).ap(); h0Tall = nc.dram_tensor("h0Tall", [NQH, 4 * D, HW_], BF16).ap()
    H0 = [Buf(None, 'h0Tall%d' % q) for q in range(NQH)]
    pid = nc.sync.partition_id()
    rank = pid % 4
    tag_of = {}
    def y_hooks(ysrc, yall, YB):
        def y_dst(which, blk):
            q = blk // BPC; c0 = (blk % BPC) * TB
            return ysrc[q][which * 128:(which + 1) * 128, c0:c0 + TB]
        def after_blk(blk):
            if blk % BPC == BPC - 1:
                q = blk // BPC
                k.allgather(ysrc[q], yall[q], YB[q], GROUPS)
        NM = NT // CW
        ymine = nc.dram_tensor(tag_of[id(ysrc)] + "mine", [NM, 1024, CW], F32).ap()
        YM = [[Buf(None, 'ym%d_%d' % (m, hh)) for hh in range(2)] for m in range(NM)]
        def pre():
            for m in range(NM):
                qe = rank * NM + m
                for hh in range(2):
                    rs = slice(hh * 512, (hh + 1) * 512)
                    k.dma(ymine[m][rs, :], yall[bass.ds(qe, 1)].rearrange("o r t -> (o r) t")[rs, :], r=YB, w=[YM[m][hh]])
        def yT_src(i):
            m = (i * 128) // CW
            c0 = (i * 128) % CW
            v = ymine[m].rearrange("(r two p) t -> p r two t", two=2, p=128)
            return v[:, :, 0, c0:c0 + 128], v[:, :, 1, c0:c0 + 128], YM[m]
        yT_src.pre = pre
        return y_dst, after_blk, yT_src
    tag_of[id(y0src)] = 'y0'; tag_of[id(y1src)] = 'y1'
    yd0, ab0, ys0 = y_hooks(y0src, y0all, Y0)
    yd1, ab1, ys1 = y_hooks(y1src, y1all, Y1)
    build_mix0(L, ctx=dict(nc=nc, k=k, pfx='m0_', over={"y_dst": yd0, "after_blk": ab0}))
    def hT_dst(i):
        q = (i * 128) // HW_; c0 = (i * 128) % HW_
        return h0Tsrc[q].rearrange("(kc p) t -> p kc t", p=128)[:, :, c0:c0 + 128]
    def after_tile(i):
        if ((i + 1) * 128) % HW_ == 0:
            q = (i * 128) // HW_
            k.allgather(h0Tsrc[q], h0Tall[q], H0[q], GROUPS)
    build_post(NT, False, ctx=dict(nc=nc, k=k, pfx='p0_', over={"yT_src": ys0, "out": h0tok, "hT_dst": hT_dst, "after_tile": after_tile}))
    def hT_blk(blk):
        r = blk // NQH; lb = blk % NQH
        return h0Tall[lb].rearrange("(r kc p) t -> p r kc t", r=4, p=128)[:, r, :, :], [H0[lb]]
    build_mix1(L, ctx=dict(nc=nc, k=k, pfx='m1_', over={"y_dst": yd1, "after_blk": ab1, "hT_blk": hT_blk}))
    build_post(NT, True, ctx=dict(nc=nc, k=k, pfx='p1_', over={"yT_src": ys1, "h_tok": h0tok}))
    k.finish()
    k.close()
    return nc

_CACHE = {}

def _post_inputs(d, L_, NT_):
    im = {"w_out": (d['ab_w_out'][0] if L_ == 0 else d['cd_w_out'][0]),
          "ln": np.stack([d['ln1_g'][L_], d['ln1_b'][L_], d['ln2_g'][L_], d['ln2_b'][L_]]),
          "wr": np.ascontiguousarray(np.concatenate([d['moe_wg'][L_], d['moe_we'][L_].transpose(1, 0, 2).reshape(1024, 32)], 1)),
          "br": np.concatenate([d['moe_bg'][L_], d['moe_be'][L_].reshape(32)])[None, :],
          "w1r": np.ascontiguousarray(d['moe_w1'][L_].reshape(32, 8, 128, 512).transpose(0, 2, 1, 3)).reshape(4096, 4096),
          "w3r": np.ascontiguousarray(d['moe_w3'][L_].reshape(32, 8, 128, 512).transpose(0, 2, 1, 3)).reshape(4096, 4096),
          "w2r": np.ascontiguousarray(d['moe_w2'][L_].reshape(32, 4, 128, 1024).transpose(0, 2, 1, 3)).reshape(4096, 4096)}
    im.update(post_consts(NT_))
    if L_ == 1:
        im["glu_w"] = d['s5_glu_w'][0]
        im["glu_b"] = np.ascontiguousarray(d["s5_glu_b"][0].reshape(4, 128).T)
    return im

def kernel(**inputs):
    d = {k_: np.asarray(v, dtype=np.float32) for k_, v in inputs.items()}
    x = d['x']
    B, L, D = x.shape
    NC = 8
    NT = B * L // NC
    cores = list(range(NC))
    if 'fused' not in _CACHE:
        _CACHE['fused'] = build_fused(L, NT)
    nc = _CACHE['fused']
    xT = [np.ascontiguousarray(x[b].T) for b in range(B)]
    xflat = x.reshape(B * L, D)
    p0 = _post_inputs(d, 0, NT); p1 = _post_inputs(d, 1, NT)
    ims = []
    for c in cores:
        b, j = c // 4, c % 4
        im = {}
        for n_, v_ in mix0_inputs(d, xT[b], j).items():
            im['m0_' + n_] = v_
        for n_, v_ in mix1_inputs(d, None, j).items():
            if n_ != 'hT':
                im['m1_' + n_] = v_
        for n_, v_ in p0.items():
            im['p0_' + n_] = v_
        for n_, v_ in p1.items():
            im['p1_' + n_] = v_
        im['p0_h_tok'] = np.ascontiguousarray(xflat[c * NT:(c + 1) * NT])
        ims.append(im)
    res = run_bass_kernel_spmd(nc, ims, core_ids=cores).results
    out = np.concatenate([res[c]["p1_out"] for c in cores], 0).reshape(B, L, D)
    return out.astype(np.float32)
```
